# Optimizing a Trainium2 kernel written in Bass

```python
import math
import jax, jax.numpy as jnp
from jax import lax
import numpy as np

D_MODEL = 1024
BATCH = 8
SEQ = 4096
DEPTH = 4

N_MIXERS = 3
EPS = 1e-6
D_FF = 2816

GLA_HEADS = 4
GLA_DK = D_MODEL // 2
GLA_DV = D_MODEL
GLA_DK_H = GLA_DK // GLA_HEADS
GLA_DV_H = GLA_DV // GLA_HEADS
GLA_GATE_RANK = 16
GLA_GATE_TEMP = 16.0
GLA_CHUNK = 64
GLA_IN = 2 * GLA_DK + 2 * GLA_DV + GLA_GATE_RANK

SSD_D_INNER = 2 * D_MODEL
SSD_HEAD_DIM = 64
SSD_HEADS = SSD_D_INNER // SSD_HEAD_DIM
SSD_GROUPS = 4
SSD_HPG = SSD_HEADS // SSD_GROUPS
SSD_STATE = 128
SSD_CONV = 4
SSD_CHUNK = 64
SSD_CONV_DIM = SSD_D_INNER + 2 * SSD_GROUPS * SSD_STATE
SSD_IN = SSD_D_INNER + SSD_CONV_DIM + SSD_HEADS

MLA_HEADS = 8
MLA_Q_LORA = 384
MLA_KV_LORA = 256
MLA_NOPE = 128
MLA_ROPE = 64
MLA_V = 128
MLA_QK = MLA_NOPE + MLA_ROPE
MLA_IN = MLA_Q_LORA + MLA_KV_LORA + MLA_ROPE
MLA_BLOCK = 128
ROPE_THETA = 10000.0

kernel_name = "hybrid_gla_ssd_mla_macaron"


def rmsnorm(x, w):
    xf = x.astype(jnp.float32)
    y = xf * lax.rsqrt(jnp.mean(xf * xf, axis=-1, keepdims=True) + EPS)
    return (y * w.astype(jnp.float32)).astype(x.dtype)


def swiglu(x, w_gu, w_down):
    g, u = jnp.split(x @ w_gu, 2, axis=-1)
    return (jax.nn.silu(g) * u) @ w_down


def gla_mixer(h, w_in, w_gate_b, b_gate, gn_w, w_out):
    bsz, s, _ = h.shape
    nc = s // GLA_CHUNK
    proj = h @ w_in
    q, k, v, g, a_lr = jnp.split(
        proj, [GLA_DK, 2 * GLA_DK, 2 * GLA_DK + GLA_DV, 2 * GLA_DK + 2 * GLA_DV], axis=-1)
    log_a = jax.nn.log_sigmoid((a_lr @ w_gate_b + b_gate).astype(jnp.float32)) / GLA_GATE_TEMP

    def heads(t, d):
        return t.reshape(bsz, nc, GLA_CHUNK, GLA_HEADS, d).astype(jnp.float32)

    q = heads(q, GLA_DK_H) * (GLA_DK_H ** -0.5)
    k = heads(k, GLA_DK_H)
    v = heads(v, GLA_DV_H)
    b = jnp.cumsum(heads(log_a, GLA_DK_H), axis=2)
    b_last = b[:, :, -1:]
    q_dec = q * jnp.exp(b)
    k_inv = k * jnp.exp(-b)
    k_end = k * jnp.exp(b_last - b)

    causal = jnp.tril(jnp.ones((GLA_CHUNK, GLA_CHUNK), dtype=bool))
    attn = jnp.einsum("bnihd,bnjhd->bnhij", q_dec, k_inv)
    attn = jnp.where(causal, attn, 0.0)
    o_intra = jnp.einsum("bnhij,bnjhv->bnihv", attn, v)

    def step(state, inp):
        qd, ke, vv, dl = inp
        o = jnp.einsum("bihd,bhdv->bihv", qd, state)
        state = state * dl[..., None] + jnp.einsum("bjhd,bjhv->bhdv", ke, vv)
        return state, o

    xs = (jnp.moveaxis(q_dec, 1, 0), jnp.moveaxis(k_end, 1, 0), jnp.moveaxis(v, 1, 0),
          jnp.moveaxis(jnp.exp(b_last[:, :, 0]), 1, 0))
    state0 = jnp.zeros((bsz, GLA_HEADS, GLA_DK_H, GLA_DV_H), jnp.float32)
    _, o_inter = lax.scan(step, state0, xs)
    o = (o_intra + jnp.moveaxis(o_inter, 0, 1)).reshape(bsz, s, GLA_HEADS, GLA_DV_H)

    o = o * lax.rsqrt(jnp.mean(o * o, axis=-1, keepdims=True) + EPS)
    o = o.reshape(bsz, s, GLA_DV) * gn_w.astype(jnp.float32)
    o = o * jax.nn.silu(g.astype(jnp.float32))
    return o.astype(h.dtype) @ w_out


def causal_depthwise_conv(u, w, b):
    k_w = w.shape[0]
    out = lax.conv_general_dilated(
        u, w[:, None, :].astype(u.dtype), window_strides=(1,), padding=((k_w - 1, 0),),
        dimension_numbers=("NWC", "WIO", "NWC"), feature_group_count=u.shape[-1])
    return out + b


def ssd_mixer(h, w_in, conv_w, conv_b, dt_bias, a_log, d_skip, norm_w, w_out):
    bsz, s, _ = h.shape
    nc = s // SSD_CHUNK
    G, R, P, N, Q = SSD_GROUPS, SSD_HPG, SSD_HEAD_DIM, SSD_STATE, SSD_CHUNK
    proj = h @ w_in
    z, xbc, dt = jnp.split(proj, [SSD_D_INNER, SSD_D_INNER + SSD_CONV_DIM], axis=-1)
    xbc = jax.nn.silu(causal_depthwise_conv(xbc, conv_w, conv_b))
    xs, bm, cm = jnp.split(xbc, [SSD_D_INNER, SSD_D_INNER + G * N], axis=-1)

    x = xs.reshape(bsz, nc, Q, G, R, P).astype(jnp.float32)
    bm = bm.reshape(bsz, nc, Q, G, N).astype(jnp.float32)
    cm = cm.reshape(bsz, nc, Q, G, N).astype(jnp.float32)
    dt = jax.nn.softplus(dt.astype(jnp.float32) + dt_bias.astype(jnp.float32))
    dt = dt.reshape(bsz, nc, Q, G, R)
    a_neg = -jnp.exp(a_log.astype(jnp.float32)).reshape(G, R)
    a_cs = jnp.cumsum(dt * a_neg, axis=2)
    xdt = x * dt[..., None]

    causal = jnp.tril(jnp.ones((Q, Q), dtype=bool))[:, :, None, None]
    seg = a_cs[:, :, :, None] - a_cs[:, :, None, :]
    L = jnp.exp(jnp.where(causal, seg, -jnp.inf))
    cb = jnp.einsum("bcign,bcjgn->bcgij", cm, bm)
    y_diag = jnp.einsum("bcgij,bcijgr,bcjgrp->bcigrp", cb, L, xdt)

    decay_end = jnp.exp(a_cs[:, :, -1:] - a_cs)
    total = jnp.exp(a_cs[:, :, -1])

    def step(state, inp):
        c_c, b_c, xdt_c, acs_c, dec_c, tot_c = inp
        y_off = jnp.einsum("bign,bgrpn,bigr->bigrp", c_c, state, jnp.exp(acs_c))
        state = state * tot_c[..., None, None] + jnp.einsum(
            "bjgn,bjgr,bjgrp->bgrpn", b_c, dec_c, xdt_c)
        return state, y_off

    scan_in = tuple(jnp.moveaxis(t, 1, 0) for t in (cm, bm, xdt, a_cs, decay_end, total))
    state0 = jnp.zeros((bsz, G, R, P, N), jnp.float32)
    _, y_off = lax.scan(step, state0, scan_in)
    y = y_diag + jnp.moveaxis(y_off, 0, 1) + x * d_skip.astype(jnp.float32).reshape(G, R)[..., None]
    y = y.reshape(bsz, s, SSD_D_INNER)

    y = y * jax.nn.silu(z.astype(jnp.float32))
    y = y.reshape(bsz, s, G, SSD_D_INNER // G)
    y = y * lax.rsqrt(jnp.mean(y * y, axis=-1, keepdims=True) + EPS)
    y = y.reshape(bsz, s, SSD_D_INNER) * norm_w.astype(jnp.float32)
    return y.astype(h.dtype) @ w_out


def apply_rope(t, cos, sin):
    half = t.shape[-1] // 2
    t1, t2 = t[..., :half], t[..., half:]
    return jnp.concatenate([t1 * cos - t2 * sin, t1 * sin + t2 * cos], axis=-1)


def mla_mixer(h, positions, w_in, q_norm, w_uq, kv_norm, w_ukv, w_out):
    bsz, s, _ = h.shape
    proj = h @ w_in
    cq, ckv, k_rope = jnp.split(proj, [MLA_Q_LORA, MLA_Q_LORA + MLA_KV_LORA], axis=-1)
    q = (rmsnorm(cq, q_norm) @ w_uq).reshape(bsz, s, MLA_HEADS, MLA_QK).astype(jnp.float32)
    kv = (rmsnorm(ckv, kv_norm) @ w_ukv).reshape(bsz, s, MLA_HEADS, MLA_NOPE + MLA_V)
    kv = kv.astype(jnp.float32)
    q_nope, q_rope = q[..., :MLA_NOPE], q[..., MLA_NOPE:]
    k_nope, v = kv[..., :MLA_NOPE], kv[..., MLA_NOPE:]

    inv_freq = 1.0 / (ROPE_THETA ** (jnp.arange(0, MLA_ROPE, 2, dtype=jnp.float32) / MLA_ROPE))
    ang = positions.astype(jnp.float32)[..., None] * inv_freq
    cos = jnp.cos(ang)[:, :, None, :]
    sin = jnp.sin(ang)[:, :, None, :]
    q_rope = apply_rope(q_rope, cos, sin)
    k_rope = apply_rope(k_rope.astype(jnp.float32)[:, :, None, :], cos, sin)

    q = jnp.concatenate([q_nope, q_rope], axis=-1) * (MLA_QK ** -0.5)
    k = jnp.concatenate(
        [k_nope, jnp.broadcast_to(k_rope, (bsz, s, MLA_HEADS, MLA_ROPE))], axis=-1)

    nb = s // MLA_BLOCK
    qb = jnp.moveaxis(q.reshape(bsz, nb, MLA_BLOCK, MLA_HEADS, MLA_QK), 1, 0)
    key_idx = jnp.arange(s)

    def block(args):
        qi, blk = args
        sc = jnp.einsum("bqhd,bkhd->bhqk", qi, k)
        q_idx = blk * MLA_BLOCK + jnp.arange(MLA_BLOCK)
        mask = key_idx[None, :] <= q_idx[:, None]
        p = jax.nn.softmax(jnp.where(mask, sc, -jnp.inf), axis=-1)
        return jnp.einsum("bhqk,bkhv->bqhv", p, v)

    o = lax.map(block, (qb, jnp.arange(nb)))
    o = jnp.moveaxis(o, 0, 1).reshape(bsz, s, MLA_HEADS * MLA_V)
    return o.astype(h.dtype) @ w_out


def setup_inputs(seed: int = 0) -> dict:
    key = jax.random.key(seed)
    ks = iter(jax.random.split(key, 64))
    f32 = jnp.float32

    def nrm(shape, scale):
        return scale * jax.random.normal(next(ks), shape, f32)

    def gain(n):
        return 1.0 + 0.01 * jax.random.normal(next(ks), (n,), f32)

    p = {}
    p["x"] = jax.random.normal(next(ks), (BATCH, SEQ, D_MODEL), f32)
    offs = jax.random.randint(next(ks), (BATCH, 1), 0, 1024, dtype=jnp.int32)
    p["positions"] = jnp.arange(SEQ, dtype=jnp.int32)[None, :] + offs
    for l in range(DEPTH):
        pre = "l%d_" % l
        p[pre + "ffn1_norm"] = gain(D_MODEL)
        p[pre + "ffn1_w_gu"] = nrm((D_MODEL, 2 * D_FF), D_MODEL ** -0.5)
        p[pre + "ffn1_w_down"] = nrm((D_FF, D_MODEL), D_FF ** -0.5)
        p[pre + "mix_norm"] = gain(D_MODEL)
        kind = l % N_MIXERS
        if kind == 0:
            p[pre + "gla_w_in"] = nrm((D_MODEL, GLA_IN), D_MODEL ** -0.5)
            p[pre + "gla_w_gate_b"] = nrm((GLA_GATE_RANK, GLA_DK), GLA_GATE_RANK ** -0.5)
            p[pre + "gla_b_gate"] = nrm((GLA_DK,), 0.01)
            p[pre + "gla_norm"] = gain(GLA_DV)
            p[pre + "gla_w_out"] = nrm((GLA_DV, D_MODEL), GLA_DV ** -0.5)
        elif kind == 1:
            p[pre + "ssd_w_in"] = nrm((D_MODEL, SSD_IN), D_MODEL ** -0.5)
            p[pre + "ssd_conv_w"] = nrm((SSD_CONV, SSD_CONV_DIM), SSD_CONV ** -0.5)
            p[pre + "ssd_conv_b"] = nrm((SSD_CONV_DIM,), 0.01)
            dt0 = jnp.exp(jax.random.uniform(next(ks), (SSD_HEADS,), f32,
                                             math.log(1e-3), math.log(1e-1)))
            p[pre + "ssd_dt_bias"] = dt0 + jnp.log(-jnp.expm1(-dt0))
            p[pre + "ssd_a_log"] = jnp.log(jax.random.uniform(next(ks), (SSD_HEADS,), f32, 1.0, 16.0))
            p[pre + "ssd_d_skip"] = gain(SSD_HEADS)
            p[pre + "ssd_norm"] = gain(SSD_D_INNER)
            p[pre + "ssd_w_out"] = nrm((SSD_D_INNER, D_MODEL), SSD_D_INNER ** -0.5)
        else:
            p[pre + "mla_w_in"] = nrm((D_MODEL, MLA_IN), D_MODEL ** -0.5)
            p[pre + "mla_q_norm"] = gain(MLA_Q_LORA)
            p[pre + "mla_w_uq"] = nrm((MLA_Q_LORA, MLA_HEADS * MLA_QK), MLA_Q_LORA ** -0.5)
            p[pre + "mla_kv_norm"] = gain(MLA_KV_LORA)
            p[pre + "mla_w_ukv"] = nrm((MLA_KV_LORA, MLA_HEADS * (MLA_NOPE + MLA_V)), MLA_KV_LORA ** -0.5)
            p[pre + "mla_w_out"] = nrm((MLA_HEADS * MLA_V, D_MODEL), (MLA_HEADS * MLA_V) ** -0.5)
        p[pre + "ffn2_norm"] = gain(D_MODEL)
        p[pre + "ffn2_w_gu"] = nrm((D_MODEL, 2 * D_FF), D_MODEL ** -0.5)
        p[pre + "ffn2_w_down"] = nrm((D_FF, D_MODEL), D_FF ** -0.5)
    p["final_norm"] = gain(D_MODEL)
    return p


def reference(x, positions,
              l0_ffn1_norm, l0_ffn1_w_gu, l0_ffn1_w_down, l0_mix_norm,
              l0_gla_w_in, l0_gla_w_gate_b, l0_gla_b_gate, l0_gla_norm, l0_gla_w_out,
              l0_ffn2_norm, l0_ffn2_w_gu, l0_ffn2_w_down,
              l1_ffn1_norm, l1_ffn1_w_gu, l1_ffn1_w_down, l1_mix_norm,
              l1_ssd_w_in, l1_ssd_conv_w, l1_ssd_conv_b, l1_ssd_dt_bias, l1_ssd_a_log,
              l1_ssd_d_skip, l1_ssd_norm, l1_ssd_w_out,
              l1_ffn2_norm, l1_ffn2_w_gu, l1_ffn2_w_down,
              l2_ffn1_norm, l2_ffn1_w_gu, l2_ffn1_w_down, l2_mix_norm,
              l2_mla_w_in, l2_mla_q_norm, l2_mla_w_uq, l2_mla_kv_norm, l2_mla_w_ukv, l2_mla_w_out,
              l2_ffn2_norm, l2_ffn2_w_gu, l2_ffn2_w_down,
              l3_ffn1_norm, l3_ffn1_w_gu, l3_ffn1_w_down, l3_mix_norm,
              l3_gla_w_in, l3_gla_w_gate_b, l3_gla_b_gate, l3_gla_norm, l3_gla_w_out,
              l3_ffn2_norm, l3_ffn2_w_gu, l3_ffn2_w_down,
              final_norm):
    layers = [
        ((l0_ffn1_norm, l0_ffn1_w_gu, l0_ffn1_w_down), l0_mix_norm, gla_mixer,
         (l0_gla_w_in, l0_gla_w_gate_b, l0_gla_b_gate, l0_gla_norm, l0_gla_w_out),
         (l0_ffn2_norm, l0_ffn2_w_gu, l0_ffn2_w_down)),
        ((l1_ffn1_norm, l1_ffn1_w_gu, l1_ffn1_w_down), l1_mix_norm, ssd_mixer,
         (l1_ssd_w_in, l1_ssd_conv_w, l1_ssd_conv_b, l1_ssd_dt_bias, l1_ssd_a_log,
          l1_ssd_d_skip, l1_ssd_norm, l1_ssd_w_out),
         (l1_ffn2_norm, l1_ffn2_w_gu, l1_ffn2_w_down)),
        ((l2_ffn1_norm, l2_ffn1_w_gu, l2_ffn1_w_down), l2_mix_norm, mla_mixer,
         (positions, l2_mla_w_in, l2_mla_q_norm, l2_mla_w_uq, l2_mla_kv_norm, l2_mla_w_ukv,
          l2_mla_w_out),
         (l2_ffn2_norm, l2_ffn2_w_gu, l2_ffn2_w_down)),
        ((l3_ffn1_norm, l3_ffn1_w_gu, l3_ffn1_w_down), l3_mix_norm, gla_mixer,
         (l3_gla_w_in, l3_gla_w_gate_b, l3_gla_b_gate, l3_gla_norm, l3_gla_w_out),
         (l3_ffn2_norm, l3_ffn2_w_gu, l3_ffn2_w_down)),
    ]
    h = x
    for i in range(DEPTH):
        ffn1, mix_norm, mixer, mparams, ffn2 = layers[i]
        h = h + 0.5 * swiglu(rmsnorm(h, ffn1[0]), ffn1[1], ffn1[2])
        h = h + mixer(rmsnorm(h, mix_norm), *mparams)
        h = h + 0.5 * swiglu(rmsnorm(h, ffn2[0]), ffn2[1], ffn2[2])
    return rmsnorm(h, final_norm)
```

```python
import contextlib
import numpy as np
import concourse.bass as bass
import concourse.mybir as mybir
from concourse.bass_utils import run_bass_kernel_spmd

F32 = mybir.dt.float32
BF16 = mybir.dt.bfloat16
I32 = mybir.dt.int32
AF = mybir.ActivationFunctionType
ALU = mybir.AluOpType
AX = mybir.AxisListType

D = 1024
DFF = 2816
NFC = DFF // 128
EPS = 1e-6
SBUF_BASE = 16640
SBUF_END = 229376
DEBUG_STOP = 0


class Buf:
    __slots__ = ("name", "last_w", "readers")

    def __init__(self, name=""):
        self.name = name
        self.last_w = None
        self.readers = []


class Op:
    __slots__ = ("idx", "eng", "fn", "deps", "sem", "val", "signal", "dma", "chan", "final", "used")

    def __init__(self):
        self.signal = False
        self.sem = None
        self.val = None
        self.final = False
        self.used = False


class Chan:
    __slots__ = ("sem", "count", "name", "rec")

    def __init__(self, name):
        self.name = name
        self.sem = None
        self.count = 0
        self.rec = 0


class Prog:
    ENGS = ("pe", "act", "dve", "pool", "sp")
    SAME_ENGINE_SYNC = True
    EPOCH = 8000
    CHAN_MAX = 480

    def __init__(self, nc):
        self.nc = nc
        self.ops = []
        self.chans = []
        self.fence_deps = []
        self.last_nd = {e: None for e in self.ENGS}
        self.pending_dma = {}
        self.free_chans = {}
        self.live_chans = []

    def buf(self, name=""):
        return Buf(name)

    def chan(self, name="", eng="sp"):
        kind = "sw" if eng == "pool" else "hw"
        fl = self.free_chans.setdefault(kind, [])
        if fl:
            c = fl.pop()
        else:
            c = Chan(f"{kind}{len(self.chans)}")
            self.chans.append(c)
        self.live_chans.append((kind, c))
        return c

    def op(self, eng, fn, reads=(), writes=(), dma=False, chan=None):
        o = Op()
        o.idx = len(self.ops)
        o.eng = eng
        o.fn = fn
        o.dma = dma
        o.chan = chan
        deps = set(self.fence_deps)
        for b in reads:
            if b.last_w is not None:
                deps.add(b.last_w)
        for b in writes:
            if b.last_w is not None:
                deps.add(b.last_w)
            deps.update(b.readers)
        o.deps = deps
        for d in deps:
            d.signal = True
            if d.dma:
                self.pending_dma.pop(d, None)
        if dma:
            assert chan is not None
            o.signal = True
            chan.rec += 1
            self.pending_dma[o] = True
        else:
            self.last_nd[eng] = o
        for b in reads:
            b.readers.append(o)
        for b in writes:
            b.last_w = o
            b.readers = []
        self.ops.append(o)
        return o

    def barrier(self):
        deps = [o for o in self.last_nd.values() if o is not None]
        deps += list(self.pending_dma.keys())
        for d in deps:
            d.signal = True
        self.fence_deps = deps
        for kind, c in self.live_chans:
            if c.rec < self.CHAN_MAX:
                self.free_chans.setdefault(kind, []).append(c)
        self.live_chans = []

    def emit(self):
        nc = self.nc
        per_eng = {e: [o for o in self.ops if o.eng == e] for e in self.ENGS}
        n_sig = {e: sum(1 for o in per_eng[e] if o.signal and not o.dma) for e in self.ENGS}
        stats = {e: [0, 0] for e in self.ENGS}
        with contextlib.ExitStack() as st:
            eng_sems = {}
            for e in self.ENGS:
                n_ep = max(1, (n_sig[e] + self.EPOCH - 1) // self.EPOCH)
                eng_sems[e] = [st.enter_context(nc.semaphore(f"s_{e}{i}")) for i in range(n_ep)]
            for c in self.chans:
                c.sem = st.enter_context(nc.semaphore(f"c_{c.name}"))
                c.count = 0
            for o in self.ops:
                if o.dma:
                    o.chan.count += 1
                    o.sem = o.chan.sem
                    o.val = 16 * o.chan.count
            for e in self.ENGS:
                cnt = 0
                for o in per_eng[e]:
                    if o.dma:
                        pass
                    elif o.signal:
                        ep = cnt // self.EPOCH
                        cnt += 1
                        o.sem = eng_sems[e][ep]
                        o.val = cnt - ep * self.EPOCH
            engobj = {"pe": nc.tensor, "act": nc.scalar, "dve": nc.vector, "pool": nc.gpsimd,
                      "sp": nc.sync}

            def run_engine(e):
                eng = engobj[e]
                waited = {}
                for o in per_eng[e]:
                    for d in sorted(o.deps, key=lambda d: d.idx):
                        if d.eng == e and not d.dma:
                            if e == "pe" or not self.SAME_ENGINE_SYNC:
                                continue
                        key = d.sem.num
                        if waited.get(key, 0) >= d.val:
                            continue
                        eng.wait_ge(d.sem, d.val)
                        stats[e][1] += 1
                        waited[key] = d.val
                    ins = o.fn(eng)
                    stats[e][0] += 1
                    if o.dma:
                        ins.then_inc(o.sem, 16)
                    elif o.signal:
                        ins.then_inc(o.sem, 1)
                if e == "sp":
                    for o in self.ops:
                        if o.dma and o.final and waited.get(o.sem.num, 0) < o.val:
                            eng.wait_ge(o.sem, o.val)
                            waited[o.sem.num] = o.val

            with nc.Block() as block:
                @block.tensor
                def _(eng):
                    run_engine("pe")

                @block.scalar
                def _(eng):
                    run_engine("act")

                @block.vector
                def _(eng):
                    run_engine("dve")

                @block.gpsimd
                def _(eng):
                    run_engine("pool")

                @block.sync
                def _(eng):
                    run_engine("sp")
        return stats


class Arena:
    def __init__(self, nc, base=SBUF_BASE, end=SBUF_END):
        self.nc = nc
        self.base = base
        self.end = end
        self.off = base
        self.n = 0

    def alloc(self, shape, dtype, name="t"):
        esz = 2 if dtype == BF16 else 4
        nbytes = int(np.prod(shape[1:])) * esz
        nbytes = (nbytes + 63) // 64 * 64
        assert self.off + nbytes <= self.end, f"SBUF overflow allocating {name} {shape}: off={self.off}"
        self.n += 1
        t = self.nc.alloc_sbuf_tensor_at(f"{name}_{self.n}", list(shape), dtype, offset=self.off)
        self.off += nbytes
        return t

    def mark(self):
        return self.off

    def reset(self, m):
        self.off = m


class Ring:
    def __init__(self, K, n, shape, dtype, name, chan=True, eng="sp"):
        self.items = []
        for i in range(n):
            t = K.A.alloc(shape, dtype, name)
            self.items.append((t, K.P.buf(f"{name}{i}"), K.P.chan(f"{name}{i}", eng) if chan else None))
        self.i = 0

    def next(self):
        it = self.items[self.i % len(self.items)]
        self.i += 1
        return it


class K:
    def __init__(self, S):
        self.S = S
        self.NCB = S // 512
        self.nc = bass.Bass("TRN2", target_bir_lowering=False)
        self.P = Prog(self.nc)
        self.A = Arena(self.nc)
        self.inputs = {}
        self.dram_aps = {}
        nc = self.nc
        self.ps = [nc.alloc_psum_tensor(f"ps{i}", [128, 512], F32) for i in range(6)]
        self.psb = [self.P.buf(f"ps{i}") for i in range(6)]
        self.pst = [nc.alloc_psum_tensor(f"pst{i}", [128, 1024], BF16) for i in range(2)]
        self.pstb = [self.P.buf(f"pst{i}") for i in range(2)]
        self.xT = nc.dram_tensor("xT_scr", [D, S], F32, kind="Internal").ap()
        self.xTb = [[self.P.buf(f"xT{c}_{cb}") for cb in range(self.NCB)] for c in range(8)]
        self.xTv = self.xT.rearrange("(c p) s -> p c s", p=128)

    def dram_in(self, name, shape, dtype=F32):
        if name in self.dram_aps:
            return self.dram_aps[name]
        t = self._dram_in(name, shape, dtype)
        self.dram_aps[name] = t
        return t

    def _dram_in(self, name, shape, dtype=F32):
        t = self.nc.dram_tensor(name, list(shape), dtype, kind="ExternalInput").ap()
        self.inputs[name] = (tuple(shape), dtype)
        return t

    def load_consts(self):
        P, A = self.P, self.A
        self.c_identf = A.alloc([128, 128], F32, "identf")
        self.c_identb = A.alloc([128, 128], BF16, "identb")
        self.c_onesb = A.alloc([128, 128], BF16, "onesb")
        d_ident = self.dram_in("c_ident", [128, 128])
        self.cb_const = P.buf("consts")
        ch = P.chan("const")
        P.op("sp", lambda e: e.dma_start(out=self.c_identf[:], in_=d_ident), writes=[self.cb_const],
             dma=True, chan=ch)
        P.op("dve", lambda e: e.tensor_copy(out=self.c_identb[:], in_=self.c_identf[:]),
             reads=[self.cb_const], writes=[self.cb_const])
        P.op("dve", lambda e: e.memset(self.c_onesb[:], 1.0), writes=[self.cb_const])
        self.c_eps = A.alloc([128, 1], F32, "eps")
        P.op("dve", lambda e: e.memset(self.c_eps[:], EPS), writes=[self.cb_const])

    def mm_group(self, ps_ap, pairs, reads, psbuf):
        n = len(pairs)

        def fn(e):
            ins = None
            for i, (l, r) in enumerate(pairs):
                ins = e.matmul(ps_ap, l, r, start=(i == 0), stop=(i == n - 1))
            return ins
        return self.P.op("pe", fn, reads=reads, writes=[psbuf])

    def norm_block(self, cb, xin, xin_b, sq, sq_b, rstd, rstd_b, nw, nw_b, ps_i, out_fn):
        P = self.P
        P.op("act", lambda e: e.activation(out=sq[:], in_=xin[:], func=AF.Square),
             reads=[xin_b], writes=[sq_b])
        ps, psb = self.ps[ps_i], self.psb[ps_i]
        self.mm_group(ps[:], [(self.c_onesb[:], sq[:, c, :]) for c in range(8)],
                      [sq_b, self.cb_const], psb)
        P.op("act", lambda e: e.activation(out=rstd[:], in_=ps[:], func=AF.Sqrt, bias=self.c_eps[:], scale=1.0 / D),
             reads=[psb, self.cb_const], writes=[rstd_b])
        P.op("dve", lambda e: e.reciprocal(out=rstd[:], in_=rstd[:]), reads=[rstd_b], writes=[rstd_b])
        for c in range(8):
            o_ap, o_b = out_fn(c)
            P.op("dve", (lambda e, c=c, o_ap=o_ap: e.scalar_tensor_tensor(
                out=o_ap, in0=xin[:, c, :], scalar=nw[:, c:c + 1], in1=rstd[:],
                op0=ALU.mult, op1=ALU.mult)),
                reads=[xin_b, rstd_b, nw_b], writes=[o_b])

    def load_xcols(self, cb, xin, xin_b, ch, eng="sp"):
        return self.P.op(eng, lambda e: e.dma_start(out=xin[:], in_=self.xTv[:, :, cb * 512:(cb + 1) * 512]),
                         reads=[self.xTb[c][cb] for c in range(8)], writes=[xin_b], dma=True, chan=ch)

    def stage_in(self):
        P, A, S = self.P, self.A, self.S
        x = self.dram_in("x", [S, D])
        m = A.mark()
        rin = Ring(self, 2, [128, D], F32, "s0in")
        rout = Ring(self, 2, [128, 8, 512], F32, "s0out")
        for cb in range(self.NCB):
            xo, xo_b, xo_ch = rout.next()
            for tb in range(4):
                t0 = cb * 512 + tb * 128
                xi, xi_b, xi_ch = rin.next()
                P.op("sp", lambda e, xi=xi, t0=t0: e.dma_start(out=xi[:], in_=x[t0:t0 + 128, :]),
                     writes=[xi_b], dma=True, chan=xi_ch)
                for half in range(2):
                    pi = (tb * 2 + half) % 4
                    ps, psb = self.ps[pi], self.psb[pi]

                    def fn(e, xi=xi, ps=ps, half=half):
                        ins = None
                        for j in range(4):
                            c = half * 4 + j
                            ins = e.transpose(ps[:, j * 128:(j + 1) * 128], xi[:, c * 128:(c + 1) * 128],
                                              self.c_identf[:])
                        return ins
                    P.op("pe", fn, reads=[xi_b, self.cb_const], writes=[psb])
                    eng = "act" if half == 0 else "dve"

                    def cp(e, xo=xo, ps=ps, half=half, tb=tb, eng=eng):
                        o = xo[:, half * 4:(half + 1) * 4, tb * 128:(tb + 1) * 128]
                        i = ps[:].rearrange("p (j t) -> p j t", j=4)
                        if eng == "act":
                            return e.activation(out=o, in_=i, func=AF.Copy)
                        return e.tensor_copy(out=o, in_=i)
                    P.op(eng, cp, reads=[psb], writes=[xo_b])
            P.op("sp", lambda e, xo=xo, cb=cb: e.dma_start(out=self.xTv[:, :, cb * 512:(cb + 1) * 512], in_=xo[:]),
                 reads=[xo_b], writes=[self.xTb[c][cb] for c in range(8)], dma=True, chan=xo_ch)
        P.barrier()
        A.reset(m)

    def stage_out(self):
        P, A, S = self.P, self.A, self.S
        out = self.nc.dram_tensor("out", [S, D], F32, kind="ExternalOutput").ap()
        nw_d = self.dram_in("final_norm", [128, 8])
        m = A.mark()
        nw = A.alloc([128, 8], F32, "nw")
        nw_b = P.buf("nw")
        P.op("sp", lambda e: e.dma_start(out=nw[:], in_=nw_d), writes=[nw_b], dma=True, chan=P.chan("nwf"))
        rin = Ring(self, 2, [128, 8, 512], F32, "fin")
        sq = A.alloc([128, 8, 512], BF16, "fsq")
        sq_b = P.buf("fsq")
        rstd = A.alloc([128, 512], F32, "frstd")
        rstd_b = P.buf("frstd")
        xnf = A.alloc([128, 8, 512], F32, "fxn")
        xnf_b = [P.buf(f"fxn{c}") for c in range(8)]
        rout = Ring(self, 2, [128, D], F32, "fout")
        for cb in range(self.NCB):
            xin, xin_b, xin_ch = rin.next()
            self.load_xcols(cb, xin, xin_b, xin_ch)
            self.norm_block(cb, xin, xin_b, sq, sq_b, rstd, rstd_b, nw, nw_b, 5,
                            lambda c: (xnf[:, c, :], xnf_b[c]))
            for tb in range(4):
                yo, yo_b, yo_ch = rout.next()
                for half in range(2):
                    pi = (tb * 2 + half) % 4
                    ps, psb = self.ps[pi], self.psb[pi]

                    def fn(e, ps=ps, half=half, tb=tb):
                        ins = None
                        for j in range(4):
                            c = half * 4 + j
                            ins = e.transpose(ps[:, j * 128:(j + 1) * 128],
                                              xnf[:, c, tb * 128:(tb + 1) * 128], self.c_identf[:])
                        return ins
                    P.op("pe", fn, reads=[xnf_b[half * 4 + j] for j in range(4)] + [self.cb_const], writes=[psb])
                    eng = "act" if half == 0 else "dve"

                    def cp(e, yo=yo, ps=ps, half=half, eng=eng):
                        o = yo[:, half * 512:(half + 1) * 512]
                        if eng == "act":
                            return e.activation(out=o, in_=ps[:], func=AF.Copy)
                        return e.tensor_copy(out=o, in_=ps[:])
                    P.op(eng, cp, reads=[psb], writes=[yo_b])
                t0 = cb * 512 + tb * 128
                o = P.op("sp", lambda e, yo=yo, t0=t0: e.dma_start(out=out[t0:t0 + 128, :], in_=yo[:]),
                         reads=[yo_b], dma=True, chan=yo_ch)
                o.final = True
        A.reset(m)

    def ffn(self, pre):
        P, A, S, NCB = self.P, self.A, self.S, self.NCB
        nw_d = self.dram_in(pre + "_norm", [128, 8])
        wgu_d = self.dram_in(pre + "_w_gu", [NFC, 128, 2 * 8 * 128])
        wd_d = self.dram_in(pre + "_w_down", [2, 8, 128, 11 * 128])
        m0 = A.mark()
        nw = A.alloc([128, 8], F32, "nw")
        nw_b = P.buf("nw")
        P.op("sp", lambda e: e.dma_start(out=nw[:], in_=nw_d), writes=[nw_b], dma=True, chan=P.chan("nw"))
        xn = A.alloc([128, 8, S], BF16, "xn")
        xn_b = [P.buf(f"xn{cb}") for cb in range(NCB)]
        m1 = A.mark()
        rin = Ring(self, 2, [128, 8, 512], F32, "xin")
        sq = A.alloc([128, 8, 512], BF16, "sq")
        sq_b = P.buf("sq")
        rstd = A.alloc([128, 512], F32, "rstd")
        rstd_b = P.buf("rstd")
        for cb in range(NCB):
            xin, xin_b, xin_ch = rin.next()
            self.load_xcols(cb, xin, xin_b, xin_ch)
            self.norm_block(cb, xin, xin_b, sq, sq_b, rstd, rstd_b, nw, nw_b, 5,
                            lambda c, cb=cb: (xn[:, c, cb * 512:(cb + 1) * 512], xn_b[cb]))
        P.barrier()
        A.reset(m1)
        if DEBUG_STOP == 1:
            A.reset(m0)
            return
        h = A.alloc([128, 11, S], BF16, "h")
        h_b = [[P.buf(f"h{mi}_{cb}") for cb in range(NCB)] for mi in range(11)]
        rw = Ring(self, 3, [128, 2, 8, 128], BF16, "wgu", eng="pool")
        rsg = Ring(self, 2, [128, 512], F32, "sg", chan=False)
        rwd = Ring(self, 2, [128, 11, 128], BF16, "wd", eng="pool")
        rxr = Ring(self, 3, [128, 512], F32, "xres")
        rxo = Ring(self, 3, [128, 512], F32, "xo")
        pgu = 0
        pdn = 0
        for grp in range(2):
            for mi in range(11):
                mchunk = grp * 11 + mi
                w, w_b, w_ch = rw.next()
                P.op("pool", lambda e, w=w, mchunk=mchunk: e.dma_start(
                    out=w[:].rearrange("p a k j -> p (a k j)"), in_=wgu_d[mchunk]),
                    writes=[w_b], dma=True, chan=w_ch)
                for cb in range(NCB):
                    pa, pb = (0, 1) if pgu % 2 == 0 else (2, 3)
                    pgu += 1
                    cols = slice(cb * 512, (cb + 1) * 512)
                    self.mm_group(self.ps[pa][:], [(w[:, 0, kc, :], xn[:, kc, cols]) for kc in range(8)],
                                  [w_b, xn_b[cb]], self.psb[pa])
                    self.mm_group(self.ps[pb][:], [(w[:, 1, kc, :], xn[:, kc, cols]) for kc in range(8)],
                                  [w_b, xn_b[cb]], self.psb[pb])
                    sg, sg_b, _ = rsg.next()
                    P.op("act", lambda e, sg=sg, pa=pa: e.activation(out=sg[:], in_=self.ps[pa][:], func=AF.Silu),
                         reads=[self.psb[pa]], writes=[sg_b])
                    P.op("dve", lambda e, sg=sg, pb=pb, mi=mi, cols=cols: e.tensor_tensor(
                        out=h[:, mi, cols], in0=sg[:], in1=self.ps[pb][:], op=ALU.mult),
                        reads=[sg_b, self.psb[pb]], writes=[h_b[mi][cb]])
            if DEBUG_STOP == 2:
                continue
            for o in range(8):
                wd, wd_b, wd_ch = rwd.next()
                P.op("pool", lambda e, wd=wd, grp=grp, o=o: e.dma_start(
                    out=wd[:].rearrange("p f j -> p (f j)"), in_=wd_d[grp, o]),
                    writes=[wd_b], dma=True, chan=wd_ch)
                for cb in range(NCB):
                    cols = slice(cb * 512, (cb + 1) * 512)
                    xr, xr_b, xr_ch = rxr.next()
                    P.op("sp", lambda e, xr=xr, o=o, cols=cols: e.dma_start(
                        out=xr[:], in_=self.xT[o * 128:(o + 1) * 128, cols]),
                        reads=[self.xTb[o][cb]], writes=[xr_b], dma=True, chan=xr_ch)
                    pi = 4 + (pdn % 2)
                    pdn += 1
                    self.mm_group(self.ps[pi][:], [(wd[:, fc, :], h[:, fc, cols]) for fc in range(11)],
                                  [wd_b] + [h_b[fc][cb] for fc in range(11)], self.psb[pi])
                    xo, xo_b, xo_ch = rxo.next()
                    P.op("dve", lambda e, xo=xo, xr=xr, pi=pi: e.scalar_tensor_tensor(
                        out=xo[:], in0=self.ps[pi][:], scalar=0.5, in1=xr[:], op0=ALU.mult, op1=ALU.add),
                        reads=[self.psb[pi], xr_b], writes=[xo_b])
                    if DEBUG_STOP == 3:
                        continue
                    P.op("sp", lambda e, xo=xo, o=o, cols=cols: e.dma_start(
                        out=self.xT[o * 128:(o + 1) * 128, cols], in_=xo[:]),
                        reads=[xo_b], writes=[self.xTb[o][cb]], dma=True, chan=xo_ch)
        P.barrier()
        A.reset(m0)


def lay_vec8(v):
    return np.ascontiguousarray(np.asarray(v, np.float32).reshape(8, 128).T)


def lay_wgu(w):
    w = np.asarray(w, np.float32)
    g = w[:, :DFF].reshape(8, 128, NFC, 128)
    u = w[:, DFF:].reshape(8, 128, NFC, 128)
    a = np.stack([g, u], axis=0)
    a = a.transpose(3, 2, 0, 1, 4)
    return np.ascontiguousarray(a).reshape(NFC, 128, 2 * 8 * 128)


def lay_wdown(w):
    w = np.asarray(w, np.float32)
    a = w.reshape(2, 11, 128, 8, 128)
    a = a.transpose(0, 3, 2, 1, 4)
    return np.ascontiguousarray(a).reshape(2, 8, 128, 11 * 128)


def build(S, plan):
    k = K(S)
    k.load_consts()
    k.stage_in()
    for item in plan:
        if item[0] == "ffn":
            k.ffn(item[1])
        elif item[0] == "mla":
            k.mla(item[1])
        elif item[0] == "gla":
            k.gla(item[1])
        elif item[0] == "ssd":
            k.ssd(item[1])
        else:
            raise ValueError(item)
    k.stage_out()
    k.stats = k.P.emit()
    return k


PLAN = [
    ("ffn", "l0_ffn1"), ("gla", "l0"), ("ffn", "l0_ffn2"),
    ("ffn", "l1_ffn1"), ("ssd", "l1"), ("ffn", "l1_ffn2"),
    ("ffn", "l2_ffn1"), ("mla", "l2"), ("ffn", "l2_ffn2"),
    ("ffn", "l3_ffn1"), ("gla", "l3"), ("ffn", "l3_ffn2"),
]


def kernel(**inputs):
    S = 4096
    k = build(S, PLAN)
    shared = {}
    in_maps = []
    x = np.asarray(inputs["x"], np.float32)
    pos = np.asarray(inputs["positions"], np.int32)
    for b in range(8):
        m = {}
        for name in k.inputs:
            if name == "x":
                m[name] = np.ascontiguousarray(x[b])
            elif name == "positions64":
                m[name] = np.ascontiguousarray(np.broadcast_to(pos[b][None, :], (64, S)))
            else:
                if name not in shared:
                    shared[name] = host_inputs_sub(k, inputs, name)
                m[name] = shared[name]
        in_maps.append(m)
    res = run_bass_kernel_spmd(k.nc, in_maps, core_ids=list(range(8)))
    return np.stack([np.asarray(r["out"], np.float32) for r in res.results], axis=0)


def host_inputs_sub(k, inputs, name):
    if name == "c_ident":
        return np.eye(128, dtype=np.float32)
    c = const_tables(name)
    if c is not None:
        return c
    c = gla_consts(name)
    if c is not None:
        return c
    c = ssd_consts(name)
    if c is not None:
        return c
    if "_ssd_" in name or name == "l1_mix_norm":
        return ssd_host(inputs, name[:2], name)
    if "_mla_" in name or name == "l2_mix_norm":
        return mla_host(inputs, name[:2], name, k.S)
    if "_gla_" in name or name in ("l0_mix_norm", "l3_mix_norm"):
        return gla_host(inputs, name[:2], name)
    if name.endswith("_w_gu"):
        return lay_wgu(inputs[name])
    if name.endswith("_w_down"):
        return lay_wdown(inputs[name])
    if name.endswith("ffn1_norm") or name.endswith("ffn2_norm") or name == "final_norm":
        return lay_vec8(inputs[name])
    raise KeyError(name)


MLA_SC = float(192 ** -0.5)
NEG = -1.0e30


def _mla(self, pre):
    P, A, S, NCB = self.P, self.A, self.S, self.NCB
    NB = S // 128
    nc = self.nc
    nw_d = self.dram_in(pre + "_mix_norm", [128, 8])
    win_d = self.dram_in(pre + "_mla_w_in", [128, 8 * 768])
    qnw_d = self.dram_in(pre + "_mla_q_norm", [128, 3])
    kvnw_d = self.dram_in(pre + "_mla_kv_norm", [128, 2])
    wuq_d = self.dram_in(pre + "_mla_w_uq", [128, 3 * 8 * 256])
    wk_d = self.dram_in(pre + "_mla_w_uk", [128, 2 * 1024])
    wv_d = self.dram_in(pre + "_mla_w_uv", [128, 2 * 1024])
    wo_d = self.dram_in(pre + "_mla_w_out", [128, 8 * 1024])
    pos_d = self.dram_in("positions64", [64, S], I32)
    rt_d = self.dram_in("c_rope", [64, 4])
    mask_d = self.dram_in("c_mask", [128, 4 * 512])
    Qn = nc.dram_tensor("mla_qn", [8, 128, S], BF16, kind="Internal").ap()
    Qr = nc.dram_tensor("mla_qr", [8, 64, S], BF16, kind="Internal").ap()
    Kn = nc.dram_tensor("mla_kn", [8, 128, S], BF16, kind="Internal").ap()
    Kr = nc.dram_tensor("mla_kr", [64, S], BF16, kind="Internal").ap()
    V = nc.dram_tensor("mla_v", [8, 128, NB, 128], BF16, kind="Internal").ap()
    m0 = A.mark()

    def ld(dst, src, eng="sp", name="w"):
        b = P.buf(name)
        P.op(eng, lambda e: e.dma_start(out=dst, in_=src), writes=[b], dma=True, chan=P.chan(name, eng))
        return b

    nw = A.alloc([128, 8], F32, "nw")
    nw_b = ld(nw[:], nw_d, name="nw")
    qnw = A.alloc([128, 3], F32, "qnw")
    qnw_b = ld(qnw[:], qnw_d, name="qnw")
    kvnw = A.alloc([128, 2], F32, "kvnw")
    kvnw_b = ld(kvnw[:], kvnw_d, name="kvnw")
    rt = A.alloc([64, 4], F32, "rt")
    rt_b = ld(rt[:], rt_d, name="rt")
    m1 = A.mark()
    win = A.alloc([128, 8, 768], BF16, "win")
    win_b = ld(win[:].rearrange("p k n -> p (k n)"), win_d, "pool", "win")
    wuq = A.alloc([128, 3, 8, 256], BF16, "wuq")
    wuq_b = ld(wuq[:].rearrange("p k h n -> p (k h n)"), wuq_d, "pool", "wuq")
    wk = A.alloc([128, 2, 1024], BF16, "wk")
    wk_b = ld(wk[:].rearrange("p k n -> p (k n)"), wk_d, "pool", "wk")
    wv = A.alloc([128, 2, 1024], BF16, "wv")
    wv_b = ld(wv[:].rearrange("p k n -> p (k n)"), wv_d, "pool", "wv")
    rin = Ring(self, 2, [128, 8, 512], F32, "xin")
    sq = A.alloc([128, 8, 512], BF16, "sq")
    sq_b = P.buf("sq")
    rstd = A.alloc([128, 512], F32, "rstd")
    rstd_b = P.buf("rstd")
    xn = A.alloc([128, 8, 512], BF16, "xn")
    xn_b = P.buf("xn")
    latf = A.alloc([128, 3, 512], F32, "latf")
    latf_b = [P.buf(f"latf{c}") for c in range(3)]
    latsq = A.alloc([128, 3, 512], BF16, "latsq")
    latsq_b = [P.buf(f"latsq{c}") for c in range(3)]
    lrstd = A.alloc([128, 512], F32, "lrstd")
    lrstd_b = P.buf("lrstd")
    cqn = A.alloc([128, 3, 512], BF16, "cqn")
    cqn_b = P.buf("cqn")
    ckvn = A.alloc([128, 2, 512], BF16, "ckvn")
    ckvn_b = P.buf("ckvn")
    posi = A.alloc([64, 512], I32, "posi")
    posi_b = P.buf("posi")
    posi_ch = P.chan("posi")
    ang = A.alloc([64, 512], F32, "ang")
    ang_b = P.buf("ang")
    kf = A.alloc([64, 512], F32, "kf")
    ki = A.alloc([64, 512], I32, "ki")
    kf_b = P.buf("kf")
    arg = A.alloc([64, 512], F32, "arg")
    arg_b = P.buf("arg")
    cos2 = A.alloc([64, 512], F32, "cos2")
    cos2_b = P.buf("cos2")
    sin2 = A.alloc([64, 512], F32, "sin2")
    sin2_b = P.buf("sin2")
    t1 = A.alloc([64, 512], F32, "t1")
    t1_b = P.buf("t1")
    t2 = A.alloc([64, 512], F32, "t2")
    t2_b = P.buf("t2")
    rkr = Ring(self, 2, [64, 512], BF16, "krst")
    rqn = Ring(self, 2, [128, 8, 512], BF16, "qnst")
    rqr = Ring(self, 2, [64, 8, 512], BF16, "qrst")
    rkn = Ring(self, 2, [128, 8, 512], BF16, "knst")
    rvs = Ring(self, 2, [128, 1024], BF16, "vst")
    TWO_PI = float(2 * np.pi)
    bank = [0]

    def nb():
        bank[0] = (bank[0] + 1) % 4
        return bank[0]

    def range_reduce(shift):
        P.op("dve", lambda e: e.tensor_scalar(arg[:], ang[:], float(shift), None, ALU.add),
             reads=[ang_b], writes=[arg_b])
        P.op("dve", lambda e: e.tensor_scalar(kf[:], arg[:], float(1.0 / TWO_PI), None, ALU.mult),
             reads=[arg_b], writes=[kf_b])
        P.op("dve", lambda e: e.tensor_copy(out=ki[:], in_=kf[:]), reads=[kf_b], writes=[kf_b])
        P.op("dve", lambda e: e.tensor_copy(out=kf[:], in_=ki[:]), reads=[kf_b], writes=[kf_b])
        P.op("dve", lambda e: e.scalar_tensor_tensor(out=arg[:], in0=kf[:], scalar=-TWO_PI, in1=arg[:],
                                                     op0=ALU.mult, op1=ALU.add),
             reads=[arg_b, kf_b], writes=[arg_b])
        P.op("dve", lambda e: e.tensor_scalar(kf[:], arg[:], float(np.pi), -TWO_PI, ALU.is_gt, ALU.mult),
             reads=[arg_b], writes=[kf_b])
        P.op("dve", lambda e: e.tensor_tensor(out=arg[:], in0=arg[:], in1=kf[:], op=ALU.add),
             reads=[arg_b, kf_b], writes=[arg_b])
        P.op("dve", lambda e: e.tensor_scalar(kf[:], arg[:], float(-np.pi), TWO_PI, ALU.is_lt, ALU.mult),
             reads=[arg_b], writes=[kf_b])
        P.op("dve", lambda e: e.tensor_tensor(out=arg[:], in0=arg[:], in1=kf[:], op=ALU.add),
             reads=[arg_b, kf_b], writes=[arg_b])

    def lat_norm(nch, col0, nwt, nwt_b, dst, dst_b, dim):
        for c in range(nch):
            bk = nb()
            self.mm_group(self.ps[bk][:], [(win[:, kc, col0 + c * 128:col0 + (c + 1) * 128], xn[:, kc, :])
                                           for kc in range(8)], [win_b, xn_b], self.psb[bk])
            P.op("act", lambda e, c=c, bk=bk: e.activation(out=latf[:, c, :], in_=self.ps[bk][:], func=AF.Copy),
                 reads=[self.psb[bk]], writes=[latf_b[c]])
            P.op("act", lambda e, c=c, bk=bk: e.activation(out=latsq[:, c, :], in_=self.ps[bk][:], func=AF.Square),
                 reads=[self.psb[bk]], writes=[latsq_b[c]])
        self.mm_group(self.ps[4][:], [(self.c_onesb[:], latsq[:, c, :]) for c in range(nch)],
                      [latsq_b[c] for c in range(nch)] + [self.cb_const], self.psb[4])
        P.op("act", lambda e: e.activation(out=lrstd[:], in_=self.ps[4][:], func=AF.Sqrt, bias=self.c_eps[:],
                                           scale=1.0 / dim), reads=[self.psb[4], self.cb_const], writes=[lrstd_b])
        P.op("dve", lambda e: e.reciprocal(out=lrstd[:], in_=lrstd[:]), reads=[lrstd_b], writes=[lrstd_b])
        for c in range(nch):
            P.op("dve", lambda e, c=c: e.scalar_tensor_tensor(out=dst[:, c, :], in0=latf[:, c, :],
                                                             scalar=nwt[:, c:c + 1], in1=lrstd[:],
                                                             op0=ALU.mult, op1=ALU.mult),
                 reads=[latf_b[c], lrstd_b, nwt_b], writes=[dst_b])

    def rope_combine(bk_a, bk_b, scale, out_ap, out_b):
        P.op("dve", lambda e: e.scalar_tensor_tensor(out=t1[:], in0=self.ps[bk_a][0:64, :], scalar=float(scale),
                                                     in1=cos2[:], op0=ALU.mult, op1=ALU.mult),
             reads=[self.psb[bk_a], cos2_b], writes=[t1_b])
        P.op("dve", lambda e: e.scalar_tensor_tensor(out=t2[:], in0=self.ps[bk_b][0:64, :], scalar=float(scale),
                                                     in1=sin2[:], op0=ALU.mult, op1=ALU.mult),
             reads=[self.psb[bk_b], sin2_b], writes=[t2_b])
        P.op("dve", lambda e: e.tensor_tensor(out=out_ap, in0=t1[:], in1=t2[:], op=ALU.add),
             reads=[t1_b, t2_b], writes=[out_b])

    for cb in range(NCB):
        cols = slice(cb * 512, (cb + 1) * 512)
        xin, xin_b, xin_ch = rin.next()
        self.load_xcols(cb, xin, xin_b, xin_ch)
        self.norm_block(cb, xin, xin_b, sq, sq_b, rstd, rstd_b, nw, nw_b, 5, lambda c: (xn[:, c, :], xn_b))
        lat_norm(3, 0, qnw, qnw_b, cqn, cqn_b, 384)
        lat_norm(2, 384, kvnw, kvnw_b, ckvn, ckvn_b, 256)
        P.op("sp", lambda e, cols=cols: e.dma_start(out=posi[:], in_=pos_d[:, cols]), writes=[posi_b], dma=True,
             chan=posi_ch)
        P.op("dve", lambda e: e.tensor_copy(out=ang[:], in_=posi[:]), reads=[posi_b], writes=[ang_b])
        P.op("dve", lambda e: e.tensor_scalar(ang[:], ang[:], rt[:, 0:1], None, ALU.mult),
             reads=[ang_b, rt_b], writes=[ang_b])
        range_reduce(np.pi / 2)
        P.op("act", lambda e: e.activation(out=cos2[:], in_=arg[:], func=AF.Sin), reads=[arg_b], writes=[cos2_b])
        range_reduce(0.0)
        P.op("act", lambda e: e.activation(out=sin2[:], in_=arg[:], func=AF.Sin, scale=rt[:, 1:2]),
             reads=[arg_b, rt_b], writes=[sin2_b])
        ba, bb = nb(), nb()
        self.mm_group(self.ps[ba][0:64, :], [(win[:, kc, 640:704], xn[:, kc, :]) for kc in range(8)],
                      [win_b, xn_b], self.psb[ba])
        self.mm_group(self.ps[bb][0:64, :], [(win[:, kc, 704:768], xn[:, kc, :]) for kc in range(8)],
                      [win_b, xn_b], self.psb[bb])
        krs, krs_b, krs_ch = rkr.next()
        rope_combine(ba, bb, 1.0, krs[:], krs_b)
        P.op("sp", lambda e, krs=krs, cols=cols: e.dma_start(out=Kr[:, cols], in_=krs[:]), reads=[krs_b],
             dma=True, chan=krs_ch)
        qns, qns_b, qns_ch = rqn.next()
        qrs, qrs_b, qrs_ch = rqr.next()
        kns, kns_b, kns_ch = rkn.next()
        for h in range(8):
            bk = nb()
            self.mm_group(self.ps[bk][:], [(wuq[:, kc, h, 0:128], cqn[:, kc, :]) for kc in range(3)],
                          [wuq_b, cqn_b], self.psb[bk])
            P.op("act", lambda e, h=h, bk=bk, qns=qns: e.activation(out=qns[:, h, :], in_=self.ps[bk][:],
                                                                  func=AF.Copy, scale=MLA_SC),
                 reads=[self.psb[bk]], writes=[qns_b])
            ba, bb = nb(), nb()
            self.mm_group(self.ps[ba][0:64, :], [(wuq[:, kc, h, 128:192], cqn[:, kc, :]) for kc in range(3)],
                          [wuq_b, cqn_b], self.psb[ba])
            self.mm_group(self.ps[bb][0:64, :], [(wuq[:, kc, h, 192:256], cqn[:, kc, :]) for kc in range(3)],
                          [wuq_b, cqn_b], self.psb[bb])
            rope_combine(ba, bb, MLA_SC, qrs[:, h, :], qrs_b)
            bk = nb()
            self.mm_group(self.ps[bk][:], [(wk[:, kc, h * 128:(h + 1) * 128], ckvn[:, kc, :]) for kc in range(2)],
                          [wk_b, ckvn_b], self.psb[bk])
            P.op("act", lambda e, h=h, bk=bk, kns=kns: e.activation(out=kns[:, h, :], in_=self.ps[bk][:],
                                                                  func=AF.Copy),
                 reads=[self.psb[bk]], writes=[kns_b])
        P.op("sp", lambda e, qns=qns, cols=cols: e.dma_start(out=Qn.rearrange("h p s -> p h s")[:, :, cols],
                                                            in_=qns[:]), reads=[qns_b], dma=True, chan=qns_ch)
        P.op("sp", lambda e, qrs=qrs, cols=cols: e.dma_start(out=Qr.rearrange("h p s -> p h s")[:, :, cols],
                                                            in_=qrs[:]), reads=[qrs_b], dma=True, chan=qrs_ch)
        P.op("sp", lambda e, kns=kns, cols=cols: e.dma_start(out=Kn.rearrange("h p s -> p h s")[:, :, cols],
                                                            in_=kns[:]), reads=[kns_b], dma=True, chan=kns_ch)
        for tb in range(4):
            vs, vs_b, vs_ch = rvs.next()
            for half in range(2):
                bk = nb()
                self.mm_group(self.ps[bk][:], [(ckvn[:, kc, tb * 128:(tb + 1) * 128],
                                                wv[:, kc, half * 512:(half + 1) * 512]) for kc in range(2)],
                              [wv_b, ckvn_b], self.psb[bk])
                if half == 0:
                    P.op("act", lambda e, vs=vs, bk=bk: e.activation(out=vs[:, 0:512], in_=self.ps[bk][:],
                                                                   func=AF.Copy),
                         reads=[self.psb[bk]], writes=[vs_b])
                else:
                    P.op("dve", lambda e, vs=vs, bk=bk: e.tensor_copy(out=vs[:, 512:1024], in_=self.ps[bk][:]),
                         reads=[self.psb[bk]], writes=[vs_b])
            blk = cb * 4 + tb
            P.op("sp", lambda e, vs=vs, blk=blk: e.dma_start(
                out=V.rearrange("h p b v -> p h b v")[:, :, blk, :], in_=vs[:].rearrange("p (h v) -> p h v", h=8)),
                reads=[vs_b], dma=True, chan=vs_ch)
    P.barrier()
    A.reset(m1)

    wo = A.alloc([128, 8, 1024], BF16, "wo")
    wo_b = ld(wo[:].rearrange("p h n -> p (h n)"), wo_d, "pool", "wo")
    mask = A.alloc([128, 4, 512], F32, "mask")
    mask_b = ld(mask[:].rearrange("p t n -> p (t n)"), mask_d, "sp", "mask")
    kr = A.alloc([64, S], BF16, "kr")
    kr_b = ld(kr[:], Kr, "sp", "kr")
    rK = Ring(self, 2, [128, S], BF16, "kh")
    rV = Ring(self, 2, [128, NB, 128], BF16, "vh")
    rQn = Ring(self, 2, [128, 8, 512], BF16, "qn")
    rQr = Ring(self, 2, [64, 8, 512], BF16, "qr")
    Pm = A.alloc([128, 4, S], BF16, "Pm")
    Pm_b = [P.buf(f"Pm{t}") for t in range(4)]
    rPT = Ring(self, 3, [128, 1024], BF16, "PT", chan=False)
    oT = A.alloc([128, 8, 512], BF16, "oT")
    oT_b = [P.buf(f"oT{h}") for h in range(8)]
    mx = A.alloc([128, 4, 8], F32, "mx")
    mx_b = [[P.buf(f"mx{t}{c}") for c in range(8)] for t in range(4)]
    negm = A.alloc([128, 4], F32, "negm")
    negm_b = P.buf("negm")
    rs = A.alloc([128, 4, 8], F32, "rs")
    rs_b = [[P.buf(f"rs{t}{c}") for c in range(8)] for t in range(4)]
    rsum = A.alloc([128, 4], F32, "rsum")
    rsum_b = P.buf("rsum")
    diag = A.alloc([128, 4, 128], F32, "diag")
    diag_b = P.buf("diag")
    onesf = A.alloc([128, 128], F32, "onesf")
    onesf_b = P.buf("onesf")
    P.op("dve", lambda e: e.memset(onesf[:], 1.0), writes=[onesf_b])
    rbs = A.alloc([128, 512], F32, "rbs")
    rbs_b = P.buf("rbs")
    rdt = Ring(self, 2, [128, 512], F32, "dtmp", chan=False)
    rxr = Ring(self, 2, [128, 512], F32, "xres")
    rxo = Ring(self, 2, [128, 512], F32, "xo")
    cp_i = [0]

    for qb in range(NCB):
        cols = slice(qb * 512, (qb + 1) * 512)
        nk = (qb + 1) * 512
        nch = qb + 1
        nkb = nk // 128
        qn, qn_b, qn_ch = rQn.next()
        qr, qr_b, qr_ch = rQr.next()
        P.op("sp", lambda e, qn=qn, cols=cols: e.dma_start(out=qn[:], in_=Qn.rearrange("h p s -> p h s")[:, :, cols]),
             writes=[qn_b], dma=True, chan=qn_ch)
        P.op("sp", lambda e, qr=qr, cols=cols: e.dma_start(out=qr[:], in_=Qr.rearrange("h p s -> p h s")[:, :, cols]),
             writes=[qr_b], dma=True, chan=qr_ch)
        for h in range(8):
            kh, kh_b, kh_ch = rK.next()
            vh, vh_b, vh_ch = rV.next()
            P.op("sp", lambda e, kh=kh, h=h, nk=nk: e.dma_start(out=kh[:, 0:nk], in_=Kn[h][:, 0:nk]),
                 writes=[kh_b], dma=True, chan=kh_ch)
            P.op("sp", lambda e, vh=vh, h=h, nkb=nkb: e.dma_start(out=vh[:, 0:nkb, :], in_=V[h][:, 0:nkb, :]),
                 writes=[vh_b], dma=True, chan=vh_ch)

            def qk(t, kc, bk, qn=qn, qr=qr, kh=kh, h=h, qn_b=qn_b, qr_b=qr_b, kh_b=kh_b):
                tc_ = slice(t * 128, (t + 1) * 128)
                kcs = slice(kc * 512, (kc + 1) * 512)
                self.mm_group(self.ps[bk][:], [(qn[:, h, tc_], kh[:, kcs]), (qr[:, h, tc_], kr[:, kcs])],
                              [qn_b, qr_b, kh_b, kr_b], self.psb[bk])
            for t in range(4):
                for kc in range(nch):
                    bk = nb()
                    qk(t, kc, bk)
                    P.op("dve", lambda e, t=t, kc=kc, bk=bk: e.tensor_reduce(out=mx[:, t, kc:kc + 1], in_=self.ps[bk][:],
                                                                           axis=AX.X, op=ALU.max),
                         reads=[self.psb[bk]], writes=[mx_b[t][kc]])
            P.op("dve", lambda e, nch=nch: e.tensor_reduce(out=negm[:], in_=mx[:, :, 0:nch], axis=AX.X, op=ALU.max),
                 reads=[mx_b[t][c] for t in range(4) for c in range(nch)], writes=[negm_b])
            P.op("dve", lambda e: e.tensor_scalar(negm[:], negm[:], -1.0, None, ALU.mult),
                 reads=[negm_b], writes=[negm_b])
            P.op("dve", lambda e: e.memset(rs[:], 0.0), writes=[rs_b[t][c] for t in range(4) for c in range(8)])
            for t in range(4):
                for kc in range(nch):
                    bk = nb()
                    qk(t, kc, bk)
                    kcs = slice(kc * 512, (kc + 1) * 512)
                    if kc == nch - 1:
                        dt_, dt_b, _ = rdt.next()
                        P.op("dve", lambda e, dt_=dt_, bk=bk, t=t: e.tensor_tensor(out=dt_[:], in0=self.ps[bk][:],
                                                                                in1=mask[:, t, :], op=ALU.add),
                             reads=[self.psb[bk], mask_b], writes=[dt_b])
                        src, src_b = dt_, dt_b
                        P.op("act", lambda e, t=t, kc=kc, kcs=kcs, src=src: e.activation(
                            out=Pm[:, t, kcs], in_=src[:], func=AF.Exp, bias=negm[:, t:t + 1], scale=1.0,
                            accum_out=rs[:, t, kc:kc + 1]),
                            reads=[src_b, negm_b], writes=[Pm_b[t], rs_b[t][kc]])
                    else:
                        P.op("act", lambda e, t=t, kc=kc, kcs=kcs, bk=bk: e.activation(
                            out=Pm[:, t, kcs], in_=self.ps[bk][:], func=AF.Exp, bias=negm[:, t:t + 1], scale=1.0,
                            accum_out=rs[:, t, kc:kc + 1]),
                            reads=[self.psb[bk], negm_b], writes=[Pm_b[t], rs_b[t][kc]])
            P.op("dve", lambda e, nch=nch: e.tensor_reduce(out=rsum[:], in_=rs[:, :, 0:nch], axis=AX.X, op=ALU.add),
                 reads=[rs_b[t][c] for t in range(4) for c in range(nch)], writes=[rsum_b])
            P.op("dve", lambda e: e.reciprocal(out=rsum[:], in_=rsum[:]), reads=[rsum_b], writes=[rsum_b])
            npair = nkb // 2
            for jp in range(npair):
                tb_ = jp % 2
                pst, pst_b = self.pst[tb_], self.pstb[tb_]

                def tr(e, jp=jp, pst=pst):
                    ins = None
                    for jl in range(2):
                        for t in range(4):
                            j = jp * 2 + jl
                            ins = e.transpose(pst[:, (jl * 4 + t) * 128:(jl * 4 + t + 1) * 128],
                                              Pm[:, t, j * 128:(j + 1) * 128], self.c_identb[:])
                    return ins
                P.op("pe", tr, reads=Pm_b + [self.cb_const], writes=[pst_b])
                pt, pt_b, _ = rPT.next()
                cp_i[0] += 1
                if cp_i[0] % 2 == 0:
                    P.op("act", lambda e, pt=pt, pst=pst: e.activation(out=pt[:], in_=pst[:], func=AF.Copy),
                         reads=[pst_b], writes=[pt_b])
                else:
                    P.op("dve", lambda e, pt=pt, pst=pst: e.tensor_copy(out=pt[:], in_=pst[:]),
                         reads=[pst_b], writes=[pt_b])

                def pv(e, jp=jp, pt=pt, vh=vh, npair=npair):
                    ins = None
                    for jl in range(2):
                        j = jp * 2 + jl
                        ins = e.matmul(self.ps[4][:], vh[:, j, :], pt[:, jl * 512:(jl + 1) * 512],
                                       start=(jp == 0 and jl == 0), stop=(jp == npair - 1 and jl == 1))
                    return ins
                P.op("pe", pv, reads=[pt_b, vh_b], writes=[self.psb[4]])
            for t in range(4):
                P.op("dve", lambda e, t=t: e.tensor_scalar(diag[:, t, :], self.c_identf[:], rsum[:, t:t + 1], None,
                                                           ALU.mult),
                     reads=[rsum_b, self.cb_const], writes=[diag_b])
            self.mm_group(self.ps[5][:], [(onesf[:], diag[:].rearrange("p t n -> p (t n)"))], [onesf_b, diag_b],
                          self.psb[5])
            P.op("act", lambda e: e.activation(out=rbs[:], in_=self.ps[5][:], func=AF.Copy),
                 reads=[self.psb[5]], writes=[rbs_b])
            P.op("dve", lambda e, h=h: e.tensor_tensor(out=oT[:, h, :], in0=self.ps[4][:], in1=rbs[:], op=ALU.mult),
                 reads=[self.psb[4], rbs_b], writes=[oT_b[h]])
        for o in range(8):
            bk = nb()
            self.mm_group(self.ps[bk][:], [(wo[:, h, o * 128:(o + 1) * 128], oT[:, h, :]) for h in range(8)],
                          [wo_b] + oT_b, self.psb[bk])
            xr, xr_b, xr_ch = rxr.next()
            P.op("sp", lambda e, xr=xr, o=o, cols=cols: e.dma_start(out=xr[:], in_=self.xT[o * 128:(o + 1) * 128, cols]),
                 reads=[self.xTb[o][qb]], writes=[xr_b], dma=True, chan=xr_ch)
            xo, xo_b, xo_ch = rxo.next()
            P.op("dve", lambda e, xo=xo, xr=xr, bk=bk: e.tensor_tensor(out=xo[:], in0=self.ps[bk][:], in1=xr[:],
                                                                     op=ALU.add),
                 reads=[self.psb[bk], xr_b], writes=[xo_b])
            P.op("sp", lambda e, xo=xo, o=o, cols=cols: e.dma_start(out=self.xT[o * 128:(o + 1) * 128, cols], in_=xo[:]),
                 reads=[xo_b], writes=[self.xTb[o][qb]], dma=True, chan=xo_ch)
    P.barrier()
    A.reset(m0)


K.mla = _mla


def lay_cols(w, p=128):
    w = np.asarray(w, np.float32)
    kc = w.shape[0] // p
    return np.ascontiguousarray(w.reshape(kc, p, w.shape[1]).transpose(1, 0, 2)).reshape(p, kc * w.shape[1])


def lay_vecp(v, p=128):
    v = np.asarray(v, np.float32)
    return np.ascontiguousarray(v.reshape(-1, p).T)


def mla_host(inputs, pre, name, S):
    if name == pre + "_mla_w_in":
        w = np.asarray(inputs[name], np.float32)
        kr = w[:, 640:704]
        w2 = np.concatenate([w, kr[:, 32:64], kr[:, 0:32]], axis=1)
        return lay_cols(w2)
    if name == pre + "_mla_w_uq":
        w = np.asarray(inputs[name], np.float32).reshape(384, 8, 192)
        w2 = np.concatenate([w, w[:, :, 160:192], w[:, :, 128:160]], axis=2)
        return lay_cols(w2.reshape(384, 8 * 256))
    if name == pre + "_mla_w_uk":
        w = np.asarray(inputs[pre + "_mla_w_ukv"], np.float32).reshape(256, 8, 256)
        return lay_cols(np.ascontiguousarray(w[:, :, 0:128]).reshape(256, 1024))
    if name == pre + "_mla_w_uv":
        w = np.asarray(inputs[pre + "_mla_w_ukv"], np.float32).reshape(256, 8, 256)
        return lay_cols(np.ascontiguousarray(w[:, :, 128:256]).reshape(256, 1024))
    if name == pre + "_mla_w_out":
        return lay_cols(inputs[name])
    if name in (pre + "_mla_q_norm", pre + "_mla_kv_norm", pre + "_mix_norm"):
        return lay_vecp(inputs[name])
    return None


def const_tables(name):
    if name == "c_rope":
        inv = (1.0 / (10000.0 ** (np.arange(0, 64, 2, dtype=np.float32) / 64))).astype(np.float32)
        inv2 = np.concatenate([inv, inv])
        sgn = np.concatenate([-np.ones(32), np.ones(32)])
        return np.stack([inv2, sgn, -np.pi * sgn, -np.pi * np.ones(64)], 1).astype(np.float32)
    if name == "c_mask":
        q = np.arange(128)[:, None, None]
        t = np.arange(4)[None, :, None]
        c = np.arange(512)[None, None, :]
        m = np.where(c <= t * 128 + q, 0.0, NEG).astype(np.float32)
        return np.ascontiguousarray(m).reshape(128, 4 * 512)
    return None


def _gla(self, pre):
    P, A, S, NCB = self.P, self.A, self.S, self.NCB
    nw_d = self.dram_in(pre + "_mix_norm", [128, 8])
    win_d = self.dram_in(pre + "_gla_w_in", [128, 8 * 3088])
    wgb_d = self.dram_in(pre + "_gla_w_gate_b", [16, 512])
    bg_d = self.dram_in(pre + "_gla_b_gate", [1, 512])
    gnw_d = self.dram_in(pre + "_gla_norm", [128, 1024])
    wo_d = self.dram_in(pre + "_gla_w_out", [128, 8 * 1024])
    cum_d = self.dram_in("c_gla_cum", [128, 3 * 128])
    tri4_d = self.dram_in("c_tri4", [128, 512])
    m0 = A.mark()

    def ld(dst, src, eng="sp", name="w"):
        b = P.buf(name)
        P.op(eng, lambda e: e.dma_start(out=dst, in_=src), writes=[b], dma=True, chan=P.chan(name, eng))
        return b

    nw = A.alloc([128, 8], F32, "nw")
    nw_b = ld(nw[:], nw_d, name="nw")
    win = A.alloc([128, 8, 3088], BF16, "gwin")
    win_b = ld(win[:].rearrange("p k n -> p (k n)"), win_d, "pool", "gwin")
    wo = A.alloc([128, 8, 1024], BF16, "gwo")
    wo_b = ld(wo[:].rearrange("p k n -> p (k n)"), wo_d, "pool", "gwo")
    wgb = A.alloc([16, 512], BF16, "wgb")
    wgb_b = ld(wgb[:], wgb_d, "pool", "wgb")
    bg = A.alloc([1, 512], BF16, "bg")
    bg_b = ld(bg[:], bg_d, "pool", "bg")
    gnw = A.alloc([128, 1024], F32, "gnw")
    gnw_b = ld(gnw[:], gnw_d, "sp", "gnw")
    cum = A.alloc([128, 3, 128], F32, "cum")
    cum_b = ld(cum[:].rearrange("p a n -> p (a n)"), cum_d, "sp", "cum")
    tri4 = A.alloc([128, 4, 128], F32, "tri4")
    tri4_b = ld(tri4[:].rearrange("p a n -> p (a n)"), tri4_d, "sp", "tri4")
    ones1 = A.alloc([1, 128], BF16, "ones1")
    ones1_b = P.buf("ones1")
    P.op("dve", lambda e: e.memset(ones1[:], 1.0), writes=[ones1_b])
    c1 = A.alloc([128, 1], F32, "c1")
    c1_b = P.buf("c1")
    P.op("dve", lambda e: e.memset(c1[:], 1.0), writes=[c1_b])

    rin = Ring(self, 2, [128, 8, 512], F32, "xin")
    sq = A.alloc([128, 8, 512], BF16, "sq")
    sq_b = P.buf("sq")
    rstd = A.alloc([128, 512], F32, "rstd")
    rstd_b = P.buf("rstd")
    xn = A.alloc([128, 8, 512], BF16, "xn")
    xn_b = P.buf("xn")
    qTf = A.alloc([128, 4, 512], F32, "qTf")
    qTf_b = P.buf("qTf")
    kTf = A.alloc([128, 4, 512], F32, "kTf")
    kTf_b = P.buf("kTf")
    alr = A.alloc([16, 512], BF16, "alr")
    alr_b = P.buf("alr")
    ktm = A.alloc([128, 512], F32, "ktm")
    ktm_b = P.buf("ktm")
    vsb = A.alloc([128, 1024], BF16, "vsb")
    vsb_b = P.buf("vsb")
    sgw = A.alloc([128, 1024], F32, "sgw")
    sgw_b = P.buf("sgw")
    sp_ = A.alloc([128, 512], F32, "sp")
    sp_b = P.buf("sp")
    dend = A.alloc([128, 512], F32, "dend")
    dend_b = P.buf("dend")
    kend = A.alloc([128, 512], BF16, "kend")
    kend_b = P.buf("kend")
    eb = A.alloc([128, 4, 128], F32, "eb")
    eb_b = P.buf("eb")
    enb = A.alloc([128, 4, 128], F32, "enb")
    enb_b = P.buf("enb")
    qdec = A.alloc([128, 4, 128], BF16, "qdec")
    qdec_b = P.buf("qdec")
    kinv = A.alloc([128, 4, 128], BF16, "kinv")
    kinv_b = P.buf("kinv")
    attnT = A.alloc([128, 4, 128], BF16, "attnT")
    attnT_b = P.buf("attnT")
    stf = A.alloc([128, 4, 256], F32, "stf")
    stf_b = [P.buf(f"stf{h}") for h in range(4)]
    stb = A.alloc([128, 4, 256], BF16, "stb")
    stb_b = [P.buf(f"stb{h}") for h in range(4)]
    junk = A.alloc([128, 256], F32, "junk")
    junk_b = P.buf("junk")
    ssq = A.alloc([128, 4], F32, "ssq")
    ssq_b = [P.buf(f"ssq{h}") for h in range(4)]
    orstd = A.alloc([128, 4], F32, "orstd")
    orstd_b = P.buf("orstd")
    og = A.alloc([128, 1024], BF16, "og")
    og_b = P.buf("og")
    ogT = A.alloc([128, 8, 512], BF16, "ogT")
    ogT_b = [P.buf(f"ogT{t}") for t in range(4)]
    rxr = Ring(self, 2, [128, 512], F32, "xres")
    rxo = Ring(self, 2, [128, 512], F32, "xo")
    for h in range(4):
        P.op("dve", lambda e, h=h: e.memset(stf[:, h, :], 0.0), writes=[stf_b[h]])
        P.op("pool", lambda e, h=h: e.memset(stb[:, h, :], 0.0), writes=[stb_b[h]])
    bank = [0]

    def nb():
        bank[0] = (bank[0] + 1) % 5
        return bank[0]
    SCQ = float(128 ** -0.5)

    for cb in range(NCB):
        cols = slice(cb * 512, (cb + 1) * 512)
        xin, xin_b, xin_ch = rin.next()
        self.load_xcols(cb, xin, xin_b, xin_ch)
        self.norm_block(cb, xin, xin_b, sq, sq_b, rstd, rstd_b, nw, nw_b, 5, lambda c: (xn[:, c, :], xn_b))
        for h in range(4):
            bk = nb()
            self.mm_group(self.ps[bk][:], [(win[:, kc, h * 128:(h + 1) * 128], xn[:, kc, :]) for kc in range(8)],
                          [win_b, xn_b], self.psb[bk])
            P.op("act", lambda e, h=h, bk=bk: e.activation(out=qTf[:, h, :], in_=self.ps[bk][:], func=AF.Copy,
                                                           scale=SCQ), reads=[self.psb[bk]], writes=[qTf_b])
            bk = nb()
            self.mm_group(self.ps[bk][:], [(win[:, kc, 512 + h * 128:512 + (h + 1) * 128], xn[:, kc, :])
                                           for kc in range(8)], [win_b, xn_b], self.psb[bk])
            P.op("dve", lambda e, h=h, bk=bk: e.tensor_copy(out=kTf[:, h, :], in_=self.ps[bk][:]),
                 reads=[self.psb[bk]], writes=[kTf_b])
        bk = nb()
        self.mm_group(self.ps[bk][0:16, :], [(win[:, kc, 3072:3088], xn[:, kc, :]) for kc in range(8)],
                      [win_b, xn_b], self.psb[bk])
        P.op("act", lambda e, bk=bk: e.activation(out=alr[:], in_=self.ps[bk][0:16, :], func=AF.Copy),
             reads=[self.psb[bk]], writes=[alr_b])
        for tb in range(4):
            tcs = slice(tb * 128, (tb + 1) * 128)
            bk = nb()
            self.mm_group(self.ps[bk][:], [(xn[:, kc, tcs], win[:, kc, 512:1024]) for kc in range(8)],
                          [win_b, xn_b], self.psb[bk])
            P.op("act", lambda e, bk=bk: e.activation(out=ktm[:], in_=self.ps[bk][:], func=AF.Copy),
                 reads=[self.psb[bk]], writes=[ktm_b])
            for half in range(2):
                bk = nb()
                self.mm_group(self.ps[bk][:], [(xn[:, kc, tcs], win[:, kc, 1024 + half * 512:1024 + (half + 1) * 512])
                                               for kc in range(8)], [win_b, xn_b], self.psb[bk])
                P.op("dve", lambda e, bk=bk, half=half: e.tensor_copy(out=vsb[:, half * 512:(half + 1) * 512],
                                                                      in_=self.ps[bk][:]),
                     reads=[self.psb[bk]], writes=[vsb_b])
            for half in range(2):
                bk = nb()
                self.mm_group(self.ps[bk][:], [(xn[:, kc, tcs], win[:, kc, 2048 + half * 512:2048 + (half + 1) * 512])
                                               for kc in range(8)], [win_b, xn_b], self.psb[bk])
                hs = slice(half * 512, (half + 1) * 512)
                P.op("act", lambda e, bk=bk, hs=hs: e.activation(out=sgw[:, hs], in_=self.ps[bk][:], func=AF.Silu),
                     reads=[self.psb[bk]], writes=[sgw_b])
                P.op("dve", lambda e, hs=hs: e.tensor_tensor(out=sgw[:, hs], in0=sgw[:, hs], in1=gnw[:, hs],
                                                             op=ALU.mult), reads=[sgw_b, gnw_b], writes=[sgw_b])
            bk = nb()
            self.mm_group(self.ps[bk][:], [(alr[:, tcs], wgb[:]), (ones1[:], bg[:])],
                          [alr_b, wgb_b, ones1_b, bg_b], self.psb[bk])
            P.op("act", lambda e, bk=bk: e.activation(out=sp_[:], in_=self.ps[bk][:], func=AF.Exp, scale=-1.0),
                 reads=[self.psb[bk]], writes=[sp_b])
            P.op("act", lambda e: e.activation(out=sp_[:], in_=sp_[:], func=AF.Ln, bias=c1[:], scale=1.0),
                 reads=[sp_b, c1_b], writes=[sp_b])
            bk = nb()
            self.mm_group(self.ps[bk][:], [(cum[:, 0, :], sp_[:])], [cum_b, sp_b], self.psb[bk])
            P.op("act", lambda e, bk=bk: e.activation(out=dend[:], in_=self.ps[bk][:], func=AF.Exp),
                 reads=[self.psb[bk]], writes=[dend_b])
            P.op("dve", lambda e: e.tensor_tensor(out=kend[:], in0=ktm[:], in1=dend[:], op=ALU.mult),
                 reads=[ktm_b, dend_b], writes=[kend_b])
            bk = nb()

            def cumT(e, bk=bk):
                ins = None
                for h in range(4):
                    ins = e.matmul(self.ps[bk][:, h * 128:(h + 1) * 128], sp_[:, h * 128:(h + 1) * 128],
                                   cum[:, 1, :], start=True, stop=True)
                return ins
            P.op("pe", cumT, reads=[sp_b, cum_b], writes=[self.psb[bk]])
            P.op("act", lambda e, bk=bk: e.activation(out=eb[:].rearrange("p h n -> p (h n)"), in_=self.ps[bk][:],
                                                      func=AF.Exp), reads=[self.psb[bk]], writes=[eb_b])
            P.op("act", lambda e, bk=bk: e.activation(out=enb[:].rearrange("p h n -> p (h n)"), in_=self.ps[bk][:],
                                                      func=AF.Exp, scale=-1.0), reads=[self.psb[bk]], writes=[enb_b])
            P.op("dve", lambda e, tcs=tcs: e.tensor_tensor(out=qdec[:], in0=qTf[:, :, tcs], in1=eb[:], op=ALU.mult),
                 reads=[qTf_b, eb_b], writes=[qdec_b])
            P.op("dve", lambda e, tcs=tcs: e.tensor_tensor(out=kinv[:], in0=kTf[:, :, tcs], in1=enb[:], op=ALU.mult),
                 reads=[kTf_b, enb_b], writes=[kinv_b])
            bk = nb()

            def att(e, bk=bk):
                ins = None
                for h in range(4):
                    ins = e.matmul(self.ps[bk][:, h * 128:(h + 1) * 128], kinv[:, h, :], qdec[:, h, :],
                                   start=True, stop=True)
                return ins
            P.op("pe", att, reads=[kinv_b, qdec_b], writes=[self.psb[bk]])
            P.op("dve", lambda e, bk=bk: e.tensor_tensor(out=attnT[:].rearrange("p h n -> p (h n)"), in0=self.ps[bk][:],
                                                         in1=tri4[:].rearrange("p h n -> p (h n)"), op=ALU.mult),
                 reads=[self.psb[bk], tri4_b], writes=[attnT_b])
            obanks = []
            for hp in range(2):
                bk = nb()
                obanks.append(bk)

                def omm(e, bk=bk, hp=hp):
                    ins = None
                    for hl in range(2):
                        h = hp * 2 + hl
                        e.matmul(self.ps[bk][:, hl * 256:(hl + 1) * 256], attnT[:, h, :], vsb[:, h * 256:(h + 1) * 256],
                                 start=True, stop=False)
                        ins = e.matmul(self.ps[bk][:, hl * 256:(hl + 1) * 256], qdec[:, h, :], stb[:, h, :],
                                       start=False, stop=True)
                    return ins
                P.op("pe", omm, reads=[attnT_b, vsb_b, qdec_b, stb_b[hp * 2], stb_b[hp * 2 + 1]],
                     writes=[self.psb[bk]])
            for hp in range(2):
                bk = nb()

                def smm(e, bk=bk, hp=hp):
                    ins = None
                    for hl in range(2):
                        h = hp * 2 + hl
                        ins = e.matmul(self.ps[bk][:, hl * 256:(hl + 1) * 256], kend[:, h * 128:(h + 1) * 128],
                                       vsb[:, h * 256:(h + 1) * 256], start=True, stop=True)
                    return ins
                P.op("pe", smm, reads=[kend_b, vsb_b], writes=[self.psb[bk]])
                for hl in range(2):
                    h = hp * 2 + hl
                    P.op("dve", lambda e, bk=bk, hl=hl, h=h: e.scalar_tensor_tensor(
                        out=stf[:, h, :], in0=stf[:, h, :], scalar=eb[:, h, 127:128],
                        in1=self.ps[bk][:, hl * 256:(hl + 1) * 256], op0=ALU.mult, op1=ALU.add),
                        reads=[stf_b[h], eb_b, self.psb[bk]], writes=[stf_b[h]])
                    P.op("pool", lambda e, h=h: e.tensor_copy(out=stb[:, h, :], in_=stf[:, h, :]),
                         reads=[stf_b[h]], writes=[stb_b[h]])
            for hp in range(2):
                bk = obanks[hp]
                for hl in range(2):
                    h = hp * 2 + hl
                    P.op("act", lambda e, bk=bk, hl=hl, h=h: e.activation(
                        out=junk[:], in_=self.ps[bk][:, hl * 256:(hl + 1) * 256], func=AF.Square,
                        accum_out=ssq[:, h:h + 1]), reads=[self.psb[bk]], writes=[junk_b, ssq_b[h]])
            P.op("act", lambda e: e.activation(out=orstd[:], in_=ssq[:], func=AF.Sqrt, bias=self.c_eps[:],
                                               scale=1.0 / 256), reads=ssq_b + [self.cb_const], writes=[orstd_b])
            P.op("dve", lambda e: e.reciprocal(out=orstd[:], in_=orstd[:]), reads=[orstd_b], writes=[orstd_b])
            for hp in range(2):
                bk = obanks[hp]
                for hl in range(2):
                    h = hp * 2 + hl
                    P.op("dve", lambda e, bk=bk, hl=hl, h=h: e.scalar_tensor_tensor(
                        out=og[:, h * 256:(h + 1) * 256], in0=self.ps[bk][:, hl * 256:(hl + 1) * 256],
                        scalar=orstd[:, h:h + 1], in1=sgw[:, h * 256:(h + 1) * 256], op0=ALU.mult, op1=ALU.mult),
                        reads=[self.psb[bk], orstd_b, sgw_b], writes=[og_b])
            pst, pst_b = self.pst[tb % 2], self.pstb[tb % 2]

            def tr(e, pst=pst):
                ins = None
                for c in range(8):
                    ins = e.transpose(pst[:, c * 128:(c + 1) * 128], og[:, c * 128:(c + 1) * 128], self.c_identb[:])
                return ins
            P.op("pe", tr, reads=[og_b, self.cb_const], writes=[pst_b])
            P.op("act", lambda e, pst=pst, tcs=tcs: e.activation(out=ogT[:, :, tcs],
                                                                in_=pst[:].rearrange("p (c n) -> p c n", c=8),
                                                                func=AF.Copy), reads=[pst_b], writes=[ogT_b[tb]])
        for o in range(8):
            bk = nb()
            self.mm_group(self.ps[bk][:], [(wo[:, kc, o * 128:(o + 1) * 128], ogT[:, kc, :]) for kc in range(8)],
                          [wo_b] + ogT_b, self.psb[bk])
            xr, xr_b, xr_ch = rxr.next()
            P.op("sp", lambda e, xr=xr, o=o, cols=cols: e.dma_start(out=xr[:], in_=self.xT[o * 128:(o + 1) * 128, cols]),
                 reads=[self.xTb[o][cb]], writes=[xr_b], dma=True, chan=xr_ch)
            xo, xo_b, xo_ch = rxo.next()
            P.op("dve", lambda e, xo=xo, xr=xr, bk=bk: e.tensor_tensor(out=xo[:], in0=self.ps[bk][:], in1=xr[:],
                                                                     op=ALU.add),
                 reads=[self.psb[bk], xr_b], writes=[xo_b])
            P.op("sp", lambda e, xo=xo, o=o, cols=cols: e.dma_start(out=self.xT[o * 128:(o + 1) * 128, cols], in_=xo[:]),
                 reads=[xo_b], writes=[self.xTb[o][cb]], dma=True, chan=xo_ch)
    P.barrier()
    A.reset(m0)


K.gla = _gla


def gla_host(inputs, pre, name):
    if name == pre + "_gla_w_in" or name == pre + "_gla_w_out":
        return lay_cols(inputs[name])
    if name == pre + "_gla_w_gate_b":
        return np.ascontiguousarray(np.asarray(inputs[name], np.float32))
    if name == pre + "_gla_b_gate":
        return np.ascontiguousarray(np.asarray(inputs[name], np.float32).reshape(1, 512))
    if name == pre + "_gla_norm":
        return np.ascontiguousarray(np.broadcast_to(np.asarray(inputs[name], np.float32)[None, :], (128, 1024)))
    if name == pre + "_mix_norm":
        return lay_vecp(inputs[name])
    return None


def gla_consts(name):
    j = np.arange(128)[:, None]
    i = np.arange(128)[None, :]
    if name == "c_gla_cum":
        ms = np.where(j > i, -1.0 / 16.0, 0.0)
        mi = np.where(j <= i, -1.0 / 16.0, 0.0)
        tri = np.where(j <= i, 1.0, 0.0)
        return np.ascontiguousarray(np.concatenate([ms, mi, tri], axis=1).astype(np.float32))
    if name == "c_tri4":
        tri = np.where(j <= i, 1.0, 0.0)
        return np.ascontiguousarray(np.concatenate([tri] * 4, axis=1).astype(np.float32))
    return None


def _ssd(self, pre):
    P, A, S, NCB = self.P, self.A, self.S, self.NCB
    nw_d = self.dram_in(pre + "_mix_norm", [128, 8])
    wz_d = self.dram_in(pre + "_ssd_wz", [16, 128, 1024])
    wx_d = self.dram_in(pre + "_ssd_wxbc", [24, 128, 1024])
    wdt_d = self.dram_in(pre + "_ssd_wdt", [128, 8 * 32])
    cw_d = self.dram_in(pre + "_ssd_conv_w", [128, 24 * 4])
    cbias_d = self.dram_in(pre + "_ssd_conv_b", [128, 24])
    dtb_d = self.dram_in(pre + "_ssd_dt_bias", [128, 32])
    alog_d = self.dram_in(pre + "_ssd_a_log", [128, 32])
    dsk_d = self.dram_in(pre + "_ssd_d_skip", [128, 32])
    snw_d = self.dram_in(pre + "_ssd_norm", [128, 16])
    wo_d = self.dram_in(pre + "_ssd_w_out", [8, 128, 2048])
    cs_d = self.dram_in("c_ssd", [128, 3 * 512])
    m0 = A.mark()

    def ld(shape, dtype, src, eng="sp", name="w", view=None):
        t = A.alloc(shape, dtype, name)
        b = P.buf(name)
        dst = t[:] if view is None else view(t)
        P.op(eng, lambda e: e.dma_start(out=dst, in_=src), writes=[b], dma=True, chan=P.chan(name, eng))
        return t, b

    nw, nw_b = ld([128, 8], F32, nw_d, name="nw")
    wdt, wdt_b = ld([128, 8, 32], BF16, wdt_d, "pool", "wdt", lambda t: t[:].rearrange("p k n -> p (k n)"))
    cw, cw_b = ld([128, 24, 4], F32, cw_d, name="cw", view=lambda t: t[:].rearrange("p c k -> p (c k)"))
    cbias, cbias_b = ld([128, 24], F32, cbias_d, name="cbias")
    dtb, dtb_b = ld([128, 32], F32, dtb_d, name="dtb")
    ealog, ealog_b = ld([128, 32], F32, alog_d, name="alog")
    dsk, dsk_b = ld([128, 32], F32, dsk_d, name="dsk")
    snw, snw_b = ld([128, 16], F32, snw_d, name="snw")
    cs, cs_b = ld([128, 3, 512], F32, cs_d, name="cssd", view=lambda t: t[:].rearrange("p a n -> p (a n)"))
    ident4 = cs[:, 0, :]
    maskT4 = cs[:, 1, :]
    tri = cs[:, 2, 0:128]
    sel = cs[:, 2, 128:256]
    onesf = cs[:, 2, 256:384]
    P.op("act", lambda e: e.activation(out=ealog[:], in_=ealog[:], func=AF.Exp), reads=[ealog_b], writes=[ealog_b])
    c1 = A.alloc([128, 1], F32, "c1")
    c1_b = P.buf("c1")
    P.op("dve", lambda e: e.memset(c1[:], 1.0), writes=[c1_b])

    xin = A.alloc([128, 8, 512], F32, "xin")
    xin_b = P.buf("xin")
    xin_ch = P.chan("xin")
    sq = A.alloc([128, 8, 512], BF16, "sq")
    sq_b = P.buf("sq")
    rstd = A.alloc([128, 512], F32, "rstd")
    rstd_b = P.buf("rstd")
    xn = A.alloc([128, 8, 512], BF16, "xn")
    xn_b = P.buf("xn")
    rw = Ring(self, 3, [128, 8, 128], BF16, "wst", eng="pool")
    ru = Ring(self, 2, [128, 515], F32, "u", chan=False)
    racc = Ring(self, 2, [128, 512], F32, "acc", chan=False)
    rxc = Ring(self, 2, [128, 512], BF16, "xc", chan=False)
    halo = A.alloc([128, 24, 3], F32, "halo")
    halo_b = [P.buf(f"halo{c}") for c in range(24)]
    P.op("pool", lambda e: e.memset(halo[:], 0.0), writes=halo_b)
    xtm = A.alloc([128, 4, 2048], BF16, "xtm")
    xtm_b = [P.buf(f"xtm{c}") for c in range(16)]
    btm = A.alloc([128, 4, 512], BF16, "btm")
    btm_b = [P.buf(f"btm{g}") for g in range(4)]
    BT = A.alloc([128, 4, 512], BF16, "BT")
    BT_b = [P.buf(f"BT{g}") for g in range(4)]
    CT = A.alloc([128, 4, 512], BF16, "CT")
    CT_b = [P.buf(f"CT{g}") for g in range(4)]
    szT = A.alloc([128, 16, 512], BF16, "szT")
    szT_b = [P.buf(f"szT{c}") for c in range(16)]
    dtp = A.alloc([128, 32], F32, "dtp")
    dtp_b = P.buf("dtp")
    dt = A.alloc([128, 32], F32, "dt")
    dt_b = P.buf("dt")
    dta = A.alloc([128, 32], F32, "dta")
    dta_b = P.buf("dta")
    acs = A.alloc([128, 32], F32, "acs")
    acs_b = P.buf("acs")
    ea = A.alloc([128, 32], F32, "ea")
    ea_b = P.buf("ea")
    dec = A.alloc([128, 32], F32, "dec")
    dec_b = P.buf("dec")
    r2 = A.alloc([128, 8, 64], F32, "r2")
    r2_b = P.buf("r2")
    rtot = Ring(self, 2, [128, 512], F32, "totbc", chan=False)
    xdt = A.alloc([128, 4, 512], BF16, "xdt")
    xdt_b = [P.buf(f"xdt{g}") for g in range(4)]
    xdd = A.alloc([128, 4, 512], BF16, "xdd")
    xdd_b = [P.buf(f"xdd{g}") for g in range(4)]
    cbT = A.alloc([128, 4, 128], F32, "cbT")
    cbT_b = P.buf("cbT")
    rD = Ring(self, 2, [128, 4, 128], F32, "Dm", chan=False)
    rR = Ring(self, 2, [128, 4, 128], F32, "r23", chan=False)
    rLT = Ring(self, 2, [128, 4, 128], F32, "LT", chan=False)
    rWT = Ring(self, 2, [128, 4, 128], BF16, "WT", chan=False)
    stf = A.alloc([128, 4, 512], F32, "sstf")
    stf_b = [P.buf(f"sstf{g}") for g in range(4)]
    stb = A.alloc([128, 4, 512], BF16, "sstb")
    stb_b = [P.buf(f"sstb{g}") for g in range(4)]
    for g in range(4):
        P.op("dve", lambda e, g=g: e.memset(stf[:, g, :], 0.0), writes=[stf_b[g]])
        P.op("pool", lambda e, g=g: e.memset(stb[:, g, :], 0.0), writes=[stb_b[g]])
    rtmp = Ring(self, 2, [128, 8, 64], F32, "ytmp", chan=False)
    rtmp2 = Ring(self, 2, [128, 8, 64], F32, "ytmp2", chan=False)
    ry = Ring(self, 2, [128, 512], F32, "yg", chan=False)
    ygT = A.alloc([128, 16, 128], F32, "ygT")
    ygT_b = [P.buf(f"ygT{g}") for g in range(4)]
    ysq = A.alloc([128, 16, 128], BF16, "ysq")
    ysq_b = [P.buf(f"ysq{g}") for g in range(4)]
    yrs = A.alloc([128, 4, 128], F32, "yrs")
    yrs_b = P.buf("yrs")
    ynT = A.alloc([128, 16, 512], BF16, "ynT")
    ynT_b = [P.buf(f"ynT{t}") for t in range(4)]
    rwo = Ring(self, 2, [128, 16, 128], BF16, "swo", eng="pool")
    rxr = Ring(self, 2, [128, 512], F32, "xres")
    rxo = Ring(self, 2, [128, 512], F32, "xo")
    bank = [0]
    cpi = [0]

    def nb():
        bank[0] = (bank[0] + 1) % 5
        return bank[0]

    def bc(ap2d, n):
        k = ap2d.shape[1]
        return ap2d.unsqueeze(2).to_broadcast([128, k, n])

    for cb in range(NCB):
        cols = slice(cb * 512, (cb + 1) * 512)
        self.load_xcols(cb, xin, xin_b, xin_ch)
        self.norm_block(cb, xin, xin_b, sq, sq_b, rstd, rstd_b, nw, nw_b, 5, lambda c: (xn[:, c, :], xn_b))
        for c in range(24):
            w, w_b, w_ch = rw.next()
            P.op("pool", lambda e, w=w, c=c: e.dma_start(out=w[:].rearrange("p k j -> p (k j)"), in_=wx_d[c]),
                 writes=[w_b], dma=True, chan=w_ch)
            bk = nb()
            self.mm_group(self.ps[bk][:], [(w[:, kc, :], xn[:, kc, :]) for kc in range(8)], [w_b, xn_b], self.psb[bk])
            u, u_b, _ = ru.next()
            P.op("act", lambda e, u=u, bk=bk: e.activation(out=u[:, 3:515], in_=self.ps[bk][:], func=AF.Copy),
                 reads=[self.psb[bk]], writes=[u_b])
            P.op("pool", lambda e, u=u, c=c: e.tensor_copy(out=u[:, 0:3], in_=halo[:, c, :]),
                 reads=[halo_b[c]], writes=[u_b])
            P.op("pool", lambda e, u=u, c=c: e.tensor_copy(out=halo[:, c, :], in_=u[:, 512:515]),
                 reads=[u_b], writes=[halo_b[c]])
            acc, acc_b, _ = racc.next()
            P.op("pool", lambda e, u=u, acc=acc, c=c: e.tensor_scalar(acc[:], u[:, 0:512], cw[:, c, 0:1], None, ALU.mult),
                 reads=[u_b, cw_b], writes=[acc_b])
            for k in range(1, 4):
                P.op("dve", lambda e, u=u, acc=acc, c=c, k=k: e.scalar_tensor_tensor(
                    out=acc[:], in0=u[:, k:k + 512], scalar=cw[:, c, k:k + 1], in1=acc[:], op0=ALU.mult, op1=ALU.add),
                    reads=[u_b, cw_b, acc_b], writes=[acc_b])
            if c < 20:
                xc, xc_b, _ = rxc.next()
                if c >= 16:
                    dst, dst_b = BT[:, c - 16, :], BT_b[c - 16]
                    P.op("act", lambda e, acc=acc, dst=dst, c=c: e.activation(out=dst, in_=acc[:], func=AF.Silu,
                                                                            bias=cbias[:, c:c + 1], scale=1.0),
                         reads=[acc_b, cbias_b], writes=[dst_b])
                    src, src_b = dst, dst_b
                else:
                    P.op("act", lambda e, acc=acc, xc=xc, c=c: e.activation(out=xc[:], in_=acc[:], func=AF.Silu,
                                                                          bias=cbias[:, c:c + 1], scale=1.0),
                         reads=[acc_b, cbias_b], writes=[xc_b])
                    src, src_b = xc[:], xc_b
                pst, pst_b = self.pst[c % 2], self.pstb[c % 2]

                def tr(e, pst=pst, src=src):
                    ins = None
                    for tb in range(4):
                        ins = e.transpose(pst[:, tb * 128:(tb + 1) * 128], src[:, tb * 128:(tb + 1) * 128],
                                          self.c_identb[:])
                    return ins
                P.op("pe", tr, reads=[src_b, self.cb_const], writes=[pst_b])
                if c < 16:
                    o_ap, o_b = xtm[:, :, c * 128:(c + 1) * 128], xtm_b[c]
                else:
                    o_ap, o_b = btm[:, :, (c - 16) * 128:(c - 15) * 128], btm_b[c - 16]
                cpi[0] += 1
                i_ap = pst[:, 0:512].rearrange("p (t n) -> p t n", t=4)
                if cpi[0] % 2 == 0:
                    P.op("act", lambda e, o_ap=o_ap, i_ap=i_ap: e.activation(out=o_ap, in_=i_ap, func=AF.Copy),
                         reads=[pst_b], writes=[o_b])
                else:
                    P.op("dve", lambda e, o_ap=o_ap, i_ap=i_ap: e.tensor_copy(out=o_ap, in_=i_ap),
                         reads=[pst_b], writes=[o_b])
            else:
                g = c - 20
                P.op("act", lambda e, acc=acc, g=g, c=c: e.activation(out=CT[:, g, :], in_=acc[:], func=AF.Silu,
                                                                    bias=cbias[:, c:c + 1], scale=1.0),
                     reads=[acc_b, cbias_b], writes=[CT_b[g]])
        for c in range(16):
            w, w_b, w_ch = rw.next()
            P.op("pool", lambda e, w=w, c=c: e.dma_start(out=w[:].rearrange("p k j -> p (k j)"), in_=wz_d[c]),
                 writes=[w_b], dma=True, chan=w_ch)
            bk = nb()
            self.mm_group(self.ps[bk][:], [(w[:, kc, :], xn[:, kc, :]) for kc in range(8)], [w_b, xn_b], self.psb[bk])
            P.op("act", lambda e, c=c, bk=bk: e.activation(out=szT[:, c, :], in_=self.ps[bk][:], func=AF.Silu),
                 reads=[self.psb[bk]], writes=[szT_b[c]])
        for tb in range(4):
            tcs = slice(tb * 128, (tb + 1) * 128)
            bk = nb()
            self.mm_group(self.ps[bk][:, 0:32], [(xn[:, kc, tcs], wdt[:, kc, :]) for kc in range(8)],
                          [xn_b, wdt_b], self.psb[bk])
            P.op("dve", lambda e, bk=bk: e.tensor_tensor(out=dtp[:], in0=self.ps[bk][:, 0:32], in1=dtb[:], op=ALU.add),
                 reads=[self.psb[bk], dtb_b], writes=[dtp_b])
            P.op("act", lambda e: e.activation(out=dtp[:], in_=dtp[:], func=AF.Exp), reads=[dtp_b], writes=[dtp_b])
            P.op("act", lambda e: e.activation(out=dt[:], in_=dtp[:], func=AF.Ln, bias=c1[:], scale=1.0),
                 reads=[dtp_b, c1_b], writes=[dt_b])
            P.op("dve", lambda e: e.scalar_tensor_tensor(out=dta[:], in0=dt[:], scalar=-1.0, in1=ealog[:],
                                                         op0=ALU.mult, op1=ALU.mult),
                 reads=[dt_b, ealog_b], writes=[dta_b])
            bk = nb()
            self.mm_group(self.ps[bk][:, 0:32], [(tri, dta[:])], [cs_b, dta_b], self.psb[bk])
            P.op("dve", lambda e, bk=bk: e.tensor_copy(out=acs[:], in_=self.ps[bk][:, 0:32]),
                 reads=[self.psb[bk]], writes=[acs_b])
            P.op("act", lambda e: e.activation(out=ea[:], in_=acs[:], func=AF.Exp), reads=[acs_b], writes=[ea_b])
            bk = nb()
            self.mm_group(self.ps[bk][:, 0:32], [(sel, acs[:])], [cs_b, acs_b], self.psb[bk])
            P.op("dve", lambda e, bk=bk: e.tensor_tensor(out=dec[:], in0=self.ps[bk][:, 0:32], in1=acs[:],
                                                         op=ALU.subtract),
                 reads=[self.psb[bk], acs_b], writes=[dec_b])
            P.op("act", lambda e: e.activation(out=dec[:], in_=dec[:], func=AF.Exp), reads=[dec_b], writes=[dec_b])
            for g in range(4):
                gs = slice(g * 8, (g + 1) * 8)
                gc = slice(g * 512, (g + 1) * 512)
                P.op("pool", lambda e, g=g, gs=gs, gc=gc, tb=tb: e.tensor_tensor(
                    out=xdt[:, g, :].rearrange("p (h n) -> p h n", h=8),
                    in0=xtm[:, tb, gc].rearrange("p (h n) -> p h n", h=8), in1=bc(dt[:, gs], 64), op=ALU.mult),
                    reads=xtm_b[g * 4:(g + 1) * 4] + [dt_b], writes=[xdt_b[g]])
                P.op("pool", lambda e, g=g, gs=gs: e.tensor_tensor(
                    out=xdd[:, g, :].rearrange("p (h n) -> p h n", h=8),
                    in0=xdt[:, g, :].rearrange("p (h n) -> p h n", h=8), in1=bc(dec[:, gs], 64), op=ALU.mult),
                    reads=[xdt_b[g], dec_b], writes=[xdd_b[g]])
            bk = nb()

            def cbmm(e, bk=bk, tcs=tcs):
                ins = None
                for g in range(4):
                    ins = e.matmul(self.ps[bk][:, g * 128:(g + 1) * 128], BT[:, g, tcs], CT[:, g, tcs],
                                   start=True, stop=True)
                return ins
            P.op("pe", cbmm, reads=BT_b + CT_b, writes=[self.psb[bk]])
            P.op("act", lambda e, bk=bk: e.activation(out=cbT[:].rearrange("p g n -> p (g n)"), in_=self.ps[bk][:],
                                                      func=AF.Copy), reads=[self.psb[bk]], writes=[cbT_b])
            for g in range(4):
                gs = slice(g * 8, (g + 1) * 8)
                gc = slice(g * 512, (g + 1) * 512)
                ybk = nb()
                for sl in range(2):
                    h0 = g * 8 + sl * 4
                    hs = slice(h0, h0 + 4)
                    Dm, Dm_b, _ = rD.next()
                    r23, r23_b, _ = rR.next()
                    P.op("pool", lambda e, Dm=Dm, hs=hs: e.tensor_tensor(
                        out=Dm[:], in0=ident4.rearrange("p (h n) -> p h n", h=4), in1=bc(acs[:, hs], 128), op=ALU.mult),
                        reads=[cs_b, acs_b], writes=[Dm_b])
                    P.op("pool", lambda e, r23=r23, hs=hs: e.tensor_tensor(
                        out=r23[:], in0=maskT4.rearrange("p (h n) -> p h n", h=4), in1=bc(acs[:, hs], 128),
                        op=ALU.subtract), reads=[cs_b, acs_b], writes=[r23_b])
                    bk = nb()
                    if bk == ybk:
                        bk = nb()
                    self.mm_group(self.ps[bk][:], [(onesf, Dm[:].rearrange("p h n -> p (h n)")),
                                                   (self.c_identf[:], r23[:].rearrange("p h n -> p (h n)"))],
                                  [cs_b, Dm_b, r23_b, self.cb_const], self.psb[bk])
                    LT, LT_b, _ = rLT.next()
                    P.op("act", lambda e, LT=LT, bk=bk: e.activation(out=LT[:].rearrange("p h n -> p (h n)"),
                                                                   in_=self.ps[bk][:], func=AF.Exp),
                         reads=[self.psb[bk]], writes=[LT_b])
                    WT, WT_b, _ = rWT.next()
                    P.op("dve", lambda e, WT=WT, LT=LT, g=g: e.tensor_tensor(
                        out=WT[:], in0=LT[:], in1=cbT[:, g:g + 1, :].to_broadcast([128, 4, 128]), op=ALU.mult),
                        reads=[LT_b, cbT_b], writes=[WT_b])

                    def ydm(e, WT=WT, ybk=ybk, g=g, sl=sl):
                        ins = None
                        for hl in range(4):
                            hh = sl * 4 + hl
                            ins = e.matmul(self.ps[ybk][:, hh * 64:(hh + 1) * 64], WT[:, hl, :],
                                           xdt[:, g, hh * 64:(hh + 1) * 64], start=True, stop=True)
                        return ins
                    P.op("pe", ydm, reads=[WT_b, xdt_b[g]], writes=[self.psb[ybk]])
                obk = nb()
                if obk == ybk:
                    obk = nb()
                self.mm_group(self.ps[obk][:], [(CT[:, g, tcs], stb[:, g, :])], [CT_b[g], stb_b[g]], self.psb[obk])
                tmp, tmp_b, _ = rtmp.next()
                tmp2, tmp2_b, _ = rtmp2.next()
                P.op("dve", lambda e, tmp=tmp, obk=obk, gs=gs: e.tensor_tensor(
                    out=tmp[:], in0=self.ps[obk][:].rearrange("p (h n) -> p h n", h=8), in1=bc(ea[:, gs], 64),
                    op=ALU.mult), reads=[self.psb[obk], ea_b], writes=[tmp_b])
                P.op("pool", lambda e, tmp2=tmp2, gs=gs, gc=gc, tb=tb: e.tensor_tensor(
                    out=tmp2[:], in0=xtm[:, tb, gc].rearrange("p (h n) -> p h n", h=8), in1=bc(dsk[:, gs], 64),
                    op=ALU.mult), reads=xtm_b[g * 4:(g + 1) * 4] + [dsk_b], writes=[tmp2_b])
                P.op("pool", lambda e, tmp=tmp, tmp2=tmp2: e.tensor_tensor(out=tmp[:], in0=tmp[:], in1=tmp2[:],
                                                                          op=ALU.add),
                     reads=[tmp_b, tmp2_b], writes=[tmp_b])
                y, y_b, _ = ry.next()
                P.op("dve", lambda e, y=y, tmp=tmp, ybk=ybk: e.tensor_tensor(
                    out=y[:], in0=self.ps[ybk][:], in1=tmp[:].rearrange("p h n -> p (h n)"), op=ALU.add),
                    reads=[self.psb[ybk], tmp_b], writes=[y_b])
                P.op("pool", lambda e, gs=gs: e.tensor_copy(out=r2[:], in_=bc(acs[:, gs], 64)),
                     reads=[acs_b], writes=[r2_b])
                bk = nb()
                self.mm_group(self.ps[bk][:], [(sel, r2[:].rearrange("p h n -> p (h n)"))], [cs_b, r2_b], self.psb[bk])
                tot, tot_b, _ = rtot.next()
                P.op("act", lambda e, bk=bk, tot=tot: e.activation(out=tot[:], in_=self.ps[bk][:], func=AF.Exp),
                     reads=[self.psb[bk]], writes=[tot_b])
                sbk = nb()
                self.mm_group(self.ps[sbk][:], [(btm[:, tb, g * 128:(g + 1) * 128], xdd[:, g, :])],
                              [btm_b[g], xdd_b[g]], self.psb[sbk])
                P.op("dve", lambda e, g=g, tot=tot: e.tensor_tensor(out=stf[:, g, :], in0=stf[:, g, :], in1=tot[:],
                                                                    op=ALU.mult),
                     reads=[stf_b[g], tot_b], writes=[stf_b[g]])
                P.op("dve", lambda e, g=g, sbk=sbk: e.tensor_tensor(out=stf[:, g, :], in0=stf[:, g, :],
                                                                    in1=self.ps[sbk][:], op=ALU.add),
                     reads=[stf_b[g], self.psb[sbk]], writes=[stf_b[g]])
                P.op("pool", lambda e, g=g: e.tensor_copy(out=stb[:, g, :], in_=stf[:, g, :]),
                     reads=[stf_b[g]], writes=[stb_b[g]])
                tbk = nb()

                def ytr(e, tbk=tbk, y=y):
                    ins = None
                    for q in range(4):
                        ins = e.transpose(self.ps[tbk][:, q * 128:(q + 1) * 128], y[:, q * 128:(q + 1) * 128],
                                          self.c_identf[:])
                    return ins
                P.op("pe", ytr, reads=[y_b, self.cb_const], writes=[self.psb[tbk]])
                P.op("dve", lambda e, tbk=tbk, g=g, tcs=tcs: e.tensor_tensor(
                    out=ygT[:, g * 4:(g + 1) * 4, :], in0=self.ps[tbk][:].rearrange("p (q n) -> p q n", q=4),
                    in1=szT[:, g * 4:(g + 1) * 4, tcs], op=ALU.mult),
                    reads=[self.psb[tbk]] + szT_b[g * 4:(g + 1) * 4], writes=[ygT_b[g]])
                P.op("act", lambda e, g=g: e.activation(out=ysq[:, g * 4:(g + 1) * 4, :], in_=ygT[:, g * 4:(g + 1) * 4, :],
                                                        func=AF.Square), reads=[ygT_b[g]], writes=[ysq_b[g]])
            nbk = nb()

            def nrm(e, nbk=nbk):
                ins = None
                for g in range(4):
                    for q in range(4):
                        ins = e.matmul(self.ps[nbk][:, g * 128:(g + 1) * 128], self.c_onesb[:], ysq[:, g * 4 + q, :],
                                       start=(q == 0), stop=(q == 3))
                return ins
            P.op("pe", nrm, reads=ysq_b + [self.cb_const], writes=[self.psb[nbk]])
            P.op("act", lambda e, nbk=nbk: e.activation(out=yrs[:].rearrange("p g n -> p (g n)"), in_=self.ps[nbk][:],
                                                        func=AF.Sqrt, bias=self.c_eps[:], scale=1.0 / 512),
                 reads=[self.psb[nbk], self.cb_const], writes=[yrs_b])
            P.op("dve", lambda e: e.reciprocal(out=yrs[:], in_=yrs[:]), reads=[yrs_b], writes=[yrs_b])
            for kc in range(16):
                g = kc // 4
                P.op("dve", lambda e, kc=kc, g=g, tcs=tcs: e.scalar_tensor_tensor(
                    out=ynT[:, kc, tcs], in0=ygT[:, kc, :], scalar=snw[:, kc:kc + 1], in1=yrs[:, g, :],
                    op0=ALU.mult, op1=ALU.mult), reads=[ygT_b[g], snw_b, yrs_b], writes=[ynT_b[tb]])
        for o in range(8):
            wo, wo_b, wo_ch = rwo.next()
            P.op("pool", lambda e, wo=wo, o=o: e.dma_start(out=wo[:].rearrange("p k j -> p (k j)"), in_=wo_d[o]),
                 writes=[wo_b], dma=True, chan=wo_ch)
            bk = nb()
            self.mm_group(self.ps[bk][:], [(wo[:, kc, :], ynT[:, kc, :]) for kc in range(16)], [wo_b] + ynT_b,
                          self.psb[bk])
            xr, xr_b, xr_ch = rxr.next()
            P.op("sp", lambda e, xr=xr, o=o, cols=cols: e.dma_start(out=xr[:], in_=self.xT[o * 128:(o + 1) * 128, cols]),
                 reads=[self.xTb[o][cb]], writes=[xr_b], dma=True, chan=xr_ch)
            xo, xo_b, xo_ch = rxo.next()
            P.op("dve", lambda e, xo=xo, xr=xr, bk=bk: e.tensor_tensor(out=xo[:], in0=self.ps[bk][:], in1=xr[:],
                                                                     op=ALU.add),
                 reads=[self.psb[bk], xr_b], writes=[xo_b])
            P.op("sp", lambda e, xo=xo, o=o, cols=cols: e.dma_start(out=self.xT[o * 128:(o + 1) * 128, cols], in_=xo[:]),
                 reads=[xo_b], writes=[self.xTb[o][cb]], dma=True, chan=xo_ch)
    P.barrier()
    A.reset(m0)


K.ssd = _ssd


def lay_chunks(w):
    w = np.asarray(w, np.float32)
    kc, n = w.shape[0] // 128, w.shape[1] // 128
    a = w.reshape(kc, 128, n, 128).transpose(2, 1, 0, 3)
    return np.ascontiguousarray(a).reshape(n, 128, kc * 128)


def rep128(v):
    v = np.asarray(v, np.float32).reshape(1, -1)
    return np.ascontiguousarray(np.broadcast_to(v, (128, v.shape[1])))


def ssd_host(inputs, pre, name):
    if name == pre + "_ssd_wz":
        return lay_chunks(np.asarray(inputs[pre + "_ssd_w_in"])[:, 0:2048])
    if name == pre + "_ssd_wxbc":
        return lay_chunks(np.asarray(inputs[pre + "_ssd_w_in"])[:, 2048:5120])
    if name == pre + "_ssd_wdt":
        return lay_cols(np.asarray(inputs[pre + "_ssd_w_in"])[:, 5120:5152])
    if name == pre + "_ssd_conv_w":
        w = np.asarray(inputs[name], np.float32)
        return np.ascontiguousarray(w.reshape(4, 24, 128).transpose(2, 1, 0)).reshape(128, 96)
    if name == pre + "_ssd_conv_b":
        return lay_vecp(inputs[name])
    if name in (pre + "_ssd_dt_bias", pre + "_ssd_a_log", pre + "_ssd_d_skip"):
        return rep128(inputs[name])
    if name == pre + "_ssd_norm" or name == pre + "_mix_norm":
        return lay_vecp(inputs[name])
    if name == pre + "_ssd_w_out":
        return lay_chunks(inputs[name])
    return None


def ssd_consts(name):
    if name != "c_ssd":
        return None
    j = np.arange(128)[:, None]
    i = np.arange(128)[None, :]
    ident = (j == i).astype(np.float32)
    maskT = np.where(i < j, -30000.0, 0.0).astype(np.float32)
    tri = (j <= i).astype(np.float32)
    sel = np.zeros((128, 128), np.float32)
    sel[127, :] = 1.0
    ones = np.ones((128, 128), np.float32)
    z = np.zeros((128, 128), np.float32)
    return np.ascontiguousarray(np.concatenate([ident] * 4 + [maskT] * 4 + [tri, sel, ones, z], axis=1))
```

```python
import contextlib
import numpy as np
import concourse.bass as bass
import concourse.mybir as mybir
from concourse.bass_utils import run_bass_kernel_spmd

F32 = mybir.dt.float32
BF16 = mybir.dt.bfloat16
I32 = mybir.dt.int32
AF = mybir.ActivationFunctionType
ALU = mybir.AluOpType
AX = mybir.AxisListType

D = 1024
DFF = 2816
NFC = DFF // 128
EPS = 1e-6
SBUF_BASE = 16640
SBUF_END = 229376
DEBUG_STOP = 0


class Buf:
    __slots__ = ("name", "last_w", "readers")

    def __init__(self, name=""):
        self.name = name
        self.last_w = None
        self.readers = []


class Op:
    __slots__ = ("idx", "eng", "fn", "deps", "sem", "val", "signal", "dma", "chan", "final", "used")

    def __init__(self):
        self.signal = False
        self.sem = None
        self.val = None
        self.final = False
        self.used = False


class Chan:
    __slots__ = ("sem", "count", "name", "rec")

    def __init__(self, name):
        self.name = name
        self.sem = None
        self.count = 0
        self.rec = 0


class Prog:
    ENGS = ("pe", "act", "dve", "pool", "sp")
    SAME_ENGINE_SYNC = True
    EPOCH = 8000
    CHAN_MAX = 480

    def __init__(self, nc):
        self.nc = nc
        self.ops = []
        self.chans = []
        self.fence_deps = []
        self.last_nd = {e: None for e in self.ENGS}
        self.pending_dma = {}
        self.free_chans = {}
        self.live_chans = []

    def buf(self, name=""):
        return Buf(name)

    def chan(self, name="", eng="sp"):
        kind = "sw" if eng == "pool" else "hw"
        fl = self.free_chans.setdefault(kind, [])
        if fl:
            c = fl.pop()
        else:
            c = Chan(f"{kind}{len(self.chans)}")
            self.chans.append(c)
        self.live_chans.append((kind, c))
        return c

    def op(self, eng, fn, reads=(), writes=(), dma=False, chan=None):
        o = Op()
        o.idx = len(self.ops)
        o.eng = eng
        o.fn = fn
        o.dma = dma
        o.chan = chan
        deps = set(self.fence_deps)
        for b in reads:
            if b.last_w is not None:
                deps.add(b.last_w)
        for b in writes:
            if b.last_w is not None:
                deps.add(b.last_w)
            deps.update(b.readers)
        o.deps = deps
        for d in deps:
            d.signal = True
            if d.dma:
                self.pending_dma.pop(d, None)
        if dma:
            assert chan is not None
            o.signal = True
            chan.rec += 1
            self.pending_dma[o] = True
        else:
            self.last_nd[eng] = o
        for b in reads:
            b.readers.append(o)
        for b in writes:
            b.last_w = o
            b.readers = []
        self.ops.append(o)
        return o

    def barrier(self):
        deps = [o for o in self.last_nd.values() if o is not None]
        deps += list(self.pending_dma.keys())
        for d in deps:
            d.signal = True
        self.fence_deps = deps
        for kind, c in self.live_chans:
            if c.rec < self.CHAN_MAX:
                self.free_chans.setdefault(kind, []).append(c)
        self.live_chans = []

    def emit(self):
        nc = self.nc
        per_eng = {e: [o for o in self.ops if o.eng == e] for e in self.ENGS}
        n_sig = {e: sum(1 for o in per_eng[e] if o.signal and not o.dma) for e in self.ENGS}
        stats = {e: [0, 0] for e in self.ENGS}
        with contextlib.ExitStack() as st:
            eng_sems = {}
            for e in self.ENGS:
                n_ep = max(1, (n_sig[e] + self.EPOCH - 1) // self.EPOCH)
                eng_sems[e] = [st.enter_context(nc.semaphore(f"s_{e}{i}")) for i in range(n_ep)]
            for c in self.chans:
                c.sem = st.enter_context(nc.semaphore(f"c_{c.name}"))
                c.count = 0
            for o in self.ops:
                if o.dma:
                    o.chan.count += 1
                    o.sem = o.chan.sem
                    o.val = 16 * o.chan.count
            for e in self.ENGS:
                cnt = 0
                for o in per_eng[e]:
                    if o.dma:
                        pass
                    elif o.signal:
                        ep = cnt // self.EPOCH
                        cnt += 1
                        o.sem = eng_sems[e][ep]
                        o.val = cnt - ep * self.EPOCH
            engobj = {"pe": nc.tensor, "act": nc.scalar, "dve": nc.vector, "pool": nc.gpsimd,
                      "sp": nc.sync}

            def run_engine(e):
                eng = engobj[e]
                waited = {}
                for o in per_eng[e]:
                    for d in sorted(o.deps, key=lambda d: d.idx):
                        if d.eng == e and not d.dma:
                            if e == "pe" or not self.SAME_ENGINE_SYNC:
                                continue
                        key = d.sem.num
                        if waited.get(key, 0) >= d.val:
                            continue
                        eng.wait_ge(d.sem, d.val)
                        stats[e][1] += 1
                        waited[key] = d.val
                    ins = o.fn(eng)
                    stats[e][0] += 1
                    if o.dma:
                        ins.then_inc(o.sem, 16)
                    elif o.signal:
                        ins.then_inc(o.sem, 1)
                if e == "sp":
                    for o in self.ops:
                        if o.dma and o.final and waited.get(o.sem.num, 0) < o.val:
                            eng.wait_ge(o.sem, o.val)
                            waited[o.sem.num] = o.val

            with nc.Block() as block:
                @block.tensor
                def _(eng):
                    run_engine("pe")

                @block.scalar
                def _(eng):
                    run_engine("act")

                @block.vector
                def _(eng):
                    run_engine("dve")

                @block.gpsimd
                def _(eng):
                    run_engine("pool")

                @block.sync
                def _(eng):
                    run_engine("sp")
        return stats


class Arena:
    def __init__(self, nc, base=SBUF_BASE, end=SBUF_END):
        self.nc = nc
        self.base = base
        self.end = end
        self.off = base
        self.n = 0

    def alloc(self, shape, dtype, name="t"):
        esz = 2 if dtype == BF16 else 4
        nbytes = int(np.prod(shape[1:])) * esz
        nbytes = (nbytes + 63) // 64 * 64
        assert self.off + nbytes <= self.end, f"SBUF overflow allocating {name} {shape}: off={self.off}"
        self.n += 1
        t = self.nc.alloc_sbuf_tensor_at(f"{name}_{self.n}", list(shape), dtype, offset=self.off)
        self.off += nbytes
        return t

    def mark(self):
        return self.off

    def reset(self, m):
        self.off = m


class Ring:
    def __init__(self, K, n, shape, dtype, name, chan=True, eng="sp"):
        self.items = []
        for i in range(n):
            t = K.A.alloc(shape, dtype, name)
            self.items.append((t, K.P.buf(f"{name}{i}"), K.P.chan(f"{name}{i}", eng) if chan else None))
        self.i = 0

    def next(self):
        it = self.items[self.i % len(self.items)]
        self.i += 1
        return it


class K:
    def __init__(self, S):
        self.S = S
        self.NCB = S // 512
        self.nc = bass.Bass("TRN2", target_bir_lowering=False)
        self.P = Prog(self.nc)
        self.A = Arena(self.nc)
        self.inputs = {}
        self.dram_aps = {}
        nc = self.nc
        self.ps = [nc.alloc_psum_tensor(f"ps{i}", [128, 512], F32) for i in range(6)]
        self.psb = [self.P.buf(f"ps{i}") for i in range(6)]
        self.pst = [nc.alloc_psum_tensor(f"pst{i}", [128, 1024], BF16) for i in range(2)]
        self.pstb = [self.P.buf(f"pst{i}") for i in range(2)]
        self.xT = nc.dram_tensor("xT_scr", [D, S], F32, kind="Internal").ap()
        self.xTb = [[self.P.buf(f"xT{c}_{cb}") for cb in range(self.NCB)] for c in range(8)]
        self.xTv = self.xT.rearrange("(c p) s -> p c s", p=128)

    def dram_in(self, name, shape, dtype=F32):
        if name in self.dram_aps:
            return self.dram_aps[name]
        t = self._dram_in(name, shape, dtype)
        self.dram_aps[name] = t
        return t

    def _dram_in(self, name, shape, dtype=F32):
        t = self.nc.dram_tensor(name, list(shape), dtype, kind="ExternalInput").ap()
        self.inputs[name] = (tuple(shape), dtype)
        return t

    def load_consts(self):
        P, A = self.P, self.A
        self.c_identf = A.alloc([128, 128], F32, "identf")
        self.c_identb = A.alloc([128, 128], BF16, "identb")
        self.c_onesb = A.alloc([128, 128], BF16, "onesb")
        d_ident = self.dram_in("c_ident", [128, 128])
        self.cb_const = P.buf("consts")
        ch = P.chan("const")
        P.op("sp", lambda e: e.dma_start(out=self.c_identf[:], in_=d_ident), writes=[self.cb_const],
             dma=True, chan=ch)
        P.op("dve", lambda e: e.tensor_copy(out=self.c_identb[:], in_=self.c_identf[:]),
             reads=[self.cb_const], writes=[self.cb_const])
        P.op("dve", lambda e: e.memset(self.c_onesb[:], 1.0), writes=[self.cb_const])
        self.c_eps = A.alloc([128, 1], F32, "eps")
        P.op("dve", lambda e: e.memset(self.c_eps[:], EPS), writes=[self.cb_const])

    def mm_group(self, ps_ap, pairs, reads, psbuf):
        n = len(pairs)

        def fn(e):
            ins = None
            for i, (l, r) in enumerate(pairs):
                ins = e.matmul(ps_ap, l, r, start=(i == 0), stop=(i == n - 1))
            return ins
        return self.P.op("pe", fn, reads=reads, writes=[psbuf])

    def norm_block(self, cb, xin, xin_b, sq, sq_b, rstd, rstd_b, nw, nw_b, ps_i, out_fn):
        P = self.P
        P.op("act", lambda e: e.activation(out=sq[:], in_=xin[:], func=AF.Square),
             reads=[xin_b], writes=[sq_b])
        ps, psb = self.ps[ps_i], self.psb[ps_i]
        self.mm_group(ps[:], [(self.c_onesb[:], sq[:, c, :]) for c in range(8)],
                      [sq_b, self.cb_const], psb)
        P.op("act", lambda e: e.activation(out=rstd[:], in_=ps[:], func=AF.Sqrt, bias=self.c_eps[:], scale=1.0 / D),
             reads=[psb, self.cb_const], writes=[rstd_b])
        P.op("dve", lambda e: e.reciprocal(out=rstd[:], in_=rstd[:]), reads=[rstd_b], writes=[rstd_b])
        for c in range(8):
            o_ap, o_b = out_fn(c)
            P.op("dve", (lambda e, c=c, o_ap=o_ap: e.scalar_tensor_tensor(
                out=o_ap, in0=xin[:, c, :], scalar=nw[:, c:c + 1], in1=rstd[:],
                op0=ALU.mult, op1=ALU.mult)),
                reads=[xin_b, rstd_b, nw_b], writes=[o_b])

    def load_xcols(self, cb, xin, xin_b, ch, eng="sp"):
        return self.P.op(eng, lambda e: e.dma_start(out=xin[:], in_=self.xTv[:, :, cb * 512:(cb + 1) * 512]),
                         reads=[self.xTb[c][cb] for c in range(8)], writes=[xin_b], dma=True, chan=ch)

    def stage_in(self):
        P, A, S = self.P, self.A, self.S
        x = self.dram_in("x", [S, D])
        m = A.mark()
        rin = Ring(self, 2, [128, D], F32, "s0in")
        rout = Ring(self, 2, [128, 8, 512], F32, "s0out")
        for cb in range(self.NCB):
            xo, xo_b, xo_ch = rout.next()
            for tb in range(4):
                t0 = cb * 512 + tb * 128
                xi, xi_b, xi_ch = rin.next()
                P.op("sp", lambda e, xi=xi, t0=t0: e.dma_start(out=xi[:], in_=x[t0:t0 + 128, :]),
                     writes=[xi_b], dma=True, chan=xi_ch)
                for half in range(2):
                    pi = (tb * 2 + half) % 4
                    ps, psb = self.ps[pi], self.psb[pi]

                    def fn(e, xi=xi, ps=ps, half=half):
                        ins = None
                        for j in range(4):
                            c = half * 4 + j
                            ins = e.transpose(ps[:, j * 128:(j + 1) * 128], xi[:, c * 128:(c + 1) * 128],
                                              self.c_identf[:])
                        return ins
                    P.op("pe", fn, reads=[xi_b, self.cb_const], writes=[psb])
                    eng = "act" if half == 0 else "dve"

                    def cp(e, xo=xo, ps=ps, half=half, tb=tb, eng=eng):
                        o = xo[:, half * 4:(half + 1) * 4, tb * 128:(tb + 1) * 128]
                        i = ps[:].rearrange("p (j t) -> p j t", j=4)
                        if eng == "act":
                            return e.activation(out=o, in_=i, func=AF.Copy)
                        return e.tensor_copy(out=o, in_=i)
                    P.op(eng, cp, reads=[psb], writes=[xo_b])
            P.op("sp", lambda e, xo=xo, cb=cb: e.dma_start(out=self.xTv[:, :, cb * 512:(cb + 1) * 512], in_=xo[:]),
                 reads=[xo_b], writes=[self.xTb[c][cb] for c in range(8)], dma=True, chan=xo_ch)
        P.barrier()
        A.reset(m)

    def stage_out(self):
        P, A, S = self.P, self.A, self.S
        out = self.nc.dram_tensor("out", [S, D], F32, kind="ExternalOutput").ap()
        nw_d = self.dram_in("final_norm", [128, 8])
        m = A.mark()
        nw = A.alloc([128, 8], F32, "nw")
        nw_b = P.buf("nw")
        P.op("sp", lambda e: e.dma_start(out=nw[:], in_=nw_d), writes=[nw_b], dma=True, chan=P.chan("nwf"))
        rin = Ring(self, 2, [128, 8, 512], F32, "fin")
        sq = A.alloc([128, 8, 512], BF16, "fsq")
        sq_b = P.buf("fsq")
        rstd = A.alloc([128, 512], F32, "frstd")
        rstd_b = P.buf("frstd")
        xnf = A.alloc([128, 8, 512], F32, "fxn")
        xnf_b = [P.buf(f"fxn{c}") for c in range(8)]
        rout = Ring(self, 2, [128, D], F32, "fout")
        for cb in range(self.NCB):
            xin, xin_b, xin_ch = rin.next()
            self.load_xcols(cb, xin, xin_b, xin_ch)
            self.norm_block(cb, xin, xin_b, sq, sq_b, rstd, rstd_b, nw, nw_b, 5,
                            lambda c: (xnf[:, c, :], xnf_b[c]))
            for tb in range(4):
                yo, yo_b, yo_ch = rout.next()
                for half in range(2):
                    pi = (tb * 2 + half) % 4
                    ps, psb = self.ps[pi], self.psb[pi]

                    def fn(e, ps=ps, half=half, tb=tb):
                        ins = None
                        for j in range(4):
                            c = half * 4 + j
                            ins = e.transpose(ps[:, j * 128:(j + 1) * 128],
                                              xnf[:, c, tb * 128:(tb + 1) * 128], self.c_identf[:])
                        return ins
                    P.op("pe", fn, reads=[xnf_b[half * 4 + j] for j in range(4)] + [self.cb_const], writes=[psb])
                    eng = "act" if half == 0 else "dve"

                    def cp(e, yo=yo, ps=ps, half=half, eng=eng):
                        o = yo[:, half * 512:(half + 1) * 512]
                        if eng == "act":
                            return e.activation(out=o, in_=ps[:], func=AF.Copy)
                        return e.tensor_copy(out=o, in_=ps[:])
                    P.op(eng, cp, reads=[psb], writes=[yo_b])
                t0 = cb * 512 + tb * 128
                o = P.op("sp", lambda e, yo=yo, t0=t0: e.dma_start(out=out[t0:t0 + 128, :], in_=yo[:]),
                         reads=[yo_b], dma=True, chan=yo_ch)
                o.final = True
        A.reset(m)

    def ffn(self, pre):
        P, A, S, NCB = self.P, self.A, self.S, self.NCB
        nw_d = self.dram_in(pre + "_norm", [128, 8])
        wgu_d = self.dram_in(pre + "_w_gu", [NFC, 128, 2 * 8 * 128])
        wd_d = self.dram_in(pre + "_w_down", [2, 8, 128, 11 * 128])
        m0 = A.mark()
        nw = A.alloc([128, 8], F32, "nw")
        nw_b = P.buf("nw")
        P.op("sp", lambda e: e.dma_start(out=nw[:], in_=nw_d), writes=[nw_b], dma=True, chan=P.chan("nw"))
        xn = A.alloc([128, 8, S], BF16, "xn")
        xn_b = [P.buf(f"xn{cb}") for cb in range(NCB)]
        m1 = A.mark()
        rin = Ring(self, 2, [128, 8, 512], F32, "xin")
        sq = A.alloc([128, 8, 512], BF16, "sq")
        sq_b = P.buf("sq")
        rstd = A.alloc([128, 512], F32, "rstd")
        rstd_b = P.buf("rstd")
        for cb in range(NCB):
            xin, xin_b, xin_ch = rin.next()
            self.load_xcols(cb, xin, xin_b, xin_ch)
            self.norm_block(cb, xin, xin_b, sq, sq_b, rstd, rstd_b, nw, nw_b, 5,
                            lambda c, cb=cb: (xn[:, c, cb * 512:(cb + 1) * 512], xn_b[cb]))
        P.barrier()
        A.reset(m1)
        if DEBUG_STOP == 1:
            A.reset(m0)
            return
        h = A.alloc([128, 11, S], BF16, "h")
        h_b = [[P.buf(f"h{mi}_{cb}") for cb in range(NCB)] for mi in range(11)]
        rw = Ring(self, 3, [128, 2, 8, 128], BF16, "wgu", eng="pool")
        rsg = Ring(self, 2, [128, 512], F32, "sg", chan=False)
        rwd = Ring(self, 2, [128, 11, 128], BF16, "wd", eng="pool")
        rxr = Ring(self, 3, [128, 512], F32, "xres")
        rxo = Ring(self, 3, [128, 512], F32, "xo")
        pgu = 0
        pdn = 0
        for grp in range(2):
            for mi in range(11):
                mchunk = grp * 11 + mi
                w, w_b, w_ch = rw.next()
                P.op("pool", lambda e, w=w, mchunk=mchunk: e.dma_start(
                    out=w[:].rearrange("p a k j -> p (a k j)"), in_=wgu_d[mchunk]),
                    writes=[w_b], dma=True, chan=w_ch)
                for cb in range(NCB):
                    pa, pb = (0, 1) if pgu % 2 == 0 else (2, 3)
                    pgu += 1
                    cols = slice(cb * 512, (cb + 1) * 512)
                    self.mm_group(self.ps[pa][:], [(w[:, 0, kc, :], xn[:, kc, cols]) for kc in range(8)],
                                  [w_b, xn_b[cb]], self.psb[pa])
                    self.mm_group(self.ps[pb][:], [(w[:, 1, kc, :], xn[:, kc, cols]) for kc in range(8)],
                                  [w_b, xn_b[cb]], self.psb[pb])
                    sg, sg_b, _ = rsg.next()
                    P.op("act", lambda e, sg=sg, pa=pa: e.activation(out=sg[:], in_=self.ps[pa][:], func=AF.Silu),
                         reads=[self.psb[pa]], writes=[sg_b])
                    P.op("dve", lambda e, sg=sg, pb=pb, mi=mi, cols=cols: e.tensor_tensor(
                        out=h[:, mi, cols], in0=sg[:], in1=self.ps[pb][:], op=ALU.mult),
                        reads=[sg_b, self.psb[pb]], writes=[h_b[mi][cb]])
            if DEBUG_STOP == 2:
                continue
            for o in range(8):
                wd, wd_b, wd_ch = rwd.next()
                P.op("pool", lambda e, wd=wd, grp=grp, o=o: e.dma_start(
                    out=wd[:].rearrange("p f j -> p (f j)"), in_=wd_d[grp, o]),
                    writes=[wd_b], dma=True, chan=wd_ch)
                for cb in range(NCB):
                    cols = slice(cb * 512, (cb + 1) * 512)
                    xr, xr_b, xr_ch = rxr.next()
                    P.op("sp", lambda e, xr=xr, o=o, cols=cols: e.dma_start(
                        out=xr[:], in_=self.xT[o * 128:(o + 1) * 128, cols]),
                        reads=[self.xTb[o][cb]], writes=[xr_b], dma=True, chan=xr_ch)
                    pi = 4 + (pdn % 2)
                    pdn += 1
                    self.mm_group(self.ps[pi][:], [(wd[:, fc, :], h[:, fc, cols]) for fc in range(11)],
                                  [wd_b] + [h_b[fc][cb] for fc in range(11)], self.psb[pi])
                    xo, xo_b, xo_ch = rxo.next()
                    P.op("dve", lambda e, xo=xo, xr=xr, pi=pi: e.scalar_tensor_tensor(
                        out=xo[:], in0=self.ps[pi][:], scalar=0.5, in1=xr[:], op0=ALU.mult, op1=ALU.add),
                        reads=[self.psb[pi], xr_b], writes=[xo_b])
                    if DEBUG_STOP == 3:
                        continue
                    P.op("sp", lambda e, xo=xo, o=o, cols=cols: e.dma_start(
                        out=self.xT[o * 128:(o + 1) * 128, cols], in_=xo[:]),
                        reads=[xo_b], writes=[self.xTb[o][cb]], dma=True, chan=xo_ch)
        P.barrier()
        A.reset(m0)


def lay_vec8(v):
    return np.ascontiguousarray(np.asarray(v, np.float32).reshape(8, 128).T)


def lay_wgu(w):
    w = np.asarray(w, np.float32)
    g = w[:, :DFF].reshape(8, 128, NFC, 128)
    u = w[:, DFF:].reshape(8, 128, NFC, 128)
    a = np.stack([g, u], axis=0)
    a = a.transpose(3, 2, 0, 1, 4)
    return np.ascontiguousarray(a).reshape(NFC, 128, 2 * 8 * 128)


def lay_wdown(w):
    w = np.asarray(w, np.float32)
    a = w.reshape(2, 11, 128, 8, 128)
    a = a.transpose(0, 3, 2, 1, 4)
    return np.ascontiguousarray(a).reshape(2, 8, 128, 11 * 128)


def build(S, plan):
    k = K(S)
    k.load_consts()
    k.stage_in()
    for item in plan:
        if item[0] == "ffn":
            k.ffn(item[1])
        elif item[0] == "mla":
            k.mla(item[1])
        elif item[0] == "gla":
            k.gla(item[1])
        elif item[0] == "ssd":
            k.ssd(item[1])
        else:
            raise ValueError(item)
    k.stage_out()
    k.stats = k.P.emit()
    return k


PLAN = [
    ("ffn", "l0_ffn1"), ("gla", "l0"), ("ffn", "l0_ffn2"),
    ("ffn", "l1_ffn1"), ("ssd", "l1"), ("ffn", "l1_ffn2"),
    ("ffn", "l2_ffn1"), ("mla", "l2"), ("ffn", "l2_ffn2"),
    ("ffn", "l3_ffn1"), ("gla", "l3"), ("ffn", "l3_ffn2"),
]


def kernel(**inputs):
    S = 4096
    k = build(S, PLAN)
    shared = {}
    in_maps = []
    x = np.asarray(inputs["x"], np.float32)
    pos = np.asarray(inputs["positions"], np.int32)
    for b in range(8):
        m = {}
        for name in k.inputs:
            if name == "x":
                m[name] = np.ascontiguousarray(x[b])
            elif name == "positions64":
                m[name] = np.ascontiguousarray(np.broadcast_to(pos[b][None, :], (64, S)))
            else:
                if name not in shared:
                    shared[name] = host_inputs_sub(k, inputs, name)
                m[name] = shared[name]
        in_maps.append(m)
    res = run_bass_kernel_spmd(k.nc, in_maps, core_ids=list(range(8)))
    return np.stack([np.asarray(r["out"], np.float32) for r in res.results], axis=0)


def host_inputs_sub(k, inputs, name):
    if name == "c_ident":
        return np.eye(128, dtype=np.float32)
    c = const_tables(name)
    if c is not None:
        return c
    c = gla_consts(name)
    if c is not None:
        return c
    c = ssd_consts(name)
    if c is not None:
        return c
    if "_ssd_" in name or name == "l1_mix_norm":
        return ssd_host(inputs, name[:2], name)
    if "_mla_" in name or name == "l2_mix_norm":
        return mla_host(inputs, name[:2], name, k.S)
    if "_gla_" in name or name in ("l0_mix_norm", "l3_mix_norm"):
        return gla_host(inputs, name[:2], name)
    if name.endswith("_w_gu"):
        return lay_wgu(inputs[name])
    if name.endswith("_w_down"):
        return lay_wdown(inputs[name])
    if name.endswith("ffn1_norm") or name.endswith("ffn2_norm") or name == "final_norm":
        return lay_vec8(inputs[name])
    raise KeyError(name)


MLA_SC = float(192 ** -0.5)
NEG = -1.0e30


def _mla(self, pre):
    P, A, S, NCB = self.P, self.A, self.S, self.NCB
    NB = S // 128
    nc = self.nc
    nw_d = self.dram_in(pre + "_mix_norm", [128, 8])
    win_d = self.dram_in(pre + "_mla_w_in", [128, 8 * 768])
    qnw_d = self.dram_in(pre + "_mla_q_norm", [128, 3])
    kvnw_d = self.dram_in(pre + "_mla_kv_norm", [128, 2])
    wuq_d = self.dram_in(pre + "_mla_w_uq", [128, 3 * 8 * 256])
    wk_d = self.dram_in(pre + "_mla_w_uk", [128, 2 * 1024])
    wv_d = self.dram_in(pre + "_mla_w_uv", [128, 2 * 1024])
    wo_d = self.dram_in(pre + "_mla_w_out", [128, 8 * 1024])
    pos_d = self.dram_in("positions64", [64, S], I32)
    rt_d = self.dram_in("c_rope", [64, 4])
    mask_d = self.dram_in("c_mask", [128, 4 * 512])
    Qn = nc.dram_tensor("mla_qn", [8, 128, S], BF16, kind="Internal").ap()
    Qr = nc.dram_tensor("mla_qr", [8, 64, S], BF16, kind="Internal").ap()
    Kn = nc.dram_tensor("mla_kn", [8, 128, S], BF16, kind="Internal").ap()
    Kr = nc.dram_tensor("mla_kr", [64, S], BF16, kind="Internal").ap()
    V = nc.dram_tensor("mla_v", [8, 128, NB, 128], BF16, kind="Internal").ap()
    m0 = A.mark()

    def ld(dst, src, eng="sp", name="w"):
        b = P.buf(name)
        P.op(eng, lambda e: e.dma_start(out=dst, in_=src), writes=[b], dma=True, chan=P.chan(name, eng))
        return b

    nw = A.alloc([128, 8], F32, "nw")
    nw_b = ld(nw[:], nw_d, name="nw")
    qnw = A.alloc([128, 3], F32, "qnw")
    qnw_b = ld(qnw[:], qnw_d, name="qnw")
    kvnw = A.alloc([128, 2], F32, "kvnw")
    kvnw_b = ld(kvnw[:], kvnw_d, name="kvnw")
    rt = A.alloc([64, 4], F32, "rt")
    rt_b = ld(rt[:], rt_d, name="rt")
    m1 = A.mark()
    win = A.alloc([128, 8, 768], BF16, "win")
    win_b = ld(win[:].rearrange("p k n -> p (k n)"), win_d, "pool", "win")
    wuq = A.alloc([128, 3, 8, 256], BF16, "wuq")
    wuq_b = ld(wuq[:].rearrange("p k h n -> p (k h n)"), wuq_d, "pool", "wuq")
    wk = A.alloc([128, 2, 1024], BF16, "wk")
    wk_b = ld(wk[:].rearrange("p k n -> p (k n)"), wk_d, "pool", "wk")
    wv = A.alloc([128, 2, 1024], BF16, "wv")
    wv_b = ld(wv[:].rearrange("p k n -> p (k n)"), wv_d, "pool", "wv")
    rin = Ring(self, 2, [128, 8, 512], F32, "xin")
    sq = A.alloc([128, 8, 512], BF16, "sq")
    sq_b = P.buf("sq")
    rstd = A.alloc([128, 512], F32, "rstd")
    rstd_b = P.buf("rstd")
    xn = A.alloc([128, 8, 512], BF16, "xn")
    xn_b = P.buf("xn")
    latf = A.alloc([128, 3, 512], F32, "latf")
    latf_b = [P.buf(f"latf{c}") for c in range(3)]
    latsq = A.alloc([128, 3, 512], BF16, "latsq")
    latsq_b = [P.buf(f"latsq{c}") for c in range(3)]
    lrstd = A.alloc([128, 512], F32, "lrstd")
    lrstd_b = P.buf("lrstd")
    cqn = A.alloc([128, 3, 512], BF16, "cqn")
    cqn_b = P.buf("cqn")
    ckvn = A.alloc([128, 2, 512], BF16, "ckvn")
    ckvn_b = P.buf("ckvn")
    posi = A.alloc([64, 512], I32, "posi")
    posi_b = P.buf("posi")
    posi_ch = P.chan("posi")
    ang = A.alloc([64, 512], F32, "ang")
    ang_b = P.buf("ang")
    kf = A.alloc([64, 512], F32, "kf")
    ki = A.alloc([64, 512], I32, "ki")
    kf_b = P.buf("kf")
    arg = A.alloc([64, 512], F32, "arg")
    arg_b = P.buf("arg")
    cos2 = A.alloc([64, 512], F32, "cos2")
    cos2_b = P.buf("cos2")
    sin2 = A.alloc([64, 512], F32, "sin2")
    sin2_b = P.buf("sin2")
    t1 = A.alloc([64, 512], F32, "t1")
    t1_b = P.buf("t1")
    t2 = A.alloc([64, 512], F32, "t2")
    t2_b = P.buf("t2")
    rkr = Ring(self, 2, [64, 512], BF16, "krst")
    rqn = Ring(self, 2, [128, 8, 512], BF16, "qnst")
    rqr = Ring(self, 2, [64, 8, 512], BF16, "qrst")
    rkn = Ring(self, 2, [128, 8, 512], BF16, "knst")
    rvs = Ring(self, 2, [128, 1024], BF16, "vst")
    TWO_PI = float(2 * np.pi)
    bank = [0]

    def nb():
        bank[0] = (bank[0] + 1) % 4
        return bank[0]

    def range_reduce(shift):
        P.op("dve", lambda e: e.tensor_scalar(arg[:], ang[:], float(shift), None, ALU.add),
             reads=[ang_b], writes=[arg_b])
        P.op("dve", lambda e: e.tensor_scalar(kf[:], arg[:], float(1.0 / TWO_PI), None, ALU.mult),
             reads=[arg_b], writes=[kf_b])
        P.op("dve", lambda e: e.tensor_copy(out=ki[:], in_=kf[:]), reads=[kf_b], writes=[kf_b])
        P.op("dve", lambda e: e.tensor_copy(out=kf[:], in_=ki[:]), reads=[kf_b], writes=[kf_b])
        P.op("dve", lambda e: e.scalar_tensor_tensor(out=arg[:], in0=kf[:], scalar=-TWO_PI, in1=arg[:],
                                                     op0=ALU.mult, op1=ALU.add),
             reads=[arg_b, kf_b], writes=[arg_b])
        P.op("dve", lambda e: e.tensor_scalar(kf[:], arg[:], float(np.pi), -TWO_PI, ALU.is_gt, ALU.mult),
             reads=[arg_b], writes=[kf_b])
        P.op("dve", lambda e: e.tensor_tensor(out=arg[:], in0=arg[:], in1=kf[:], op=ALU.add),
             reads=[arg_b, kf_b], writes=[arg_b])
        P.op("dve", lambda e: e.tensor_scalar(kf[:], arg[:], float(-np.pi), TWO_PI, ALU.is_lt, ALU.mult),
             reads=[arg_b], writes=[kf_b])
        P.op("dve", lambda e: e.tensor_tensor(out=arg[:], in0=arg[:], in1=kf[:], op=ALU.add),
             reads=[arg_b, kf_b], writes=[arg_b])

    def lat_norm(nch, col0, nwt, nwt_b, dst, dst_b, dim):
        for c in range(nch):
            bk = nb()
            self.mm_group(self.ps[bk][:], [(win[:, kc, col0 + c * 128:col0 + (c + 1) * 128], xn[:, kc, :])
                                           for kc in range(8)], [win_b, xn_b], self.psb[bk])
            P.op("act", lambda e, c=c, bk=bk: e.activation(out=latf[:, c, :], in_=self.ps[bk][:], func=AF.Copy),
                 reads=[self.psb[bk]], writes=[latf_b[c]])
            P.op("act", lambda e, c=c, bk=bk: e.activation(out=latsq[:, c, :], in_=self.ps[bk][:], func=AF.Square),
                 reads=[self.psb[bk]], writes=[latsq_b[c]])
        self.mm_group(self.ps[4][:], [(self.c_onesb[:], latsq[:, c, :]) for c in range(nch)],
                      [latsq_b[c] for c in range(nch)] + [self.cb_const], self.psb[4])
        P.op("act", lambda e: e.activation(out=lrstd[:], in_=self.ps[4][:], func=AF.Sqrt, bias=self.c_eps[:],
                                           scale=1.0 / dim), reads=[self.psb[4], self.cb_const], writes=[lrstd_b])
        P.op("dve", lambda e: e.reciprocal(out=lrstd[:], in_=lrstd[:]), reads=[lrstd_b], writes=[lrstd_b])
        for c in range(nch):
            P.op("dve", lambda e, c=c: e.scalar_tensor_tensor(out=dst[:, c, :], in0=latf[:, c, :],
                                                             scalar=nwt[:, c:c + 1], in1=lrstd[:],
                                                             op0=ALU.mult, op1=ALU.mult),
                 reads=[latf_b[c], lrstd_b, nwt_b], writes=[dst_b])

    def rope_combine(bk_a, bk_b, scale, out_ap, out_b):
        P.op("dve", lambda e: e.scalar_tensor_tensor(out=t1[:], in0=self.ps[bk_a][0:64, :], scalar=float(scale),
                                                     in1=cos2[:], op0=ALU.mult, op1=ALU.mult),
             reads=[self.psb[bk_a], cos2_b], writes=[t1_b])
        P.op("dve", lambda e: e.scalar_tensor_tensor(out=t2[:], in0=self.ps[bk_b][0:64, :], scalar=float(scale),
                                                     in1=sin2[:], op0=ALU.mult, op1=ALU.mult),
             reads=[self.psb[bk_b], sin2_b], writes=[t2_b])
        P.op("dve", lambda e: e.tensor_tensor(out=out_ap, in0=t1[:], in1=t2[:], op=ALU.add),
             reads=[t1_b, t2_b], writes=[out_b])

    for cb in range(NCB):
        cols = slice(cb * 512, (cb + 1) * 512)
        xin, xin_b, xin_ch = rin.next()
        self.load_xcols(cb, xin, xin_b, xin_ch)
        self.norm_block(cb, xin, xin_b, sq, sq_b, rstd, rstd_b, nw, nw_b, 5, lambda c: (xn[:, c, :], xn_b))
        lat_norm(3, 0, qnw, qnw_b, cqn, cqn_b, 384)
        lat_norm(2, 384, kvnw, kvnw_b, ckvn, ckvn_b, 256)
        P.op("sp", lambda e, cols=cols: e.dma_start(out=posi[:], in_=pos_d[:, cols]), writes=[posi_b], dma=True,
             chan=posi_ch)
        P.op("dve", lambda e: e.tensor_copy(out=ang[:], in_=posi[:]), reads=[posi_b], writes=[ang_b])
        P.op("dve", lambda e: e.tensor_scalar(ang[:], ang[:], rt[:, 0:1], None, ALU.mult),
             reads=[ang_b, rt_b], writes=[ang_b])
        range_reduce(np.pi / 2)
        P.op("act", lambda e: e.activation(out=cos2[:], in_=arg[:], func=AF.Sin), reads=[arg_b], writes=[cos2_b])
        range_reduce(0.0)
        P.op("act", lambda e: e.activation(out=sin2[:], in_=arg[:], func=AF.Sin, scale=rt[:, 1:2]),
             reads=[arg_b, rt_b], writes=[sin2_b])
        ba, bb = nb(), nb()
        self.mm_group(self.ps[ba][0:64, :], [(win[:, kc, 640:704], xn[:, kc, :]) for kc in range(8)],
                      [win_b, xn_b], self.psb[ba])
        self.mm_group(self.ps[bb][0:64, :], [(win[:, kc, 704:768], xn[:, kc, :]) for kc in range(8)],
                      [win_b, xn_b], self.psb[bb])
        krs, krs_b, krs_ch = rkr.next()
        rope_combine(ba, bb, 1.0, krs[:], krs_b)
        P.op("sp", lambda e, krs=krs, cols=cols: e.dma_start(out=Kr[:, cols], in_=krs[:]), reads=[krs_b],
             dma=True, chan=krs_ch)
        qns, qns_b, qns_ch = rqn.next()
        qrs, qrs_b, qrs_ch = rqr.next()
        kns, kns_b, kns_ch = rkn.next()
        for h in range(8):
            bk = nb()
            self.mm_group(self.ps[bk][:], [(wuq[:, kc, h, 0:128], cqn[:, kc, :]) for kc in range(3)],
                          [wuq_b, cqn_b], self.psb[bk])
            P.op("act", lambda e, h=h, bk=bk, qns=qns: e.activation(out=qns[:, h, :], in_=self.ps[bk][:],
                                                                  func=AF.Copy, scale=MLA_SC),
                 reads=[self.psb[bk]], writes=[qns_b])
            ba, bb = nb(), nb()
            self.mm_group(self.ps[ba][0:64, :], [(wuq[:, kc, h, 128:192], cqn[:, kc, :]) for kc in range(3)],
                          [wuq_b, cqn_b], self.psb[ba])
            self.mm_group(self.ps[bb][0:64, :], [(wuq[:, kc, h, 192:256], cqn[:, kc, :]) for kc in range(3)],
                          [wuq_b, cqn_b], self.psb[bb])
            rope_combine(ba, bb, MLA_SC, qrs[:, h, :], qrs_b)
            bk = nb()
            self.mm_group(self.ps[bk][:], [(wk[:, kc, h * 128:(h + 1) * 128], ckvn[:, kc, :]) for kc in range(2)],
                          [wk_b, ckvn_b], self.psb[bk])
            P.op("act", lambda e, h=h, bk=bk, kns=kns: e.activation(out=kns[:, h, :], in_=self.ps[bk][:],
                                                                  func=AF.Copy),
                 reads=[self.psb[bk]], writes=[kns_b])
        P.op("sp", lambda e, qns=qns, cols=cols: e.dma_start(out=Qn.rearrange("h p s -> p h s")[:, :, cols],
                                                            in_=qns[:]), reads=[qns_b], dma=True, chan=qns_ch)
        P.op("sp", lambda e, qrs=qrs, cols=cols: e.dma_start(out=Qr.rearrange("h p s -> p h s")[:, :, cols],
                                                            in_=qrs[:]), reads=[qrs_b], dma=True, chan=qrs_ch)
        P.op("sp", lambda e, kns=kns, cols=cols: e.dma_start(out=Kn.rearrange("h p s -> p h s")[:, :, cols],
                                                            in_=kns[:]), reads=[kns_b], dma=True, chan=kns_ch)
        for tb in range(4):
            vs, vs_b, vs_ch = rvs.next()
            for half in range(2):
                bk = nb()
                self.mm_group(self.ps[bk][:], [(ckvn[:, kc, tb * 128:(tb + 1) * 128],
                                                wv[:, kc, half * 512:(half + 1) * 512]) for kc in range(2)],
                              [wv_b, ckvn_b], self.psb[bk])
                if half == 0:
                    P.op("act", lambda e, vs=vs, bk=bk: e.activation(out=vs[:, 0:512], in_=self.ps[bk][:],
                                                                   func=AF.Copy),
                         reads=[self.psb[bk]], writes=[vs_b])
                else:
                    P.op("dve", lambda e, vs=vs, bk=bk: e.tensor_copy(out=vs[:, 512:1024], in_=self.ps[bk][:]),
                         reads=[self.psb[bk]], writes=[vs_b])
            blk = cb * 4 + tb
            P.op("sp", lambda e, vs=vs, blk=blk: e.dma_start(
                out=V.rearrange("h p b v -> p h b v")[:, :, blk, :], in_=vs[:].rearrange("p (h v) -> p h v", h=8)),
                reads=[vs_b], dma=True, chan=vs_ch)
    P.barrier()
    A.reset(m1)

    wo = A.alloc([128, 8, 1024], BF16, "wo")
    wo_b = ld(wo[:].rearrange("p h n -> p (h n)"), wo_d, "pool", "wo")
    mask = A.alloc([128, 4, 512], F32, "mask")
    mask_b = ld(mask[:].rearrange("p t n -> p (t n)"), mask_d, "sp", "mask")
    kr = A.alloc([64, S], BF16, "kr")
    kr_b = ld(kr[:], Kr, "sp", "kr")
    rK = Ring(self, 3, [128, S], BF16, "kh")
    rV = Ring(self, 3, [128, NB, 128], BF16, "vh")
    rQn = Ring(self, 2, [128, 8, 512], BF16, "qn")
    rQr = Ring(self, 2, [64, 8, 512], BF16, "qr")
    NPM = 1
    Pms = [A.alloc([128, 4, S], BF16, "Pm") for _ in range(NPM)]
    Pms_b = [[P.buf(f"Pm{i}{t}") for t in range(4)] for i in range(NPM)]
    rPT = Ring(self, 3, [128, 1024], BF16, "PT", chan=False)
    oT = A.alloc([128, 8, 512], BF16, "oT")
    oT_b = [P.buf(f"oT{h}") for h in range(8)]
    mxs = [A.alloc([128, 4, 8], F32, "mx") for _ in range(2)]
    mxs_b = [[[P.buf(f"mx{i}{t}{c}") for c in range(8)] for t in range(4)] for i in range(2)]
    negms = [A.alloc([128, 4], F32, "negm") for _ in range(2)]
    negms_b = [P.buf(f"negm{i}") for i in range(2)]
    rss = [A.alloc([128, 4, 8], F32, "rs") for _ in range(2)]
    rss_b = [[[P.buf(f"rs{i}{t}{c}") for c in range(8)] for t in range(4)] for i in range(2)]
    rsums = [A.alloc([128, 4], F32, "rsum") for _ in range(2)]
    rsums_b = [P.buf(f"rsum{i}") for i in range(2)]
    diag = A.alloc([128, 4, 128], F32, "diag")
    diag_b = P.buf("diag")
    onesf = A.alloc([128, 128], F32, "onesf")
    onesf_b = P.buf("onesf")
    P.op("dve", lambda e: e.memset(onesf[:], 1.0), writes=[onesf_b])
    rbs = A.alloc([128, 512], F32, "rbs")
    rbs_b = P.buf("rbs")
    rdt = Ring(self, 2, [128, 512], F32, "dtmp", chan=False)
    rxr = Ring(self, 2, [128, 512], F32, "xres")
    rxo = Ring(self, 2, [128, 512], F32, "xo")
    cp_i = [0]
    qbufs = {}

    class Item:
        pass

    items = []
    for qb in range(NCB):
        for h in range(8):
            it = Item()
            it.qb, it.h, it.idx = qb, h, len(items)
            it.nk = (qb + 1) * 512
            it.nch = qb + 1
            it.nkb = it.nk // 128
            items.append(it)

    def start_item(it):
        qb, h = it.qb, it.h
        cols = slice(qb * 512, (qb + 1) * 512)
        if h == 0:
            qn, qn_b, qn_ch = rQn.next()
            qr, qr_b, qr_ch = rQr.next()
            P.op("sp", lambda e: e.dma_start(out=qn[:], in_=Qn.rearrange("h p s -> p h s")[:, :, cols]),
                 writes=[qn_b], dma=True, chan=qn_ch)
            P.op("sp", lambda e: e.dma_start(out=qr[:], in_=Qr.rearrange("h p s -> p h s")[:, :, cols]),
                 writes=[qr_b], dma=True, chan=qr_ch)
            qbufs[qb] = (qn, qn_b, qr, qr_b)
        it.qn, it.qn_b, it.qr, it.qr_b = qbufs[qb]
        it.kh, it.kh_b, kh_ch = rK.next()
        it.vh, it.vh_b, vh_ch = rV.next()
        nk, nkb = it.nk, it.nkb
        P.op("sp", lambda e: e.dma_start(out=it.kh[:, 0:nk], in_=Kn[h][:, 0:nk]), writes=[it.kh_b], dma=True,
             chan=kh_ch)
        P.op("sp", lambda e: e.dma_start(out=it.vh[:, 0:nkb, :], in_=V[h][:, 0:nkb, :]), writes=[it.vh_b], dma=True,
             chan=vh_ch)
        it.par = it.idx % 2
        it.pm = it.idx % NPM

    def qk(it, t, kc, bk):
        tc_ = slice(t * 128, (t + 1) * 128)
        kcs = slice(kc * 512, (kc + 1) * 512)
        self.mm_group(self.ps[bk][:], [(it.qn[:, it.h, tc_], it.kh[:, kcs]), (it.qr[:, it.h, tc_], kr[:, kcs])],
                      [it.qn_b, it.qr_b, it.kh_b, kr_b], self.psb[bk])

    def s1_chunk(it, t, kc):
        mx, mx_b = mxs[it.par], mxs_b[it.par]
        bk = nb()
        qk(it, t, kc, bk)
        P.op("dve", lambda e: e.tensor_reduce(out=mx[:, t, kc:kc + 1], in_=self.ps[bk][:], axis=AX.X, op=ALU.max),
             reads=[self.psb[bk]], writes=[mx_b[t][kc]])

    def s1_end(it):
        mx, mx_b, negm, negm_b = mxs[it.par], mxs_b[it.par], negms[it.par], negms_b[it.par]
        rs, rs_b = rss[it.par], rss_b[it.par]
        nch = it.nch
        P.op("dve", lambda e: e.tensor_reduce(out=negm[:], in_=mx[:, :, 0:nch], axis=AX.X, op=ALU.max),
             reads=[mx_b[t][c] for t in range(4) for c in range(nch)], writes=[negm_b])
        P.op("dve", lambda e: e.tensor_scalar(negm[:], negm[:], -1.0, None, ALU.mult), reads=[negm_b], writes=[negm_b])
        P.op("dve", lambda e: e.memset(rs[:], 0.0), writes=[rs_b[t][c] for t in range(4) for c in range(8)])

    def s2_chunk(it, t, kc):
        negm, negm_b = negms[it.par], negms_b[it.par]
        rs, rs_b = rss[it.par], rss_b[it.par]
        Pm, Pm_b = Pms[it.pm], Pms_b[it.pm]
        bk = nb()
        qk(it, t, kc, bk)
        kcs = slice(kc * 512, (kc + 1) * 512)
        if kc == it.nch - 1:
            dt_, dt_b, _ = rdt.next()
            P.op("dve", lambda e: e.tensor_tensor(out=dt_[:], in0=self.ps[bk][:], in1=mask[:, t, :], op=ALU.add),
                 reads=[self.psb[bk], mask_b], writes=[dt_b])
            P.op("act", lambda e: e.activation(out=Pm[:, t, kcs], in_=dt_[:], func=AF.Exp, bias=negm[:, t:t + 1],
                                               scale=1.0, accum_out=rs[:, t, kc:kc + 1]),
                 reads=[dt_b, negm_b], writes=[Pm_b[t], rs_b[t][kc]])
        else:
            P.op("act", lambda e: e.activation(out=Pm[:, t, kcs], in_=self.ps[bk][:], func=AF.Exp,
                                               bias=negm[:, t:t + 1], scale=1.0, accum_out=rs[:, t, kc:kc + 1]),
                 reads=[self.psb[bk], negm_b], writes=[Pm_b[t], rs_b[t][kc]])

    def s2_end(it):
        rs, rs_b, rsum, rsum_b = rss[it.par], rss_b[it.par], rsums[it.par], rsums_b[it.par]
        nch = it.nch
        P.op("dve", lambda e: e.tensor_reduce(out=rsum[:], in_=rs[:, :, 0:nch], axis=AX.X, op=ALU.add),
             reads=[rs_b[t][c] for t in range(4) for c in range(nch)], writes=[rsum_b])
        P.op("dve", lambda e: e.reciprocal(out=rsum[:], in_=rsum[:]), reads=[rsum_b], writes=[rsum_b])

    def s3(it):
        Pm, Pm_b = Pms[it.pm], Pms_b[it.pm]
        rsum, rsum_b = rsums[it.par], rsums_b[it.par]
        vh, vh_b, h = it.vh, it.vh_b, it.h
        npair = it.nkb // 2
        pend = None

        def emit_tr(jp):
            pst, pst_b = self.pst[jp % 2], self.pstb[jp % 2]

            def tr(e):
                ins = None
                for jl in range(2):
                    for t in range(4):
                        j = jp * 2 + jl
                        ins = e.transpose(pst[:, (jl * 4 + t) * 128:(jl * 4 + t + 1) * 128],
                                          Pm[:, t, j * 128:(j + 1) * 128], self.c_identb[:])
                return ins
            P.op("pe", tr, reads=Pm_b + [self.cb_const], writes=[pst_b])
            pt, pt_b, _ = rPT.next()
            cp_i[0] += 1
            if cp_i[0] % 2 == 0:
                P.op("act", lambda e: e.activation(out=pt[:], in_=pst[:], func=AF.Copy), reads=[pst_b], writes=[pt_b])
            else:
                P.op("dve", lambda e: e.tensor_copy(out=pt[:], in_=pst[:]), reads=[pst_b], writes=[pt_b])
            return pt, pt_b

        def emit_pv(jp, pt, pt_b):
            def pv(e):
                ins = None
                for jl in range(2):
                    j = jp * 2 + jl
                    ins = e.matmul(self.ps[4][:], vh[:, j, :], pt[:, jl * 512:(jl + 1) * 512],
                                   start=(jp == 0 and jl == 0), stop=(jp == npair - 1 and jl == 1))
                return ins
            P.op("pe", pv, reads=[pt_b, vh_b], writes=[self.psb[4]])

        for jp in range(npair):
            cur = emit_tr(jp)
            if pend is not None:
                emit_pv(jp - 1, *pend)
            pend = cur
        emit_pv(npair - 1, *pend)
        for t in range(4):
            P.op("dve", lambda e, t=t: e.tensor_scalar(diag[:, t, :], self.c_identf[:], rsum[:, t:t + 1], None, ALU.mult),
                 reads=[rsum_b, self.cb_const], writes=[diag_b])
        self.mm_group(self.ps[5][:], [(onesf[:], diag[:].rearrange("p t n -> p (t n)"))], [onesf_b, diag_b],
                      self.psb[5])
        P.op("act", lambda e: e.activation(out=rbs[:], in_=self.ps[5][:], func=AF.Copy), reads=[self.psb[5]],
             writes=[rbs_b])
        P.op("dve", lambda e: e.tensor_tensor(out=oT[:, h, :], in0=self.ps[4][:], in1=rbs[:], op=ALU.mult),
             reads=[self.psb[4], rbs_b], writes=[oT_b[h]])
        if h == 7:
            qb = it.qb
            cols = slice(qb * 512, (qb + 1) * 512)
            for o in range(8):
                bk = nb()
                self.mm_group(self.ps[bk][:], [(wo[:, hh, o * 128:(o + 1) * 128], oT[:, hh, :]) for hh in range(8)],
                              [wo_b] + oT_b, self.psb[bk])
                xr, xr_b, xr_ch = rxr.next()
                P.op("sp", lambda e, xr=xr, o=o: e.dma_start(out=xr[:], in_=self.xT[o * 128:(o + 1) * 128, cols]),
                     reads=[self.xTb[o][qb]], writes=[xr_b], dma=True, chan=xr_ch)
                xo, xo_b, xo_ch = rxo.next()
                P.op("dve", lambda e, xo=xo, xr=xr, bk=bk: e.tensor_tensor(out=xo[:], in0=self.ps[bk][:], in1=xr[:],
                                                                         op=ALU.add),
                     reads=[self.psb[bk], xr_b], writes=[xo_b])
                P.op("sp", lambda e, xo=xo, o=o: e.dma_start(out=self.xT[o * 128:(o + 1) * 128, cols], in_=xo[:]),
                     reads=[xo_b], writes=[self.xTb[o][qb]], dma=True, chan=xo_ch)

    n = len(items)
    for i in range(n + 2):
        a = items[i] if i < n else None
        b = items[i - 1] if 0 <= i - 1 < n else None
        c = items[i - 2] if 0 <= i - 2 < n else None
        if a is not None:
            start_item(a)
        if c is not None:
            s3(c)
        la = [(t, kc) for t in range(4) for kc in range(a.nch)] if a is not None else []
        lb = [(t, kc) for t in range(4) for kc in range(b.nch)] if b is not None else []
        for j in range(max(len(la), len(lb))):
            if j < len(la):
                s1_chunk(a, *la[j])
            if j < len(lb):
                s2_chunk(b, *lb[j])
        if a is not None:
            s1_end(a)
        if b is not None:
            s2_end(b)
    P.barrier()
    A.reset(m0)


K.mla = _mla


def lay_cols(w, p=128):
    w = np.asarray(w, np.float32)
    kc = w.shape[0] // p
    return np.ascontiguousarray(w.reshape(kc, p, w.shape[1]).transpose(1, 0, 2)).reshape(p, kc * w.shape[1])


def lay_vecp(v, p=128):
    v = np.asarray(v, np.float32)
    return np.ascontiguousarray(v.reshape(-1, p).T)


def mla_host(inputs, pre, name, S):
    if name == pre + "_mla_w_in":
        w = np.asarray(inputs[name], np.float32)
        kr = w[:, 640:704]
        w2 = np.concatenate([w, kr[:, 32:64], kr[:, 0:32]], axis=1)
        return lay_cols(w2)
    if name == pre + "_mla_w_uq":
        w = np.asarray(inputs[name], np.float32).reshape(384, 8, 192)
        w2 = np.concatenate([w, w[:, :, 160:192], w[:, :, 128:160]], axis=2)
        return lay_cols(w2.reshape(384, 8 * 256))
    if name == pre + "_mla_w_uk":
        w = np.asarray(inputs[pre + "_mla_w_ukv"], np.float32).reshape(256, 8, 256)
        return lay_cols(np.ascontiguousarray(w[:, :, 0:128]).reshape(256, 1024))
    if name == pre + "_mla_w_uv":
        w = np.asarray(inputs[pre + "_mla_w_ukv"], np.float32).reshape(256, 8, 256)
        return lay_cols(np.ascontiguousarray(w[:, :, 128:256]).reshape(256, 1024))
    if name == pre + "_mla_w_out":
        return lay_cols(inputs[name])
    if name in (pre + "_mla_q_norm", pre + "_mla_kv_norm", pre + "_mix_norm"):
        return lay_vecp(inputs[name])
    return None


def const_tables(name):
    if name == "c_rope":
        inv = (1.0 / (10000.0 ** (np.arange(0, 64, 2, dtype=np.float32) / 64))).astype(np.float32)
        inv2 = np.concatenate([inv, inv])
        sgn = np.concatenate([-np.ones(32), np.ones(32)])
        return np.stack([inv2, sgn, -np.pi * sgn, -np.pi * np.ones(64)], 1).astype(np.float32)
    if name == "c_mask":
        q = np.arange(128)[:, None, None]
        t = np.arange(4)[None, :, None]
        c = np.arange(512)[None, None, :]
        m = np.where(c <= t * 128 + q, 0.0, NEG).astype(np.float32)
        return np.ascontiguousarray(m).reshape(128, 4 * 512)
    return None


def _gla(self, pre):
    P, A, S, NCB = self.P, self.A, self.S, self.NCB
    nw_d = self.dram_in(pre + "_mix_norm", [128, 8])
    win_d = self.dram_in(pre + "_gla_w_in", [128, 8 * 3088])
    wgb_d = self.dram_in(pre + "_gla_w_gate_b", [16, 512])
    bg_d = self.dram_in(pre + "_gla_b_gate", [1, 512])
    gnw_d = self.dram_in(pre + "_gla_norm", [128, 1024])
    wo_d = self.dram_in(pre + "_gla_w_out", [128, 8 * 1024])
    cum_d = self.dram_in("c_gla_cum", [128, 3 * 128])
    tri4_d = self.dram_in("c_tri4", [128, 512])
    m0 = A.mark()

    def ld(dst, src, eng="sp", name="w"):
        b = P.buf(name)
        P.op(eng, lambda e: e.dma_start(out=dst, in_=src), writes=[b], dma=True, chan=P.chan(name, eng))
        return b

    nw = A.alloc([128, 8], F32, "nw")
    nw_b = ld(nw[:], nw_d, name="nw")
    win = A.alloc([128, 8, 3088], BF16, "gwin")
    win_b = ld(win[:].rearrange("p k n -> p (k n)"), win_d, "pool", "gwin")
    wo = A.alloc([128, 8, 1024], BF16, "gwo")
    wo_b = ld(wo[:].rearrange("p k n -> p (k n)"), wo_d, "pool", "gwo")
    wgb = A.alloc([16, 512], BF16, "wgb")
    wgb_b = ld(wgb[:], wgb_d, "pool", "wgb")
    bg = A.alloc([1, 512], BF16, "bg")
    bg_b = ld(bg[:], bg_d, "pool", "bg")
    gnw = A.alloc([128, 1024], F32, "gnw")
    gnw_b = ld(gnw[:], gnw_d, "sp", "gnw")
    cum = A.alloc([128, 3, 128], F32, "cum")
    cum_b = ld(cum[:].rearrange("p a n -> p (a n)"), cum_d, "sp", "cum")
    tri4 = A.alloc([128, 4, 128], F32, "tri4")
    tri4_b = ld(tri4[:].rearrange("p a n -> p (a n)"), tri4_d, "sp", "tri4")
    ones1 = A.alloc([1, 128], BF16, "ones1")
    ones1_b = P.buf("ones1")
    P.op("dve", lambda e: e.memset(ones1[:], 1.0), writes=[ones1_b])
    c1 = A.alloc([128, 1], F32, "c1")
    c1_b = P.buf("c1")
    P.op("dve", lambda e: e.memset(c1[:], 1.0), writes=[c1_b])

    rin = Ring(self, 2, [128, 8, 512], F32, "xin")
    sq = A.alloc([128, 8, 512], BF16, "sq")
    sq_b = P.buf("sq")
    rstd = A.alloc([128, 512], F32, "rstd")
    rstd_b = P.buf("rstd")
    xn = A.alloc([128, 8, 512], BF16, "xn")
    xn_b = P.buf("xn")
    qTf = A.alloc([128, 4, 512], F32, "qTf")
    qTf_b = P.buf("qTf")
    kTf = A.alloc([128, 4, 512], F32, "kTf")
    kTf_b = P.buf("kTf")
    alr = A.alloc([16, 512], BF16, "alr")
    alr_b = P.buf("alr")
    r_ktm = Ring(self, 2, [128, 512], F32, "ktm", chan=False)
    r_vsb = Ring(self, 2, [128, 1024], BF16, "vsb", chan=False)
    r_sgw = Ring(self, 2, [128, 1024], F32, "sgw", chan=False)
    r_sp = Ring(self, 2, [128, 512], F32, "sp", chan=False)
    dend = A.alloc([128, 512], F32, "dend")
    dend_b = P.buf("dend")
    kend = A.alloc([128, 512], BF16, "kend")
    kend_b = P.buf("kend")
    eb = A.alloc([128, 4, 128], F32, "eb")
    eb_b = P.buf("eb")
    enb = A.alloc([128, 4, 128], F32, "enb")
    enb_b = P.buf("enb")
    qdec = A.alloc([128, 4, 128], BF16, "qdec")
    qdec_b = P.buf("qdec")
    kinv = A.alloc([128, 4, 128], BF16, "kinv")
    kinv_b = P.buf("kinv")
    attnT = A.alloc([128, 4, 128], BF16, "attnT")
    attnT_b = P.buf("attnT")
    stf = A.alloc([128, 4, 256], F32, "stf")
    stf_b = [P.buf(f"stf{h}") for h in range(4)]
    stb = A.alloc([128, 4, 256], BF16, "stb")
    stb_b = [P.buf(f"stb{h}") for h in range(4)]
    junk = A.alloc([128, 256], F32, "junk")
    junk_b = P.buf("junk")
    ssq = A.alloc([128, 4], F32, "ssq")
    ssq_b = [P.buf(f"ssq{h}") for h in range(4)]
    orstd = A.alloc([128, 4], F32, "orstd")
    orstd_b = P.buf("orstd")
    og = A.alloc([128, 1024], BF16, "og")
    og_b = P.buf("og")
    ogT = A.alloc([128, 8, 512], BF16, "ogT")
    ogT_b = [P.buf(f"ogT{t}") for t in range(4)]
    rxr = Ring(self, 2, [128, 512], F32, "xres")
    rxo = Ring(self, 2, [128, 512], F32, "xo")
    for h in range(4):
        P.op("dve", lambda e, h=h: e.memset(stf[:, h, :], 0.0), writes=[stf_b[h]])
        P.op("pool", lambda e, h=h: e.memset(stb[:, h, :], 0.0), writes=[stb_b[h]])
    bank = [0]

    def nb():
        bank[0] = (bank[0] + 1) % 5
        return bank[0]
    SCQ = float(128 ** -0.5)

    for cb in range(NCB):
        cols = slice(cb * 512, (cb + 1) * 512)
        xin, xin_b, xin_ch = rin.next()
        self.load_xcols(cb, xin, xin_b, xin_ch)
        self.norm_block(cb, xin, xin_b, sq, sq_b, rstd, rstd_b, nw, nw_b, 5, lambda c: (xn[:, c, :], xn_b))
        for h in range(4):
            bk = nb()
            self.mm_group(self.ps[bk][:], [(win[:, kc, h * 128:(h + 1) * 128], xn[:, kc, :]) for kc in range(8)],
                          [win_b, xn_b], self.psb[bk])
            P.op("act", lambda e, h=h, bk=bk: e.activation(out=qTf[:, h, :], in_=self.ps[bk][:], func=AF.Copy,
                                                           scale=SCQ), reads=[self.psb[bk]], writes=[qTf_b])
            bk = nb()
            self.mm_group(self.ps[bk][:], [(win[:, kc, 512 + h * 128:512 + (h + 1) * 128], xn[:, kc, :])
                                           for kc in range(8)], [win_b, xn_b], self.psb[bk])
            P.op("dve", lambda e, h=h, bk=bk: e.tensor_copy(out=kTf[:, h, :], in_=self.ps[bk][:]),
                 reads=[self.psb[bk]], writes=[kTf_b])
        bk = nb()
        self.mm_group(self.ps[bk][0:16, :], [(win[:, kc, 3072:3088], xn[:, kc, :]) for kc in range(8)],
                      [win_b, xn_b], self.psb[bk])
        P.op("act", lambda e, bk=bk: e.activation(out=alr[:], in_=self.ps[bk][0:16, :], func=AF.Copy),
             reads=[self.psb[bk]], writes=[alr_b])
        def partA(tb):
            tcs = slice(tb * 128, (tb + 1) * 128)
            ktm, ktm_b, _ = r_ktm.next()
            vsb, vsb_b, _ = r_vsb.next()
            sgw, sgw_b, _ = r_sgw.next()
            sp_, sp_b, _ = r_sp.next()
            bk = nb()
            self.mm_group(self.ps[bk][:], [(xn[:, kc, tcs], win[:, kc, 512:1024]) for kc in range(8)],
                          [win_b, xn_b], self.psb[bk])
            P.op("act", lambda e, bk=bk: e.activation(out=ktm[:], in_=self.ps[bk][:], func=AF.Copy),
                 reads=[self.psb[bk]], writes=[ktm_b])
            for half in range(2):
                bk = nb()
                self.mm_group(self.ps[bk][:], [(xn[:, kc, tcs], win[:, kc, 1024 + half * 512:1024 + (half + 1) * 512])
                                               for kc in range(8)], [win_b, xn_b], self.psb[bk])
                P.op("dve", lambda e, bk=bk, half=half: e.tensor_copy(out=vsb[:, half * 512:(half + 1) * 512],
                                                                      in_=self.ps[bk][:]),
                     reads=[self.psb[bk]], writes=[vsb_b])
            for half in range(2):
                bk = nb()
                self.mm_group(self.ps[bk][:], [(xn[:, kc, tcs], win[:, kc, 2048 + half * 512:2048 + (half + 1) * 512])
                                               for kc in range(8)], [win_b, xn_b], self.psb[bk])
                hs = slice(half * 512, (half + 1) * 512)
                P.op("act", lambda e, bk=bk, hs=hs: e.activation(out=sgw[:, hs], in_=self.ps[bk][:], func=AF.Silu),
                     reads=[self.psb[bk]], writes=[sgw_b])
                P.op("dve", lambda e, hs=hs: e.tensor_tensor(out=sgw[:, hs], in0=sgw[:, hs], in1=gnw[:, hs],
                                                             op=ALU.mult), reads=[sgw_b, gnw_b], writes=[sgw_b])
            bk = nb()
            self.mm_group(self.ps[bk][:], [(alr[:, tcs], wgb[:]), (ones1[:], bg[:])],
                          [alr_b, wgb_b, ones1_b, bg_b], self.psb[bk])
            P.op("act", lambda e, bk=bk: e.activation(out=sp_[:], in_=self.ps[bk][:], func=AF.Exp, scale=-1.0),
                 reads=[self.psb[bk]], writes=[sp_b])
            P.op("act", lambda e: e.activation(out=sp_[:], in_=sp_[:], func=AF.Ln, bias=c1[:], scale=1.0),
                 reads=[sp_b, c1_b], writes=[sp_b])
            return (ktm, ktm_b, vsb, vsb_b, sgw, sgw_b, sp_, sp_b)

        def partB(tb, bufs):
            tcs = slice(tb * 128, (tb + 1) * 128)
            ktm, ktm_b, vsb, vsb_b, sgw, sgw_b, sp_, sp_b = bufs
            bk = nb()
            self.mm_group(self.ps[bk][:], [(cum[:, 0, :], sp_[:])], [cum_b, sp_b], self.psb[bk])
            P.op("act", lambda e, bk=bk: e.activation(out=dend[:], in_=self.ps[bk][:], func=AF.Exp),
                 reads=[self.psb[bk]], writes=[dend_b])
            P.op("dve", lambda e: e.tensor_tensor(out=kend[:], in0=ktm[:], in1=dend[:], op=ALU.mult),
                 reads=[ktm_b, dend_b], writes=[kend_b])
            bk = nb()

            def cumT(e, bk=bk):
                ins = None
                for h in range(4):
                    ins = e.matmul(self.ps[bk][:, h * 128:(h + 1) * 128], sp_[:, h * 128:(h + 1) * 128],
                                   cum[:, 1, :], start=True, stop=True)
                return ins
            P.op("pe", cumT, reads=[sp_b, cum_b], writes=[self.psb[bk]])
            P.op("act", lambda e, bk=bk: e.activation(out=eb[:].rearrange("p h n -> p (h n)"), in_=self.ps[bk][:],
                                                      func=AF.Exp), reads=[self.psb[bk]], writes=[eb_b])
            P.op("act", lambda e, bk=bk: e.activation(out=enb[:].rearrange("p h n -> p (h n)"), in_=self.ps[bk][:],
                                                      func=AF.Exp, scale=-1.0), reads=[self.psb[bk]], writes=[enb_b])
            P.op("dve", lambda e, tcs=tcs: e.tensor_tensor(out=qdec[:], in0=qTf[:, :, tcs], in1=eb[:], op=ALU.mult),
                 reads=[qTf_b, eb_b], writes=[qdec_b])
            P.op("dve", lambda e, tcs=tcs: e.tensor_tensor(out=kinv[:], in0=kTf[:, :, tcs], in1=enb[:], op=ALU.mult),
                 reads=[kTf_b, enb_b], writes=[kinv_b])
            bk = nb()

            def att(e, bk=bk):
                ins = None
                for h in range(4):
                    ins = e.matmul(self.ps[bk][:, h * 128:(h + 1) * 128], kinv[:, h, :], qdec[:, h, :],
                                   start=True, stop=True)
                return ins
            P.op("pe", att, reads=[kinv_b, qdec_b], writes=[self.psb[bk]])
            P.op("dve", lambda e, bk=bk: e.tensor_tensor(out=attnT[:].rearrange("p h n -> p (h n)"), in0=self.ps[bk][:],
                                                         in1=tri4[:].rearrange("p h n -> p (h n)"), op=ALU.mult),
                 reads=[self.psb[bk], tri4_b], writes=[attnT_b])
            obanks = []
            for hp in range(2):
                bk = nb()
                obanks.append(bk)

                def omm(e, bk=bk, hp=hp):
                    ins = None
                    for hl in range(2):
                        h = hp * 2 + hl
                        e.matmul(self.ps[bk][:, hl * 256:(hl + 1) * 256], attnT[:, h, :], vsb[:, h * 256:(h + 1) * 256],
                                 start=True, stop=False)
                        ins = e.matmul(self.ps[bk][:, hl * 256:(hl + 1) * 256], qdec[:, h, :], stb[:, h, :],
                                       start=False, stop=True)
                    return ins
                P.op("pe", omm, reads=[attnT_b, vsb_b, qdec_b, stb_b[hp * 2], stb_b[hp * 2 + 1]],
                     writes=[self.psb[bk]])
            for hp in range(2):
                bk = nb()

                def smm(e, bk=bk, hp=hp):
                    ins = None
                    for hl in range(2):
                        h = hp * 2 + hl
                        ins = e.matmul(self.ps[bk][:, hl * 256:(hl + 1) * 256], kend[:, h * 128:(h + 1) * 128],
                                       vsb[:, h * 256:(h + 1) * 256], start=True, stop=True)
                    return ins
                P.op("pe", smm, reads=[kend_b, vsb_b], writes=[self.psb[bk]])
                for hl in range(2):
                    h = hp * 2 + hl
                    P.op("dve", lambda e, bk=bk, hl=hl, h=h: e.scalar_tensor_tensor(
                        out=stf[:, h, :], in0=stf[:, h, :], scalar=eb[:, h, 127:128],
                        in1=self.ps[bk][:, hl * 256:(hl + 1) * 256], op0=ALU.mult, op1=ALU.add),
                        reads=[stf_b[h], eb_b, self.psb[bk]], writes=[stf_b[h]])
                    P.op("pool", lambda e, h=h: e.tensor_copy(out=stb[:, h, :], in_=stf[:, h, :]),
                         reads=[stf_b[h]], writes=[stb_b[h]])
            for hp in range(2):
                bk = obanks[hp]
                for hl in range(2):
                    h = hp * 2 + hl
                    P.op("act", lambda e, bk=bk, hl=hl, h=h: e.activation(
                        out=junk[:], in_=self.ps[bk][:, hl * 256:(hl + 1) * 256], func=AF.Square,
                        accum_out=ssq[:, h:h + 1]), reads=[self.psb[bk]], writes=[junk_b, ssq_b[h]])
            P.op("act", lambda e: e.activation(out=orstd[:], in_=ssq[:], func=AF.Sqrt, bias=self.c_eps[:],
                                               scale=1.0 / 256), reads=ssq_b + [self.cb_const], writes=[orstd_b])
            P.op("dve", lambda e: e.reciprocal(out=orstd[:], in_=orstd[:]), reads=[orstd_b], writes=[orstd_b])
            for hp in range(2):
                bk = obanks[hp]
                for hl in range(2):
                    h = hp * 2 + hl
                    P.op("dve", lambda e, bk=bk, hl=hl, h=h: e.scalar_tensor_tensor(
                        out=og[:, h * 256:(h + 1) * 256], in0=self.ps[bk][:, hl * 256:(hl + 1) * 256],
                        scalar=orstd[:, h:h + 1], in1=sgw[:, h * 256:(h + 1) * 256], op0=ALU.mult, op1=ALU.mult),
                        reads=[self.psb[bk], orstd_b, sgw_b], writes=[og_b])
            pst, pst_b = self.pst[tb % 2], self.pstb[tb % 2]

            def tr(e, pst=pst):
                ins = None
                for c in range(8):
                    ins = e.transpose(pst[:, c * 128:(c + 1) * 128], og[:, c * 128:(c + 1) * 128], self.c_identb[:])
                return ins
            P.op("pe", tr, reads=[og_b, self.cb_const], writes=[pst_b])
            P.op("act", lambda e, pst=pst, tcs=tcs: e.activation(out=ogT[:, :, tcs],
                                                                in_=pst[:].rearrange("p (c n) -> p c n", c=8),
                                                                func=AF.Copy), reads=[pst_b], writes=[ogT_b[tb]])
        pa = {0: partA(0)}
        for tb in range(4):
            if tb + 1 < 4:
                pa[tb + 1] = partA(tb + 1)
            partB(tb, pa[tb])
        for o in range(8):
            bk = nb()
            self.mm_group(self.ps[bk][:], [(wo[:, kc, o * 128:(o + 1) * 128], ogT[:, kc, :]) for kc in range(8)],
                          [wo_b] + ogT_b, self.psb[bk])
            xr, xr_b, xr_ch = rxr.next()
            P.op("sp", lambda e, xr=xr, o=o, cols=cols: e.dma_start(out=xr[:], in_=self.xT[o * 128:(o + 1) * 128, cols]),
                 reads=[self.xTb[o][cb]], writes=[xr_b], dma=True, chan=xr_ch)
            xo, xo_b, xo_ch = rxo.next()
            P.op("dve", lambda e, xo=xo, xr=xr, bk=bk: e.tensor_tensor(out=xo[:], in0=self.ps[bk][:], in1=xr[:],
                                                                     op=ALU.add),
                 reads=[self.psb[bk], xr_b], writes=[xo_b])
            P.op("sp", lambda e, xo=xo, o=o, cols=cols: e.dma_start(out=self.xT[o * 128:(o + 1) * 128, cols], in_=xo[:]),
                 reads=[xo_b], writes=[self.xTb[o][cb]], dma=True, chan=xo_ch)
    P.barrier()
    A.reset(m0)


K.gla = _gla


def gla_host(inputs, pre, name):
    if name == pre + "_gla_w_in" or name == pre + "_gla_w_out":
        return lay_cols(inputs[name])
    if name == pre + "_gla_w_gate_b":
        return np.ascontiguousarray(np.asarray(inputs[name], np.float32))
    if name == pre + "_gla_b_gate":
        return np.ascontiguousarray(np.asarray(inputs[name], np.float32).reshape(1, 512))
    if name == pre + "_gla_norm":
        return np.ascontiguousarray(np.broadcast_to(np.asarray(inputs[name], np.float32)[None, :], (128, 1024)))
    if name == pre + "_mix_norm":
        return lay_vecp(inputs[name])
    return None


def gla_consts(name):
    j = np.arange(128)[:, None]
    i = np.arange(128)[None, :]
    if name == "c_gla_cum":
        ms = np.where(j > i, -1.0 / 16.0, 0.0)
        mi = np.where(j <= i, -1.0 / 16.0, 0.0)
        tri = np.where(j <= i, 1.0, 0.0)
        return np.ascontiguousarray(np.concatenate([ms, mi, tri], axis=1).astype(np.float32))
    if name == "c_tri4":
        tri = np.where(j <= i, 1.0, 0.0)
        return np.ascontiguousarray(np.concatenate([tri] * 4, axis=1).astype(np.float32))
    return None


def _ssd(self, pre):
    P, A, S, NCB = self.P, self.A, self.S, self.NCB
    nw_d = self.dram_in(pre + "_mix_norm", [128, 8])
    wz_d = self.dram_in(pre + "_ssd_wz", [16, 128, 1024])
    wx_d = self.dram_in(pre + "_ssd_wxbc", [24, 128, 1024])
    wdt_d = self.dram_in(pre + "_ssd_wdt", [128, 8 * 32])
    cw_d = self.dram_in(pre + "_ssd_conv_w", [128, 24 * 4])
    cbias_d = self.dram_in(pre + "_ssd_conv_b", [128, 24])
    dtb_d = self.dram_in(pre + "_ssd_dt_bias", [128, 32])
    alog_d = self.dram_in(pre + "_ssd_a_log", [128, 32])
    dsk_d = self.dram_in(pre + "_ssd_d_skip", [128, 32])
    snw_d = self.dram_in(pre + "_ssd_norm", [128, 16])
    wo_d = self.dram_in(pre + "_ssd_w_out", [8, 128, 2048])
    cs_d = self.dram_in("c_ssd", [128, 3 * 512])
    m0 = A.mark()

    def ld(shape, dtype, src, eng="sp", name="w", view=None):
        t = A.alloc(shape, dtype, name)
        b = P.buf(name)
        dst = t[:] if view is None else view(t)
        P.op(eng, lambda e: e.dma_start(out=dst, in_=src), writes=[b], dma=True, chan=P.chan(name, eng))
        return t, b

    nw, nw_b = ld([128, 8], F32, nw_d, name="nw")
    wdt, wdt_b = ld([128, 8, 32], BF16, wdt_d, "pool", "wdt", lambda t: t[:].rearrange("p k n -> p (k n)"))
    cw, cw_b = ld([128, 24, 4], F32, cw_d, name="cw", view=lambda t: t[:].rearrange("p c k -> p (c k)"))
    cbias, cbias_b = ld([128, 24], F32, cbias_d, name="cbias")
    dtb, dtb_b = ld([128, 32], F32, dtb_d, name="dtb")
    ealog, ealog_b = ld([128, 32], F32, alog_d, name="alog")
    dsk, dsk_b = ld([128, 32], F32, dsk_d, name="dsk")
    snw, snw_b = ld([128, 16], F32, snw_d, name="snw")
    cs, cs_b = ld([128, 3, 512], F32, cs_d, name="cssd", view=lambda t: t[:].rearrange("p a n -> p (a n)"))
    ident4 = cs[:, 0, :]
    maskT4 = cs[:, 1, :]
    tri = cs[:, 2, 0:128]
    sel = cs[:, 2, 128:256]
    onesf = cs[:, 2, 256:384]
    P.op("act", lambda e: e.activation(out=ealog[:], in_=ealog[:], func=AF.Exp), reads=[ealog_b], writes=[ealog_b])
    c1 = A.alloc([128, 1], F32, "c1")
    c1_b = P.buf("c1")
    P.op("dve", lambda e: e.memset(c1[:], 1.0), writes=[c1_b])

    xin = A.alloc([128, 8, 512], F32, "xin")
    xin_b = P.buf("xin")
    xin_ch = P.chan("xin")
    sq = A.alloc([128, 8, 512], BF16, "sq")
    sq_b = P.buf("sq")
    rstd = A.alloc([128, 512], F32, "rstd")
    rstd_b = P.buf("rstd")
    xn = A.alloc([128, 8, 512], BF16, "xn")
    xn_b = P.buf("xn")
    rw = Ring(self, 3, [128, 8, 128], BF16, "wst", eng="pool")
    ru = Ring(self, 2, [128, 515], F32, "u", chan=False)
    racc = Ring(self, 2, [128, 512], F32, "acc", chan=False)
    rxc = Ring(self, 2, [128, 512], BF16, "xc", chan=False)
    halo = A.alloc([128, 24, 3], F32, "halo")
    halo_b = [P.buf(f"halo{c}") for c in range(24)]
    P.op("pool", lambda e: e.memset(halo[:], 0.0), writes=halo_b)
    xtm = A.alloc([128, 4, 2048], BF16, "xtm")
    xtm_b = [P.buf(f"xtm{c}") for c in range(16)]
    btm = A.alloc([128, 4, 512], BF16, "btm")
    btm_b = [P.buf(f"btm{g}") for g in range(4)]
    BT = A.alloc([128, 4, 512], BF16, "BT")
    BT_b = [P.buf(f"BT{g}") for g in range(4)]
    CT = A.alloc([128, 4, 512], BF16, "CT")
    CT_b = [P.buf(f"CT{g}") for g in range(4)]
    szT = A.alloc([128, 16, 512], BF16, "szT")
    szT_b = [P.buf(f"szT{c}") for c in range(16)]
    dtp = A.alloc([128, 4, 32], F32, "dtp")
    dtp_b = P.buf("dtp")
    dt = A.alloc([128, 4, 32], F32, "dt")
    dt_b = P.buf("dt")
    dta = A.alloc([128, 4, 32], F32, "dta")
    dta_b = P.buf("dta")
    acs = A.alloc([128, 4, 32], F32, "acs")
    acs_b = P.buf("acs")
    ea = A.alloc([128, 4, 32], F32, "ea")
    ea_b = P.buf("ea")
    dec = A.alloc([128, 4, 32], F32, "dec")
    dec_b = P.buf("dec")
    r2 = A.alloc([128, 8, 64], F32, "r2")
    r2_b = P.buf("r2")
    rtot = Ring(self, 2, [128, 512], F32, "totbc", chan=False)
    xdt = A.alloc([128, 4, 512], BF16, "xdt")
    xdt_b = [P.buf(f"xdt{g}") for g in range(4)]
    xdd = A.alloc([128, 4, 512], BF16, "xdd")
    xdd_b = [P.buf(f"xdd{g}") for g in range(4)]
    cbT = A.alloc([128, 4, 128], F32, "cbT")
    cbT_b = P.buf("cbT")
    rD = Ring(self, 2, [128, 4, 128], F32, "Dm", chan=False)
    rR = Ring(self, 2, [128, 4, 128], F32, "r23", chan=False)
    rLT = Ring(self, 2, [128, 4, 128], F32, "LT", chan=False)
    rWT = Ring(self, 2, [128, 4, 128], BF16, "WT", chan=False)
    stf = A.alloc([128, 4, 512], F32, "sstf")
    stf_b = [P.buf(f"sstf{g}") for g in range(4)]
    stb = A.alloc([128, 4, 512], BF16, "sstb")
    stb_b = [P.buf(f"sstb{g}") for g in range(4)]
    for g in range(4):
        P.op("dve", lambda e, g=g: e.memset(stf[:, g, :], 0.0), writes=[stf_b[g]])
        P.op("pool", lambda e, g=g: e.memset(stb[:, g, :], 0.0), writes=[stb_b[g]])
    rtmp = Ring(self, 2, [128, 8, 64], F32, "ytmp", chan=False)
    rtmp2 = Ring(self, 2, [128, 8, 64], F32, "ytmp2", chan=False)
    ry = Ring(self, 2, [128, 512], F32, "yg", chan=False)
    ygT = A.alloc([128, 16, 128], F32, "ygT")
    ygT_b = [P.buf(f"ygT{g}") for g in range(4)]
    ysq = A.alloc([128, 16, 128], BF16, "ysq")
    ysq_b = [P.buf(f"ysq{g}") for g in range(4)]
    yrs = A.alloc([128, 4, 128], F32, "yrs")
    yrs_b = P.buf("yrs")
    ynT = A.alloc([128, 16, 512], BF16, "ynT")
    ynT_b = [[P.buf(f"ynT{t}_{kc}") for kc in range(16)] for t in range(4)]
    rwo = Ring(self, 2, [128, 16, 128], BF16, "swo", eng="pool")
    rxr = Ring(self, 2, [128, 512], F32, "xres")
    rxo = Ring(self, 2, [128, 512], F32, "xo")
    bank = [0]
    cpi = [0]

    def nb():
        bank[0] = (bank[0] + 1) % 5
        return bank[0]

    def bc(ap2d, n):
        k = ap2d.shape[1]
        return ap2d.unsqueeze(2).to_broadcast([128, k, n])

    for cb in range(NCB):
        cols = slice(cb * 512, (cb + 1) * 512)
        self.load_xcols(cb, xin, xin_b, xin_ch)
        self.norm_block(cb, xin, xin_b, sq, sq_b, rstd, rstd_b, nw, nw_b, 5, lambda c: (xn[:, c, :], xn_b))
        bk = nb()

        def dtmm(e, bk=bk):
            ins = None
            for tb in range(4):
                for kc in range(8):
                    ins = e.matmul(self.ps[bk][:, tb * 32:(tb + 1) * 32], xn[:, kc, tb * 128:(tb + 1) * 128],
                                   wdt[:, kc, :], start=(kc == 0), stop=(kc == 7))
            return ins
        P.op("pe", dtmm, reads=[xn_b, wdt_b], writes=[self.psb[bk]])
        P.op("dve", lambda e, bk=bk: e.tensor_tensor(
            out=dtp[:], in0=self.ps[bk][:, 0:128].rearrange("p (t h) -> p t h", t=4),
            in1=dtb[:].unsqueeze(1).to_broadcast([128, 4, 32]), op=ALU.add),
            reads=[self.psb[bk], dtb_b], writes=[dtp_b])
        P.op("act", lambda e: e.activation(out=dtp[:], in_=dtp[:], func=AF.Exp), reads=[dtp_b], writes=[dtp_b])
        P.op("act", lambda e: e.activation(out=dt[:], in_=dtp[:], func=AF.Ln, bias=c1[:], scale=1.0),
             reads=[dtp_b, c1_b], writes=[dt_b])
        P.op("dve", lambda e: e.scalar_tensor_tensor(out=dta[:], in0=dt[:], scalar=-1.0,
                                                     in1=ealog[:].unsqueeze(1).to_broadcast([128, 4, 32]),
                                                     op0=ALU.mult, op1=ALU.mult),
             reads=[dt_b, ealog_b], writes=[dta_b])
        bk = nb()
        self.mm_group(self.ps[bk][:, 0:128], [(tri, dta[:].rearrange("p t h -> p (t h)"))], [cs_b, dta_b], self.psb[bk])
        P.op("dve", lambda e, bk=bk: e.tensor_copy(out=acs[:].rearrange("p t h -> p (t h)"), in_=self.ps[bk][:, 0:128]),
             reads=[self.psb[bk]], writes=[acs_b])
        P.op("act", lambda e: e.activation(out=ea[:], in_=acs[:], func=AF.Exp), reads=[acs_b], writes=[ea_b])
        bk = nb()
        self.mm_group(self.ps[bk][:, 0:128], [(sel, acs[:].rearrange("p t h -> p (t h)"))], [cs_b, acs_b], self.psb[bk])
        P.op("dve", lambda e, bk=bk: e.tensor_tensor(out=dec[:].rearrange("p t h -> p (t h)"), in0=self.ps[bk][:, 0:128],
                                                     in1=acs[:].rearrange("p t h -> p (t h)"), op=ALU.subtract),
             reads=[self.psb[bk], acs_b], writes=[dec_b])
        P.op("act", lambda e: e.activation(out=dec[:], in_=dec[:], func=AF.Exp), reads=[dec_b], writes=[dec_b])
        for c in range(24):
            w, w_b, w_ch = rw.next()
            P.op("pool", lambda e, w=w, c=c: e.dma_start(out=w[:].rearrange("p k j -> p (k j)"), in_=wx_d[c]),
                 writes=[w_b], dma=True, chan=w_ch)
            bk = nb()
            self.mm_group(self.ps[bk][:], [(w[:, kc, :], xn[:, kc, :]) for kc in range(8)], [w_b, xn_b], self.psb[bk])
            u, u_b, _ = ru.next()
            P.op("act", lambda e, u=u, bk=bk: e.activation(out=u[:, 3:515], in_=self.ps[bk][:], func=AF.Copy),
                 reads=[self.psb[bk]], writes=[u_b])
            P.op("pool", lambda e, u=u, c=c: e.tensor_copy(out=u[:, 0:3], in_=halo[:, c, :]),
                 reads=[halo_b[c]], writes=[u_b])
            P.op("pool", lambda e, u=u, c=c: e.tensor_copy(out=halo[:, c, :], in_=u[:, 512:515]),
                 reads=[u_b], writes=[halo_b[c]])
            acc, acc_b, _ = racc.next()
            P.op("dve", lambda e, u=u, acc=acc, c=c: e.tensor_scalar(acc[:], u[:, 0:512], cw[:, c, 0:1], None, ALU.mult),
                 reads=[u_b, cw_b], writes=[acc_b])
            for k in range(1, 4):
                P.op("dve", lambda e, u=u, acc=acc, c=c, k=k: e.scalar_tensor_tensor(
                    out=acc[:], in0=u[:, k:k + 512], scalar=cw[:, c, k:k + 1], in1=acc[:], op0=ALU.mult, op1=ALU.add),
                    reads=[u_b, cw_b, acc_b], writes=[acc_b])
            if c < 20:
                xc, xc_b, _ = rxc.next()
                if c >= 16:
                    dst, dst_b = BT[:, c - 16, :], BT_b[c - 16]
                    P.op("act", lambda e, acc=acc, dst=dst, c=c: e.activation(out=dst, in_=acc[:], func=AF.Silu,
                                                                            bias=cbias[:, c:c + 1], scale=1.0),
                         reads=[acc_b, cbias_b], writes=[dst_b])
                    src, src_b = dst, dst_b
                else:
                    P.op("act", lambda e, acc=acc, xc=xc, c=c: e.activation(out=xc[:], in_=acc[:], func=AF.Silu,
                                                                          bias=cbias[:, c:c + 1], scale=1.0),
                         reads=[acc_b, cbias_b], writes=[xc_b])
                    src, src_b = xc[:], xc_b
                pst, pst_b = self.pst[c % 2], self.pstb[c % 2]

                def tr(e, pst=pst, src=src):
                    ins = None
                    for tb in range(4):
                        ins = e.transpose(pst[:, tb * 128:(tb + 1) * 128], src[:, tb * 128:(tb + 1) * 128],
                                          self.c_identb[:])
                    return ins
                P.op("pe", tr, reads=[src_b, self.cb_const], writes=[pst_b])
                if c < 16:
                    o_ap, o_b = xtm[:, :, c * 128:(c + 1) * 128], xtm_b[c]
                else:
                    o_ap, o_b = btm[:, :, (c - 16) * 128:(c - 15) * 128], btm_b[c - 16]
                cpi[0] += 1
                i_ap = pst[:, 0:512].rearrange("p (t n) -> p t n", t=4)
                if cpi[0] % 2 == 0:
                    P.op("act", lambda e, o_ap=o_ap, i_ap=i_ap: e.activation(out=o_ap, in_=i_ap, func=AF.Copy),
                         reads=[pst_b], writes=[o_b])
                else:
                    P.op("dve", lambda e, o_ap=o_ap, i_ap=i_ap: e.tensor_copy(out=o_ap, in_=i_ap),
                         reads=[pst_b], writes=[o_b])
            else:
                g = c - 20
                P.op("act", lambda e, acc=acc, g=g, c=c: e.activation(out=CT[:, g, :], in_=acc[:], func=AF.Silu,
                                                                    bias=cbias[:, c:c + 1], scale=1.0),
                     reads=[acc_b, cbias_b], writes=[CT_b[g]])
        for c in range(16):
            w, w_b, w_ch = rw.next()
            P.op("pool", lambda e, w=w, c=c: e.dma_start(out=w[:].rearrange("p k j -> p (k j)"), in_=wz_d[c]),
                 writes=[w_b], dma=True, chan=w_ch)
            bk = nb()
            self.mm_group(self.ps[bk][:], [(w[:, kc, :], xn[:, kc, :]) for kc in range(8)], [w_b, xn_b], self.psb[bk])
            P.op("act", lambda e, c=c, bk=bk: e.activation(out=szT[:, c, :], in_=self.ps[bk][:], func=AF.Silu),
                 reads=[self.psb[bk]], writes=[szT_b[c]])
        for tb in range(4):
            tcs = slice(tb * 128, (tb + 1) * 128)
            for g in range(4):
                gs = slice(g * 8, (g + 1) * 8)
                gc = slice(g * 512, (g + 1) * 512)
                P.op("pool", lambda e, g=g, gs=gs, gc=gc, tb=tb: e.tensor_tensor(
                    out=xdt[:, g, :].rearrange("p (h n) -> p h n", h=8),
                    in0=xtm[:, tb, gc].rearrange("p (h n) -> p h n", h=8), in1=bc(dt[:, tb, gs], 64), op=ALU.mult),
                    reads=xtm_b[g * 4:(g + 1) * 4] + [dt_b], writes=[xdt_b[g]])
                P.op("pool", lambda e, g=g, gs=gs, tb=tb: e.tensor_tensor(
                    out=xdd[:, g, :].rearrange("p (h n) -> p h n", h=8),
                    in0=xdt[:, g, :].rearrange("p (h n) -> p h n", h=8), in1=bc(dec[:, tb, gs], 64), op=ALU.mult),
                    reads=[xdt_b[g], dec_b], writes=[xdd_b[g]])
            bk = nb()

            def cbmm(e, bk=bk, tcs=tcs):
                ins = None
                for g in range(4):
                    ins = e.matmul(self.ps[bk][:, g * 128:(g + 1) * 128], BT[:, g, tcs], CT[:, g, tcs],
                                   start=True, stop=True)
                return ins
            P.op("pe", cbmm, reads=BT_b + CT_b, writes=[self.psb[bk]])
            P.op("act", lambda e, bk=bk: e.activation(out=cbT[:].rearrange("p g n -> p (g n)"), in_=self.ps[bk][:],
                                                      func=AF.Copy), reads=[self.psb[bk]], writes=[cbT_b])
            for g in range(4):
                gs = slice(g * 8, (g + 1) * 8)
                gc = slice(g * 512, (g + 1) * 512)
                ybk = nb()
                ydms = []
                for sl in range(2):
                    h0 = g * 8 + sl * 4
                    hs = slice(h0, h0 + 4)
                    Dm, Dm_b, _ = rD.next()
                    r23, r23_b, _ = rR.next()
                    P.op("dve", lambda e, Dm=Dm, hs=hs, tb=tb: e.tensor_tensor(
                        out=Dm[:], in0=ident4.rearrange("p (h n) -> p h n", h=4), in1=bc(acs[:, tb, hs], 128), op=ALU.mult),
                        reads=[cs_b, acs_b], writes=[Dm_b])
                    P.op("pool", lambda e, r23=r23, hs=hs, tb=tb: e.tensor_tensor(
                        out=r23[:], in0=maskT4.rearrange("p (h n) -> p h n", h=4), in1=bc(acs[:, tb, hs], 128),
                        op=ALU.subtract), reads=[cs_b, acs_b], writes=[r23_b])
                    bk = nb()
                    if bk == ybk:
                        bk = nb()
                    self.mm_group(self.ps[bk][:], [(onesf, Dm[:].rearrange("p h n -> p (h n)")),
                                                   (self.c_identf[:], r23[:].rearrange("p h n -> p (h n)"))],
                                  [cs_b, Dm_b, r23_b, self.cb_const], self.psb[bk])
                    LT, LT_b, _ = rLT.next()
                    P.op("act", lambda e, LT=LT, bk=bk: e.activation(out=LT[:].rearrange("p h n -> p (h n)"),
                                                                   in_=self.ps[bk][:], func=AF.Exp),
                         reads=[self.psb[bk]], writes=[LT_b])
                    WT, WT_b, _ = rWT.next()
                    P.op("dve", lambda e, WT=WT, LT=LT, g=g: e.tensor_tensor(
                        out=WT[:], in0=LT[:], in1=cbT[:, g:g + 1, :].to_broadcast([128, 4, 128]), op=ALU.mult),
                        reads=[LT_b, cbT_b], writes=[WT_b])

                    def ydm(e, WT=WT, ybk=ybk, g=g, sl=sl):
                        ins = None
                        for hl in range(4):
                            hh = sl * 4 + hl
                            ins = e.matmul(self.ps[ybk][:, hh * 64:(hh + 1) * 64], WT[:, hl, :],
                                           xdt[:, g, hh * 64:(hh + 1) * 64], start=True, stop=True)
                        return ins
                    ydms.append((ydm, WT_b))
                for ydm, WT_b in ydms:
                    P.op("pe", ydm, reads=[WT_b, xdt_b[g]], writes=[self.psb[ybk]])
                obk = nb()
                if obk == ybk:
                    obk = nb()
                self.mm_group(self.ps[obk][:], [(CT[:, g, tcs], stb[:, g, :])], [CT_b[g], stb_b[g]], self.psb[obk])
                tmp, tmp_b, _ = rtmp.next()
                tmp2, tmp2_b, _ = rtmp2.next()
                P.op("dve", lambda e, tmp=tmp, obk=obk, gs=gs, tb=tb: e.tensor_tensor(
                    out=tmp[:], in0=self.ps[obk][:].rearrange("p (h n) -> p h n", h=8), in1=bc(ea[:, tb, gs], 64),
                    op=ALU.mult), reads=[self.psb[obk], ea_b], writes=[tmp_b])
                P.op("pool", lambda e, tmp2=tmp2, gs=gs, gc=gc, tb=tb: e.tensor_tensor(
                    out=tmp2[:], in0=xtm[:, tb, gc].rearrange("p (h n) -> p h n", h=8), in1=bc(dsk[:, gs], 64),
                    op=ALU.mult), reads=xtm_b[g * 4:(g + 1) * 4] + [dsk_b], writes=[tmp2_b])
                P.op("pool", lambda e, tmp=tmp, tmp2=tmp2: e.tensor_tensor(out=tmp[:], in0=tmp[:], in1=tmp2[:],
                                                                          op=ALU.add),
                     reads=[tmp_b, tmp2_b], writes=[tmp_b])
                y, y_b, _ = ry.next()
                P.op("dve", lambda e, y=y, tmp=tmp, ybk=ybk: e.tensor_tensor(
                    out=y[:], in0=self.ps[ybk][:], in1=tmp[:].rearrange("p h n -> p (h n)"), op=ALU.add),
                    reads=[self.psb[ybk], tmp_b], writes=[y_b])
                P.op("pool", lambda e, gs=gs, tb=tb: e.tensor_copy(out=r2[:], in_=bc(acs[:, tb, gs], 64)),
                     reads=[acs_b], writes=[r2_b])
                bk = nb()
                self.mm_group(self.ps[bk][:], [(sel, r2[:].rearrange("p h n -> p (h n)"))], [cs_b, r2_b], self.psb[bk])
                tot, tot_b, _ = rtot.next()
                P.op("act", lambda e, bk=bk, tot=tot: e.activation(out=tot[:], in_=self.ps[bk][:], func=AF.Exp),
                     reads=[self.psb[bk]], writes=[tot_b])
                sbk = nb()
                self.mm_group(self.ps[sbk][:], [(btm[:, tb, g * 128:(g + 1) * 128], xdd[:, g, :])],
                              [btm_b[g], xdd_b[g]], self.psb[sbk])
                P.op("dve", lambda e, g=g, tot=tot: e.tensor_tensor(out=stf[:, g, :], in0=stf[:, g, :], in1=tot[:],
                                                                    op=ALU.mult),
                     reads=[stf_b[g], tot_b], writes=[stf_b[g]])
                P.op("dve", lambda e, g=g, sbk=sbk: e.tensor_tensor(out=stf[:, g, :], in0=stf[:, g, :],
                                                                    in1=self.ps[sbk][:], op=ALU.add),
                     reads=[stf_b[g], self.psb[sbk]], writes=[stf_b[g]])
                P.op("act", lambda e, g=g: e.activation(out=stb[:, g, :], in_=stf[:, g, :], func=AF.Copy),
                     reads=[stf_b[g]], writes=[stb_b[g]])
                tbk = nb()

                def ytr(e, tbk=tbk, y=y):
                    ins = None
                    for q in range(4):
                        ins = e.transpose(self.ps[tbk][:, q * 128:(q + 1) * 128], y[:, q * 128:(q + 1) * 128],
                                          self.c_identf[:])
                    return ins
                P.op("pe", ytr, reads=[y_b, self.cb_const], writes=[self.psb[tbk]])
                P.op("dve", lambda e, tbk=tbk, g=g, tcs=tcs: e.tensor_tensor(
                    out=ygT[:, g * 4:(g + 1) * 4, :], in0=self.ps[tbk][:].rearrange("p (q n) -> p q n", q=4),
                    in1=szT[:, g * 4:(g + 1) * 4, tcs], op=ALU.mult),
                    reads=[self.psb[tbk]] + szT_b[g * 4:(g + 1) * 4], writes=[ygT_b[g]])
                P.op("act", lambda e, g=g: e.activation(out=ysq[:, g * 4:(g + 1) * 4, :], in_=ygT[:, g * 4:(g + 1) * 4, :],
                                                        func=AF.Square), reads=[ygT_b[g]], writes=[ysq_b[g]])
            nbk = nb()

            def nrm(e, nbk=nbk):
                ins = None
                for g in range(4):
                    for q in range(4):
                        ins = e.matmul(self.ps[nbk][:, g * 128:(g + 1) * 128], self.c_onesb[:], ysq[:, g * 4 + q, :],
                                       start=(q == 0), stop=(q == 3))
                return ins
            P.op("pe", nrm, reads=ysq_b + [self.cb_const], writes=[self.psb[nbk]])
            P.op("act", lambda e, nbk=nbk: e.activation(out=yrs[:].rearrange("p g n -> p (g n)"), in_=self.ps[nbk][:],
                                                        func=AF.Sqrt, bias=self.c_eps[:], scale=1.0 / 512),
                 reads=[self.psb[nbk], self.cb_const], writes=[yrs_b])
            P.op("dve", lambda e: e.reciprocal(out=yrs[:], in_=yrs[:]), reads=[yrs_b], writes=[yrs_b])
            for kc in range(16):
                g = kc // 4
                P.op("dve", lambda e, kc=kc, g=g, tcs=tcs: e.scalar_tensor_tensor(
                    out=ynT[:, kc, tcs], in0=ygT[:, kc, :], scalar=snw[:, kc:kc + 1], in1=yrs[:, g, :],
                    op0=ALU.mult, op1=ALU.mult), reads=[ygT_b[g], snw_b, yrs_b], writes=[ynT_b[tb][kc]])
        for o in range(8):
            wo, wo_b, wo_ch = rwo.next()
            P.op("pool", lambda e, wo=wo, o=o: e.dma_start(out=wo[:].rearrange("p k j -> p (k j)"), in_=wo_d[o]),
                 writes=[wo_b], dma=True, chan=wo_ch)
            bk = nb()
            self.mm_group(self.ps[bk][:], [(wo[:, kc, :], ynT[:, kc, :]) for kc in range(16)],
                          [wo_b] + [ynT_b[t][kc] for t in range(4) for kc in range(16)],
                          self.psb[bk])
            xr, xr_b, xr_ch = rxr.next()
            P.op("sp", lambda e, xr=xr, o=o, cols=cols: e.dma_start(out=xr[:], in_=self.xT[o * 128:(o + 1) * 128, cols]),
                 reads=[self.xTb[o][cb]], writes=[xr_b], dma=True, chan=xr_ch)
            xo, xo_b, xo_ch = rxo.next()
            P.op("dve", lambda e, xo=xo, xr=xr, bk=bk: e.tensor_tensor(out=xo[:], in0=self.ps[bk][:], in1=xr[:],
                                                                     op=ALU.add),
                 reads=[self.psb[bk], xr_b], writes=[xo_b])
            P.op("sp", lambda e, xo=xo, o=o, cols=cols: e.dma_start(out=self.xT[o * 128:(o + 1) * 128, cols], in_=xo[:]),
                 reads=[xo_b], writes=[self.xTb[o][cb]], dma=True, chan=xo_ch)
    P.barrier()
    A.reset(m0)


K.ssd = _ssd


def lay_chunks(w):
    w = np.asarray(w, np.float32)
    kc, n = w.shape[0] // 128, w.shape[1] // 128
    a = w.reshape(kc, 128, n, 128).transpose(2, 1, 0, 3)
    return np.ascontiguousarray(a).reshape(n, 128, kc * 128)


def rep128(v):
    v = np.asarray(v, np.float32).reshape(1, -1)
    return np.ascontiguousarray(np.broadcast_to(v, (128, v.shape[1])))


def ssd_host(inputs, pre, name):
    if name == pre + "_ssd_wz":
        return lay_chunks(np.asarray(inputs[pre + "_ssd_w_in"])[:, 0:2048])
    if name == pre + "_ssd_wxbc":
        return lay_chunks(np.asarray(inputs[pre + "_ssd_w_in"])[:, 2048:5120])
    if name == pre + "_ssd_wdt":
        return lay_cols(np.asarray(inputs[pre + "_ssd_w_in"])[:, 5120:5152])
    if name == pre + "_ssd_conv_w":
        w = np.asarray(inputs[name], np.float32)
        return np.ascontiguousarray(w.reshape(4, 24, 128).transpose(2, 1, 0)).reshape(128, 96)
    if name == pre + "_ssd_conv_b":
        return lay_vecp(inputs[name])
    if name in (pre + "_ssd_dt_bias", pre + "_ssd_a_log", pre + "_ssd_d_skip"):
        return rep128(inputs[name])
    if name == pre + "_ssd_norm" or name == pre + "_mix_norm":
        return lay_vecp(inputs[name])
    if name == pre + "_ssd_w_out":
        return lay_chunks(inputs[name])
    return None


def ssd_consts(name):
    if name != "c_ssd":
        return None
    j = np.arange(128)[:, None]
    i = np.arange(128)[None, :]
    ident = (j == i).astype(np.float32)
    maskT = np.where(i < j, -30000.0, 0.0).astype(np.float32)
    tri = (j <= i).astype(np.float32)
    sel = np.zeros((128, 128), np.float32)
    sel[127, :] = 1.0
    ones = np.ones((128, 128), np.float32)
    z = np.zeros((128, 128), np.float32)
    return np.ascontiguousarray(np.concatenate([ident] * 4 + [maskT] * 4 + [tri, sel, ones, z], axis=1))
```

```python
import contextlib
import numpy as np
import concourse.bass as bass
import concourse.mybir as mybir
from concourse.bass_utils import run_bass_kernel_spmd

F32 = mybir.dt.float32
BF16 = mybir.dt.bfloat16
I32 = mybir.dt.int32
AF = mybir.ActivationFunctionType
ALU = mybir.AluOpType
AX = mybir.AxisListType

D = 1024
DFF = 2816
NFC = DFF // 128
EPS = 1e-6
SBUF_BASE = 16640
SBUF_END = 229376
DEBUG_STOP = 0


class Buf:
    __slots__ = ("name", "last_w", "readers")

    def __init__(self, name=""):
        self.name = name
        self.last_w = None
        self.readers = []


class Op:
    __slots__ = ("idx", "eng", "fn", "deps", "sem", "val", "signal", "dma", "chan", "final", "used")

    def __init__(self):
        self.signal = False
        self.sem = None
        self.val = None
        self.final = False
        self.used = False


class Chan:
    __slots__ = ("sem", "count", "name", "rec")

    def __init__(self, name):
        self.name = name
        self.sem = None
        self.count = 0
        self.rec = 0


class Prog:
    ENGS = ("pe", "act", "dve", "pool", "sp")
    SAME_ENGINE_SYNC = True
    EPOCH = 8000
    CHAN_MAX = 480

    def __init__(self, nc):
        self.nc = nc
        self.ops = []
        self.chans = []
        self.fence_deps = []
        self.last_nd = {e: None for e in self.ENGS}
        self.pending_dma = {}
        self.free_chans = {}
        self.live_chans = []

    def buf(self, name=""):
        return Buf(name)

    def chan(self, name="", eng="sp"):
        kind = "sw" if eng == "pool" else "hw"
        fl = self.free_chans.setdefault(kind, [])
        if fl:
            c = fl.pop()
        else:
            c = Chan(f"{kind}{len(self.chans)}")
            self.chans.append(c)
        self.live_chans.append((kind, c))
        return c

    def op(self, eng, fn, reads=(), writes=(), dma=False, chan=None):
        o = Op()
        o.idx = len(self.ops)
        o.eng = eng
        o.fn = fn
        o.dma = dma
        o.chan = chan
        deps = set(self.fence_deps)
        for b in reads:
            if b.last_w is not None:
                deps.add(b.last_w)
        for b in writes:
            if b.last_w is not None:
                deps.add(b.last_w)
            deps.update(b.readers)
        o.deps = deps
        for d in deps:
            d.signal = True
            if d.dma:
                self.pending_dma.pop(d, None)
        if dma:
            assert chan is not None
            o.signal = True
            chan.rec += 1
            self.pending_dma[o] = True
        else:
            self.last_nd[eng] = o
        for b in reads:
            b.readers.append(o)
        for b in writes:
            b.last_w = o
            b.readers = []
        self.ops.append(o)
        return o

    def barrier(self):
        deps = [o for o in self.last_nd.values() if o is not None]
        deps += list(self.pending_dma.keys())
        for d in deps:
            d.signal = True
        self.fence_deps = deps
        for kind, c in self.live_chans:
            if c.rec < self.CHAN_MAX:
                self.free_chans.setdefault(kind, []).append(c)
        self.live_chans = []

    def emit(self):
        nc = self.nc
        per_eng = {e: [o for o in self.ops if o.eng == e] for e in self.ENGS}
        n_sig = {e: sum(1 for o in per_eng[e] if o.signal and not o.dma) for e in self.ENGS}
        stats = {e: [0, 0] for e in self.ENGS}
        with contextlib.ExitStack() as st:
            eng_sems = {}
            for e in self.ENGS:
                n_ep = max(1, (n_sig[e] + self.EPOCH - 1) // self.EPOCH)
                eng_sems[e] = [st.enter_context(nc.semaphore(f"s_{e}{i}")) for i in range(n_ep)]
            for c in self.chans:
                c.sem = st.enter_context(nc.semaphore(f"c_{c.name}"))
                c.count = 0
            for o in self.ops:
                if o.dma:
                    o.chan.count += 1
                    o.sem = o.chan.sem
                    o.val = 16 * o.chan.count
            for e in self.ENGS:
                cnt = 0
                for o in per_eng[e]:
                    if o.dma:
                        pass
                    elif o.signal:
                        ep = cnt // self.EPOCH
                        cnt += 1
                        o.sem = eng_sems[e][ep]
                        o.val = cnt - ep * self.EPOCH
            engobj = {"pe": nc.tensor, "act": nc.scalar, "dve": nc.vector, "pool": nc.gpsimd,
                      "sp": nc.sync}

            def run_engine(e):
                eng = engobj[e]
                waited = {}
                for o in per_eng[e]:
                    for d in sorted(o.deps, key=lambda d: d.idx):
                        if d.eng == e and not d.dma:
                            if e == "pe" or not self.SAME_ENGINE_SYNC:
                                continue
                        key = d.sem.num
                        if waited.get(key, 0) >= d.val:
                            continue
                        eng.wait_ge(d.sem, d.val)
                        stats[e][1] += 1
                        waited[key] = d.val
                    ins = o.fn(eng)
                    stats[e][0] += 1
                    if o.dma:
                        ins.then_inc(o.sem, 16)
                    elif o.signal:
                        ins.then_inc(o.sem, 1)
                if e == "sp":
                    for o in self.ops:
                        if o.dma and o.final and waited.get(o.sem.num, 0) < o.val:
                            eng.wait_ge(o.sem, o.val)
                            waited[o.sem.num] = o.val

            with nc.Block() as block:
                @block.tensor
                def _(eng):
                    run_engine("pe")

                @block.scalar
                def _(eng):
                    run_engine("act")

                @block.vector
                def _(eng):
                    run_engine("dve")

                @block.gpsimd
                def _(eng):
                    run_engine("pool")

                @block.sync
                def _(eng):
                    run_engine("sp")
        return stats


class Arena:
    def __init__(self, nc, base=SBUF_BASE, end=SBUF_END):
        self.nc = nc
        self.base = base
        self.end = end
        self.off = base
        self.n = 0

    def alloc(self, shape, dtype, name="t"):
        esz = 2 if dtype == BF16 else 4
        nbytes = int(np.prod(shape[1:])) * esz
        nbytes = (nbytes + 63) // 64 * 64
        assert self.off + nbytes <= self.end, f"SBUF overflow allocating {name} {shape}: off={self.off}"
        self.n += 1
        t = self.nc.alloc_sbuf_tensor_at(f"{name}_{self.n}", list(shape), dtype, offset=self.off)
        self.off += nbytes
        return t

    def mark(self):
        return self.off

    def reset(self, m):
        self.off = m


class Ring:
    def __init__(self, K, n, shape, dtype, name, chan=True, eng="sp"):
        self.items = []
        for i in range(n):
            t = K.A.alloc(shape, dtype, name)
            self.items.append((t, K.P.buf(f"{name}{i}"), K.P.chan(f"{name}{i}", eng) if chan else None))
        self.i = 0

    def next(self):
        it = self.items[self.i % len(self.items)]
        self.i += 1
        return it


class K:
    def __init__(self, S):
        self.S = S
        self.NCB = S // 512
        self.nc = bass.Bass("TRN2", target_bir_lowering=False)
        self.P = Prog(self.nc)
        self.A = Arena(self.nc)
        self.inputs = {}
        self.dram_aps = {}
        nc = self.nc
        self.ps = [nc.alloc_psum_tensor(f"ps{i}", [128, 512], F32) for i in range(6)]
        self.psb = [self.P.buf(f"ps{i}") for i in range(6)]
        self.pst = [nc.alloc_psum_tensor(f"pst{i}", [128, 1024], BF16) for i in range(2)]
        self.pstb = [self.P.buf(f"pst{i}") for i in range(2)]
        self.xT = nc.dram_tensor("xT_scr", [D, S], F32, kind="Internal").ap()
        self.xTb = [[self.P.buf(f"xT{c}_{cb}") for cb in range(self.NCB)] for c in range(8)]
        self.xTv = self.xT.rearrange("(c p) s -> p c s", p=128)

    def dram_in(self, name, shape, dtype=F32):
        if name in self.dram_aps:
            return self.dram_aps[name]
        t = self._dram_in(name, shape, dtype)
        self.dram_aps[name] = t
        return t

    def _dram_in(self, name, shape, dtype=F32):
        t = self.nc.dram_tensor(name, list(shape), dtype, kind="ExternalInput").ap()
        self.inputs[name] = (tuple(shape), dtype)
        return t

    def load_consts(self):
        P, A = self.P, self.A
        self.c_identf = A.alloc([128, 128], F32, "identf")
        self.c_identb = A.alloc([128, 128], BF16, "identb")
        self.c_onesb = A.alloc([128, 128], BF16, "onesb")
        d_ident = self.dram_in("c_ident", [128, 128])
        self.cb_const = P.buf("consts")
        ch = P.chan("const")
        P.op("sp", lambda e: e.dma_start(out=self.c_identf[:], in_=d_ident), writes=[self.cb_const],
             dma=True, chan=ch)
        P.op("dve", lambda e: e.tensor_copy(out=self.c_identb[:], in_=self.c_identf[:]),
             reads=[self.cb_const], writes=[self.cb_const])
        P.op("dve", lambda e: e.memset(self.c_onesb[:], 1.0), writes=[self.cb_const])
        self.c_eps = A.alloc([128, 1], F32, "eps")
        P.op("dve", lambda e: e.memset(self.c_eps[:], EPS), writes=[self.cb_const])

    def mm_group(self, ps_ap, pairs, reads, psbuf):
        n = len(pairs)

        def fn(e):
            ins = None
            for i, (l, r) in enumerate(pairs):
                ins = e.matmul(ps_ap, l, r, start=(i == 0), stop=(i == n - 1))
            return ins
        return self.P.op("pe", fn, reads=reads, writes=[psbuf])

    def norm_block(self, cb, xin, xin_b, sq, sq_b, rstd, rstd_b, nw, nw_b, ps_i, out_fn):
        P = self.P
        P.op("act", lambda e: e.activation(out=sq[:], in_=xin[:], func=AF.Square),
             reads=[xin_b], writes=[sq_b])
        ps, psb = self.ps[ps_i], self.psb[ps_i]
        self.mm_group(ps[:], [(self.c_onesb[:], sq[:, c, :]) for c in range(8)],
                      [sq_b, self.cb_const], psb)
        P.op("act", lambda e: e.activation(out=rstd[:], in_=ps[:], func=AF.Sqrt, bias=self.c_eps[:], scale=1.0 / D),
             reads=[psb, self.cb_const], writes=[rstd_b])
        P.op("dve", lambda e: e.reciprocal(out=rstd[:], in_=rstd[:]), reads=[rstd_b], writes=[rstd_b])
        for c in range(8):
            o_ap, o_b = out_fn(c)
            P.op("dve", (lambda e, c=c, o_ap=o_ap: e.scalar_tensor_tensor(
                out=o_ap, in0=xin[:, c, :], scalar=nw[:, c:c + 1], in1=rstd[:],
                op0=ALU.mult, op1=ALU.mult)),
                reads=[xin_b, rstd_b, nw_b], writes=[o_b])

    def load_xcols(self, cb, xin, xin_b, ch, eng="sp"):
        return self.P.op(eng, lambda e: e.dma_start(out=xin[:], in_=self.xTv[:, :, cb * 512:(cb + 1) * 512]),
                         reads=[self.xTb[c][cb] for c in range(8)], writes=[xin_b], dma=True, chan=ch)

    def stage_in(self):
        P, A, S = self.P, self.A, self.S
        x = self.dram_in("x", [S, D])
        m = A.mark()
        rin = Ring(self, 2, [128, D], F32, "s0in")
        rout = Ring(self, 2, [128, 8, 512], F32, "s0out")
        for cb in range(self.NCB):
            xo, xo_b, xo_ch = rout.next()
            for tb in range(4):
                t0 = cb * 512 + tb * 128
                xi, xi_b, xi_ch = rin.next()
                P.op("sp", lambda e, xi=xi, t0=t0: e.dma_start(out=xi[:], in_=x[t0:t0 + 128, :]),
                     writes=[xi_b], dma=True, chan=xi_ch)
                for half in range(2):
                    pi = (tb * 2 + half) % 4
                    ps, psb = self.ps[pi], self.psb[pi]

                    def fn(e, xi=xi, ps=ps, half=half):
                        ins = None
                        for j in range(4):
                            c = half * 4 + j
                            ins = e.transpose(ps[:, j * 128:(j + 1) * 128], xi[:, c * 128:(c + 1) * 128],
                                              self.c_identf[:])
                        return ins
                    P.op("pe", fn, reads=[xi_b, self.cb_const], writes=[psb])
                    eng = "act" if half == 0 else "dve"

                    def cp(e, xo=xo, ps=ps, half=half, tb=tb, eng=eng):
                        o = xo[:, half * 4:(half + 1) * 4, tb * 128:(tb + 1) * 128]
                        i = ps[:].rearrange("p (j t) -> p j t", j=4)
                        if eng == "act":
                            return e.activation(out=o, in_=i, func=AF.Copy)
                        return e.tensor_copy(out=o, in_=i)
                    P.op(eng, cp, reads=[psb], writes=[xo_b])
            P.op("sp", lambda e, xo=xo, cb=cb: e.dma_start(out=self.xTv[:, :, cb * 512:(cb + 1) * 512], in_=xo[:]),
                 reads=[xo_b], writes=[self.xTb[c][cb] for c in range(8)], dma=True, chan=xo_ch)
        P.barrier()
        A.reset(m)

    def stage_out(self):
        P, A, S = self.P, self.A, self.S
        out = self.nc.dram_tensor("out", [S, D], F32, kind="ExternalOutput").ap()
        nw_d = self.dram_in("final_norm", [128, 8])
        m = A.mark()
        nw = A.alloc([128, 8], F32, "nw")
        nw_b = P.buf("nw")
        P.op("sp", lambda e: e.dma_start(out=nw[:], in_=nw_d), writes=[nw_b], dma=True, chan=P.chan("nwf"))
        rin = Ring(self, 2, [128, 8, 512], F32, "fin")
        sq = A.alloc([128, 8, 512], BF16, "fsq")
        sq_b = P.buf("fsq")
        rstd = A.alloc([128, 512], F32, "frstd")
        rstd_b = P.buf("frstd")
        xnf = A.alloc([128, 8, 512], F32, "fxn")
        xnf_b = [P.buf(f"fxn{c}") for c in range(8)]
        rout = Ring(self, 2, [128, D], F32, "fout")
        for cb in range(self.NCB):
            xin, xin_b, xin_ch = rin.next()
            self.load_xcols(cb, xin, xin_b, xin_ch)
            self.norm_block(cb, xin, xin_b, sq, sq_b, rstd, rstd_b, nw, nw_b, 5,
                            lambda c: (xnf[:, c, :], xnf_b[c]))
            for tb in range(4):
                yo, yo_b, yo_ch = rout.next()
                for half in range(2):
                    pi = (tb * 2 + half) % 4
                    ps, psb = self.ps[pi], self.psb[pi]

                    def fn(e, ps=ps, half=half, tb=tb):
                        ins = None
                        for j in range(4):
                            c = half * 4 + j
                            ins = e.transpose(ps[:, j * 128:(j + 1) * 128],
                                              xnf[:, c, tb * 128:(tb + 1) * 128], self.c_identf[:])
                        return ins
                    P.op("pe", fn, reads=[xnf_b[half * 4 + j] for j in range(4)] + [self.cb_const], writes=[psb])
                    eng = "act" if half == 0 else "dve"

                    def cp(e, yo=yo, ps=ps, half=half, eng=eng):
                        o = yo[:, half * 512:(half + 1) * 512]
                        if eng == "act":
                            return e.activation(out=o, in_=ps[:], func=AF.Copy)
                        return e.tensor_copy(out=o, in_=ps[:])
                    P.op(eng, cp, reads=[psb], writes=[yo_b])
                t0 = cb * 512 + tb * 128
                o = P.op("sp", lambda e, yo=yo, t0=t0: e.dma_start(out=out[t0:t0 + 128, :], in_=yo[:]),
                         reads=[yo_b], dma=True, chan=yo_ch)
                o.final = True
        A.reset(m)

    def ffn(self, pre):
        P, A, S, NCB = self.P, self.A, self.S, self.NCB
        nw_d = self.dram_in(pre + "_norm", [128, 8])
        wgu_d = self.dram_in(pre + "_w_gu", [NFC, 128, 2 * 8 * 128])
        wd_d = self.dram_in(pre + "_w_down", [2, 8, 128, 11 * 128])
        m0 = A.mark()
        nw = A.alloc([128, 8], F32, "nw")
        nw_b = P.buf("nw")
        P.op("sp", lambda e: e.dma_start(out=nw[:], in_=nw_d), writes=[nw_b], dma=True, chan=P.chan("nw"))
        xn = A.alloc([128, 8, S], BF16, "xn")
        xn_b = [P.buf(f"xn{cb}") for cb in range(NCB)]
        m1 = A.mark()
        rin = Ring(self, 2, [128, 8, 512], F32, "xin")
        sq = A.alloc([128, 8, 512], BF16, "sq")
        sq_b = P.buf("sq")
        rstd = A.alloc([128, 512], F32, "rstd")
        rstd_b = P.buf("rstd")
        for cb in range(NCB):
            xin, xin_b, xin_ch = rin.next()
            self.load_xcols(cb, xin, xin_b, xin_ch)
            self.norm_block(cb, xin, xin_b, sq, sq_b, rstd, rstd_b, nw, nw_b, 5,
                            lambda c, cb=cb: (xn[:, c, cb * 512:(cb + 1) * 512], xn_b[cb]))
        P.barrier()
        A.reset(m1)
        if DEBUG_STOP == 1:
            A.reset(m0)
            return
        h = A.alloc([128, 11, S], BF16, "h")
        h_b = [[P.buf(f"h{mi}_{cb}") for cb in range(NCB)] for mi in range(11)]
        rw = Ring(self, 3, [128, 2, 8, 128], BF16, "wgu", eng="pool")
        rsg = Ring(self, 2, [128, 512], F32, "sg", chan=False)
        rwd = Ring(self, 2, [128, 11, 128], BF16, "wd", eng="pool")
        rxr = Ring(self, 3, [128, 512], F32, "xres")
        rxo = Ring(self, 3, [128, 512], F32, "xo")
        pgu = 0
        pdn = 0
        for grp in range(2):
            for mi in range(11):
                mchunk = grp * 11 + mi
                w, w_b, w_ch = rw.next()
                P.op("pool", lambda e, w=w, mchunk=mchunk: e.dma_start(
                    out=w[:].rearrange("p a k j -> p (a k j)"), in_=wgu_d[mchunk]),
                    writes=[w_b], dma=True, chan=w_ch)
                for cb in range(NCB):
                    pa, pb = ((0, 1), (2, 3), (4, 5))[pgu % 3]
                    pgu += 1
                    cols = slice(cb * 512, (cb + 1) * 512)
                    self.mm_group(self.ps[pa][:], [(w[:, 0, kc, :], xn[:, kc, cols]) for kc in range(8)],
                                  [w_b, xn_b[cb]], self.psb[pa])
                    self.mm_group(self.ps[pb][:], [(w[:, 1, kc, :], xn[:, kc, cols]) for kc in range(8)],
                                  [w_b, xn_b[cb]], self.psb[pb])
                    sg, sg_b, _ = rsg.next()
                    P.op("act", lambda e, sg=sg, pa=pa: e.activation(out=sg[:], in_=self.ps[pa][:], func=AF.Silu),
                         reads=[self.psb[pa]], writes=[sg_b])
                    P.op("dve", lambda e, sg=sg, pb=pb, mi=mi, cols=cols: e.tensor_tensor(
                        out=h[:, mi, cols], in0=sg[:], in1=self.ps[pb][:], op=ALU.mult),
                        reads=[sg_b, self.psb[pb]], writes=[h_b[mi][cb]])
            if DEBUG_STOP == 2:
                continue
            for o in range(8):
                wd, wd_b, wd_ch = rwd.next()
                P.op("pool", lambda e, wd=wd, grp=grp, o=o: e.dma_start(
                    out=wd[:].rearrange("p f j -> p (f j)"), in_=wd_d[grp, o]),
                    writes=[wd_b], dma=True, chan=wd_ch)
                for cb in range(NCB):
                    cols = slice(cb * 512, (cb + 1) * 512)
                    xr, xr_b, xr_ch = rxr.next()
                    P.op("sp", lambda e, xr=xr, o=o, cols=cols: e.dma_start(
                        out=xr[:], in_=self.xT[o * 128:(o + 1) * 128, cols]),
                        reads=[self.xTb[o][cb]], writes=[xr_b], dma=True, chan=xr_ch)
                    pi = pdn % 6
                    pdn += 1
                    self.mm_group(self.ps[pi][:], [(wd[:, fc, :], h[:, fc, cols]) for fc in range(11)],
                                  [wd_b] + [h_b[fc][cb] for fc in range(11)], self.psb[pi])
                    xo, xo_b, xo_ch = rxo.next()
                    P.op("dve", lambda e, xo=xo, xr=xr, pi=pi: e.scalar_tensor_tensor(
                        out=xo[:], in0=self.ps[pi][:], scalar=0.5, in1=xr[:], op0=ALU.mult, op1=ALU.add),
                        reads=[self.psb[pi], xr_b], writes=[xo_b])
                    if DEBUG_STOP == 3:
                        continue
                    P.op("sp", lambda e, xo=xo, o=o, cols=cols: e.dma_start(
                        out=self.xT[o * 128:(o + 1) * 128, cols], in_=xo[:]),
                        reads=[xo_b], writes=[self.xTb[o][cb]], dma=True, chan=xo_ch)
        P.barrier()
        A.reset(m0)


def lay_vec8(v):
    return np.ascontiguousarray(np.asarray(v, np.float32).reshape(8, 128).T)


def lay_wgu(w):
    w = np.asarray(w, np.float32)
    g = w[:, :DFF].reshape(8, 128, NFC, 128)
    u = w[:, DFF:].reshape(8, 128, NFC, 128)
    a = np.stack([g, u], axis=0)
    a = a.transpose(3, 2, 0, 1, 4)
    return np.ascontiguousarray(a).reshape(NFC, 128, 2 * 8 * 128)


def lay_wdown(w):
    w = np.asarray(w, np.float32)
    a = w.reshape(2, 11, 128, 8, 128)
    a = a.transpose(0, 3, 2, 1, 4)
    return np.ascontiguousarray(a).reshape(2, 8, 128, 11 * 128)


def build(S, plan):
    k = K(S)
    k.load_consts()
    k.stage_in()
    for item in plan:
        if item[0] == "ffn":
            k.ffn(item[1])
        elif item[0] == "mla":
            k.mla(item[1])
        elif item[0] == "gla":
            k.gla(item[1])
        elif item[0] == "ssd":
            k.ssd(item[1])
        else:
            raise ValueError(item)
    k.stage_out()
    k.stats = k.P.emit()
    return k


PLAN = [
    ("ffn", "l0_ffn1"), ("gla", "l0"), ("ffn", "l0_ffn2"),
    ("ffn", "l1_ffn1"), ("ssd", "l1"), ("ffn", "l1_ffn2"),
    ("ffn", "l2_ffn1"), ("mla", "l2"), ("ffn", "l2_ffn2"),
    ("ffn", "l3_ffn1"), ("gla", "l3"), ("ffn", "l3_ffn2"),
]


def kernel(**inputs):
    S = 4096
    k = build(S, PLAN)
    shared = {}
    in_maps = []
    x = np.asarray(inputs["x"], np.float32)
    pos = np.asarray(inputs["positions"], np.int32)
    for b in range(8):
        m = {}
        for name in k.inputs:
            if name == "x":
                m[name] = np.ascontiguousarray(x[b])
            elif name == "positions64":
                m[name] = np.ascontiguousarray(np.broadcast_to(pos[b][None, :], (64, S)))
            else:
                if name not in shared:
                    shared[name] = host_inputs_sub(k, inputs, name)
                m[name] = shared[name]
        in_maps.append(m)
    res = run_bass_kernel_spmd(k.nc, in_maps, core_ids=list(range(8)))
    return np.stack([np.asarray(r["out"], np.float32) for r in res.results], axis=0)


def host_inputs_sub(k, inputs, name):
    if name == "c_ident":
        return np.eye(128, dtype=np.float32)
    c = const_tables(name)
    if c is not None:
        return c
    c = gla_consts(name)
    if c is not None:
        return c
    c = ssd_consts(name)
    if c is not None:
        return c
    if "_ssd_" in name or name == "l1_mix_norm":
        return ssd_host(inputs, name[:2], name)
    if "_mla_" in name or name == "l2_mix_norm":
        return mla_host(inputs, name[:2], name, k.S)
    if "_gla_" in name or name in ("l0_mix_norm", "l3_mix_norm"):
        return gla_host(inputs, name[:2], name)
    if name.endswith("_w_gu"):
        return lay_wgu(inputs[name])
    if name.endswith("_w_down"):
        return lay_wdown(inputs[name])
    if name.endswith("ffn1_norm") or name.endswith("ffn2_norm") or name == "final_norm":
        return lay_vec8(inputs[name])
    raise KeyError(name)


MLA_SC = float(192 ** -0.5)
NEG = -1.0e30


def _mla(self, pre):
    P, A, S, NCB = self.P, self.A, self.S, self.NCB
    NB = S // 128
    nc = self.nc
    nw_d = self.dram_in(pre + "_mix_norm", [128, 8])
    win_d = self.dram_in(pre + "_mla_w_in", [128, 8 * 768])
    qnw_d = self.dram_in(pre + "_mla_q_norm", [128, 3])
    kvnw_d = self.dram_in(pre + "_mla_kv_norm", [128, 2])
    wuq_d = self.dram_in(pre + "_mla_w_uq", [128, 3 * 8 * 256])
    wk_d = self.dram_in(pre + "_mla_w_uk", [128, 2 * 1024])
    wv_d = self.dram_in(pre + "_mla_w_uv", [128, 2 * 1024])
    wo_d = self.dram_in(pre + "_mla_w_out", [128, 8 * 1024])
    pos_d = self.dram_in("positions64", [64, S], I32)
    rt_d = self.dram_in("c_rope", [64, 4])
    mask_d = self.dram_in("c_mask", [128, 4 * 512])
    Qn = nc.dram_tensor("mla_qn", [8, 128, S], BF16, kind="Internal").ap()
    Qr = nc.dram_tensor("mla_qr", [8, 64, S], BF16, kind="Internal").ap()
    Kn = nc.dram_tensor("mla_kn", [8, 128, S], BF16, kind="Internal").ap()
    Kr = nc.dram_tensor("mla_kr", [64, S], BF16, kind="Internal").ap()
    V = nc.dram_tensor("mla_v", [8, 128, NB, 128], BF16, kind="Internal").ap()
    m0 = A.mark()

    def ld(dst, src, eng="sp", name="w"):
        b = P.buf(name)
        P.op(eng, lambda e: e.dma_start(out=dst, in_=src), writes=[b], dma=True, chan=P.chan(name, eng))
        return b

    nw = A.alloc([128, 8], F32, "nw")
    nw_b = ld(nw[:], nw_d, name="nw")
    qnw = A.alloc([128, 3], F32, "qnw")
    qnw_b = ld(qnw[:], qnw_d, name="qnw")
    kvnw = A.alloc([128, 2], F32, "kvnw")
    kvnw_b = ld(kvnw[:], kvnw_d, name="kvnw")
    rt = A.alloc([64, 4], F32, "rt")
    rt_b = ld(rt[:], rt_d, name="rt")
    m1 = A.mark()
    win = A.alloc([128, 8, 768], BF16, "win")
    win_b = ld(win[:].rearrange("p k n -> p (k n)"), win_d, "pool", "win")
    wuq = A.alloc([128, 3, 8, 256], BF16, "wuq")
    wuq_b = ld(wuq[:].rearrange("p k h n -> p (k h n)"), wuq_d, "pool", "wuq")
    wk = A.alloc([128, 2, 1024], BF16, "wk")
    wk_b = ld(wk[:].rearrange("p k n -> p (k n)"), wk_d, "pool", "wk")
    wv = A.alloc([128, 2, 1024], BF16, "wv")
    wv_b = ld(wv[:].rearrange("p k n -> p (k n)"), wv_d, "pool", "wv")
    rin = Ring(self, 2, [128, 8, 512], F32, "xin")
    sq = A.alloc([128, 8, 512], BF16, "sq")
    sq_b = P.buf("sq")
    rstd = A.alloc([128, 512], F32, "rstd")
    rstd_b = P.buf("rstd")
    xn = A.alloc([128, 8, 512], BF16, "xn")
    xn_b = P.buf("xn")
    latf = A.alloc([128, 3, 512], F32, "latf")
    latf_b = [P.buf(f"latf{c}") for c in range(3)]
    latsq = A.alloc([128, 3, 512], BF16, "latsq")
    latsq_b = [P.buf(f"latsq{c}") for c in range(3)]
    lrstd = A.alloc([128, 512], F32, "lrstd")
    lrstd_b = P.buf("lrstd")
    cqn = A.alloc([128, 3, 512], BF16, "cqn")
    cqn_b = P.buf("cqn")
    ckvn = A.alloc([128, 2, 512], BF16, "ckvn")
    ckvn_b = P.buf("ckvn")
    posi = A.alloc([64, 512], I32, "posi")
    posi_b = P.buf("posi")
    posi_ch = P.chan("posi")
    ang = A.alloc([64, 512], F32, "ang")
    ang_b = P.buf("ang")
    kf = A.alloc([64, 512], F32, "kf")
    ki = A.alloc([64, 512], I32, "ki")
    kf_b = P.buf("kf")
    arg = A.alloc([64, 512], F32, "arg")
    arg_b = P.buf("arg")
    cos2 = A.alloc([64, 512], F32, "cos2")
    cos2_b = P.buf("cos2")
    sin2 = A.alloc([64, 512], F32, "sin2")
    sin2_b = P.buf("sin2")
    t1 = A.alloc([64, 512], F32, "t1")
    t1_b = P.buf("t1")
    t2 = A.alloc([64, 512], F32, "t2")
    t2_b = P.buf("t2")
    rkr = Ring(self, 2, [64, 512], BF16, "krst")
    rqn = Ring(self, 2, [128, 8, 512], BF16, "qnst")
    rqr = Ring(self, 2, [64, 8, 512], BF16, "qrst")
    rkn = Ring(self, 2, [128, 8, 512], BF16, "knst")
    rvs = Ring(self, 2, [128, 1024], BF16, "vst")
    TWO_PI = float(2 * np.pi)
    bank = [0]

    def nb():
        bank[0] = (bank[0] + 1) % 4
        return bank[0]

    def range_reduce(shift):
        P.op("dve", lambda e: e.tensor_scalar(arg[:], ang[:], float(shift), None, ALU.add),
             reads=[ang_b], writes=[arg_b])
        P.op("dve", lambda e: e.tensor_scalar(kf[:], arg[:], float(1.0 / TWO_PI), None, ALU.mult),
             reads=[arg_b], writes=[kf_b])
        P.op("dve", lambda e: e.tensor_copy(out=ki[:], in_=kf[:]), reads=[kf_b], writes=[kf_b])
        P.op("dve", lambda e: e.tensor_copy(out=kf[:], in_=ki[:]), reads=[kf_b], writes=[kf_b])
        P.op("dve", lambda e: e.scalar_tensor_tensor(out=arg[:], in0=kf[:], scalar=-TWO_PI, in1=arg[:],
                                                     op0=ALU.mult, op1=ALU.add),
             reads=[arg_b, kf_b], writes=[arg_b])
        P.op("dve", lambda e: e.tensor_scalar(kf[:], arg[:], float(np.pi), -TWO_PI, ALU.is_gt, ALU.mult),
             reads=[arg_b], writes=[kf_b])
        P.op("dve", lambda e: e.tensor_tensor(out=arg[:], in0=arg[:], in1=kf[:], op=ALU.add),
             reads=[arg_b, kf_b], writes=[arg_b])
        P.op("dve", lambda e: e.tensor_scalar(kf[:], arg[:], float(-np.pi), TWO_PI, ALU.is_lt, ALU.mult),
             reads=[arg_b], writes=[kf_b])
        P.op("dve", lambda e: e.tensor_tensor(out=arg[:], in0=arg[:], in1=kf[:], op=ALU.add),
             reads=[arg_b, kf_b], writes=[arg_b])

    def lat_norm(nch, col0, nwt, nwt_b, dst, dst_b, dim):
        for c in range(nch):
            bk = nb()
            self.mm_group(self.ps[bk][:], [(win[:, kc, col0 + c * 128:col0 + (c + 1) * 128], xn[:, kc, :])
                                           for kc in range(8)], [win_b, xn_b], self.psb[bk])
            P.op("act", lambda e, c=c, bk=bk: e.activation(out=latf[:, c, :], in_=self.ps[bk][:], func=AF.Copy),
                 reads=[self.psb[bk]], writes=[latf_b[c]])
            P.op("act", lambda e, c=c, bk=bk: e.activation(out=latsq[:, c, :], in_=self.ps[bk][:], func=AF.Square),
                 reads=[self.psb[bk]], writes=[latsq_b[c]])
        self.mm_group(self.ps[4][:], [(self.c_onesb[:], latsq[:, c, :]) for c in range(nch)],
                      [latsq_b[c] for c in range(nch)] + [self.cb_const], self.psb[4])
        P.op("act", lambda e: e.activation(out=lrstd[:], in_=self.ps[4][:], func=AF.Sqrt, bias=self.c_eps[:],
                                           scale=1.0 / dim), reads=[self.psb[4], self.cb_const], writes=[lrstd_b])
        P.op("dve", lambda e: e.reciprocal(out=lrstd[:], in_=lrstd[:]), reads=[lrstd_b], writes=[lrstd_b])
        for c in range(nch):
            P.op("dve", lambda e, c=c: e.scalar_tensor_tensor(out=dst[:, c, :], in0=latf[:, c, :],
                                                             scalar=nwt[:, c:c + 1], in1=lrstd[:],
                                                             op0=ALU.mult, op1=ALU.mult),
                 reads=[latf_b[c], lrstd_b, nwt_b], writes=[dst_b])

    def rope_combine(bk_a, bk_b, scale, out_ap, out_b):
        P.op("dve", lambda e: e.scalar_tensor_tensor(out=t1[:], in0=self.ps[bk_a][0:64, :], scalar=float(scale),
                                                     in1=cos2[:], op0=ALU.mult, op1=ALU.mult),
             reads=[self.psb[bk_a], cos2_b], writes=[t1_b])
        P.op("dve", lambda e: e.scalar_tensor_tensor(out=t2[:], in0=self.ps[bk_b][0:64, :], scalar=float(scale),
                                                     in1=sin2[:], op0=ALU.mult, op1=ALU.mult),
             reads=[self.psb[bk_b], sin2_b], writes=[t2_b])
        P.op("dve", lambda e: e.tensor_tensor(out=out_ap, in0=t1[:], in1=t2[:], op=ALU.add),
             reads=[t1_b, t2_b], writes=[out_b])

    for cb in range(NCB):
        cols = slice(cb * 512, (cb + 1) * 512)
        xin, xin_b, xin_ch = rin.next()
        self.load_xcols(cb, xin, xin_b, xin_ch)
        self.norm_block(cb, xin, xin_b, sq, sq_b, rstd, rstd_b, nw, nw_b, 5, lambda c: (xn[:, c, :], xn_b))
        lat_norm(3, 0, qnw, qnw_b, cqn, cqn_b, 384)
        lat_norm(2, 384, kvnw, kvnw_b, ckvn, ckvn_b, 256)
        P.op("sp", lambda e, cols=cols: e.dma_start(out=posi[:], in_=pos_d[:, cols]), writes=[posi_b], dma=True,
             chan=posi_ch)
        P.op("dve", lambda e: e.tensor_copy(out=ang[:], in_=posi[:]), reads=[posi_b], writes=[ang_b])
        P.op("dve", lambda e: e.tensor_scalar(ang[:], ang[:], rt[:, 0:1], None, ALU.mult),
             reads=[ang_b, rt_b], writes=[ang_b])
        range_reduce(np.pi / 2)
        P.op("act", lambda e: e.activation(out=cos2[:], in_=arg[:], func=AF.Sin), reads=[arg_b], writes=[cos2_b])
        range_reduce(0.0)
        P.op("act", lambda e: e.activation(out=sin2[:], in_=arg[:], func=AF.Sin, scale=rt[:, 1:2]),
             reads=[arg_b, rt_b], writes=[sin2_b])
        ba, bb = nb(), nb()
        self.mm_group(self.ps[ba][0:64, :], [(win[:, kc, 640:704], xn[:, kc, :]) for kc in range(8)],
                      [win_b, xn_b], self.psb[ba])
        self.mm_group(self.ps[bb][0:64, :], [(win[:, kc, 704:768], xn[:, kc, :]) for kc in range(8)],
                      [win_b, xn_b], self.psb[bb])
        krs, krs_b, krs_ch = rkr.next()
        rope_combine(ba, bb, 1.0, krs[:], krs_b)
        P.op("sp", lambda e, krs=krs, cols=cols: e.dma_start(out=Kr[:, cols], in_=krs[:]), reads=[krs_b],
             dma=True, chan=krs_ch)
        qns, qns_b, qns_ch = rqn.next()
        qrs, qrs_b, qrs_ch = rqr.next()
        kns, kns_b, kns_ch = rkn.next()
        for h in range(8):
            bk = nb()
            self.mm_group(self.ps[bk][:], [(wuq[:, kc, h, 0:128], cqn[:, kc, :]) for kc in range(3)],
                          [wuq_b, cqn_b], self.psb[bk])
            P.op("act", lambda e, h=h, bk=bk, qns=qns: e.activation(out=qns[:, h, :], in_=self.ps[bk][:],
                                                                  func=AF.Copy, scale=MLA_SC),
                 reads=[self.psb[bk]], writes=[qns_b])
            ba, bb = nb(), nb()
            self.mm_group(self.ps[ba][0:64, :], [(wuq[:, kc, h, 128:192], cqn[:, kc, :]) for kc in range(3)],
                          [wuq_b, cqn_b], self.psb[ba])
            self.mm_group(self.ps[bb][0:64, :], [(wuq[:, kc, h, 192:256], cqn[:, kc, :]) for kc in range(3)],
                          [wuq_b, cqn_b], self.psb[bb])
            rope_combine(ba, bb, MLA_SC, qrs[:, h, :], qrs_b)
            bk = nb()
            self.mm_group(self.ps[bk][:], [(wk[:, kc, h * 128:(h + 1) * 128], ckvn[:, kc, :]) for kc in range(2)],
                          [wk_b, ckvn_b], self.psb[bk])
            P.op("act", lambda e, h=h, bk=bk, kns=kns: e.activation(out=kns[:, h, :], in_=self.ps[bk][:],
                                                                  func=AF.Copy),
                 reads=[self.psb[bk]], writes=[kns_b])
        P.op("sp", lambda e, qns=qns, cols=cols: e.dma_start(out=Qn.rearrange("h p s -> p h s")[:, :, cols],
                                                            in_=qns[:]), reads=[qns_b], dma=True, chan=qns_ch)
        P.op("sp", lambda e, qrs=qrs, cols=cols: e.dma_start(out=Qr.rearrange("h p s -> p h s")[:, :, cols],
                                                            in_=qrs[:]), reads=[qrs_b], dma=True, chan=qrs_ch)
        P.op("sp", lambda e, kns=kns, cols=cols: e.dma_start(out=Kn.rearrange("h p s -> p h s")[:, :, cols],
                                                            in_=kns[:]), reads=[kns_b], dma=True, chan=kns_ch)
        for tb in range(4):
            vs, vs_b, vs_ch = rvs.next()
            for half in range(2):
                bk = nb()
                self.mm_group(self.ps[bk][:], [(ckvn[:, kc, tb * 128:(tb + 1) * 128],
                                                wv[:, kc, half * 512:(half + 1) * 512]) for kc in range(2)],
                              [wv_b, ckvn_b], self.psb[bk])
                if half == 0:
                    P.op("act", lambda e, vs=vs, bk=bk: e.activation(out=vs[:, 0:512], in_=self.ps[bk][:],
                                                                   func=AF.Copy),
                         reads=[self.psb[bk]], writes=[vs_b])
                else:
                    P.op("dve", lambda e, vs=vs, bk=bk: e.tensor_copy(out=vs[:, 512:1024], in_=self.ps[bk][:]),
                         reads=[self.psb[bk]], writes=[vs_b])
            blk = cb * 4 + tb
            P.op("sp", lambda e, vs=vs, blk=blk: e.dma_start(
                out=V.rearrange("h p b v -> p h b v")[:, :, blk, :], in_=vs[:].rearrange("p (h v) -> p h v", h=8)),
                reads=[vs_b], dma=True, chan=vs_ch)
    P.barrier()
    A.reset(m1)

    wo = A.alloc([128, 8, 1024], BF16, "wo")
    wo_b = ld(wo[:].rearrange("p h n -> p (h n)"), wo_d, "pool", "wo")
    mask = A.alloc([128, 4, 512], F32, "mask")
    mask_b = ld(mask[:].rearrange("p t n -> p (t n)"), mask_d, "sp", "mask")
    kr = A.alloc([64, S], BF16, "kr")
    kr_b = ld(kr[:], Kr, "sp", "kr")
    rK = Ring(self, 3, [128, S], BF16, "kh")
    rV = Ring(self, 3, [128, NB, 128], BF16, "vh")
    rQn = Ring(self, 2, [128, 8, 512], BF16, "qn")
    rQr = Ring(self, 2, [64, 8, 512], BF16, "qr")
    NPM = 1
    Pms = [A.alloc([128, 4, S], BF16, "Pm") for _ in range(NPM)]
    Pms_b = [[P.buf(f"Pm{i}{t}") for t in range(4)] for i in range(NPM)]
    rPT = Ring(self, 3, [128, 1024], BF16, "PT", chan=False)
    oT = A.alloc([128, 8, 512], BF16, "oT")
    oT_b = [P.buf(f"oT{h}") for h in range(8)]
    mxs = [A.alloc([128, 4, 8], F32, "mx") for _ in range(2)]
    mxs_b = [[[P.buf(f"mx{i}{t}{c}") for c in range(8)] for t in range(4)] for i in range(2)]
    negms = [A.alloc([128, 4], F32, "negm") for _ in range(2)]
    negms_b = [P.buf(f"negm{i}") for i in range(2)]
    rss = [A.alloc([128, 4, 8], F32, "rs") for _ in range(2)]
    rss_b = [[[P.buf(f"rs{i}{t}{c}") for c in range(8)] for t in range(4)] for i in range(2)]
    rsums = [A.alloc([128, 4], F32, "rsum") for _ in range(2)]
    rsums_b = [P.buf(f"rsum{i}") for i in range(2)]
    diag = A.alloc([128, 4, 128], F32, "diag")
    diag_b = P.buf("diag")
    onesf = A.alloc([128, 128], F32, "onesf")
    onesf_b = P.buf("onesf")
    P.op("dve", lambda e: e.memset(onesf[:], 1.0), writes=[onesf_b])
    rbs = A.alloc([128, 512], F32, "rbs")
    rbs_b = P.buf("rbs")
    rdt = Ring(self, 2, [128, 512], F32, "dtmp", chan=False)
    rxr = Ring(self, 2, [128, 512], F32, "xres")
    rxo = Ring(self, 2, [128, 512], F32, "xo")
    cp_i = [0]
    qbufs = {}

    class Item:
        pass

    items = []
    for qb in range(NCB):
        for h in range(8):
            it = Item()
            it.qb, it.h, it.idx = qb, h, len(items)
            it.nk = (qb + 1) * 512
            it.nch = qb + 1
            it.nkb = it.nk // 128
            items.append(it)

    def start_item(it):
        qb, h = it.qb, it.h
        cols = slice(qb * 512, (qb + 1) * 512)
        if h == 0:
            qn, qn_b, qn_ch = rQn.next()
            qr, qr_b, qr_ch = rQr.next()
            P.op("sp", lambda e: e.dma_start(out=qn[:], in_=Qn.rearrange("h p s -> p h s")[:, :, cols]),
                 writes=[qn_b], dma=True, chan=qn_ch)
            P.op("sp", lambda e: e.dma_start(out=qr[:], in_=Qr.rearrange("h p s -> p h s")[:, :, cols]),
                 writes=[qr_b], dma=True, chan=qr_ch)
            qbufs[qb] = (qn, qn_b, qr, qr_b)
        it.qn, it.qn_b, it.qr, it.qr_b = qbufs[qb]
        it.kh, it.kh_b, kh_ch = rK.next()
        it.vh, it.vh_b, vh_ch = rV.next()
        nk, nkb = it.nk, it.nkb
        P.op("sp", lambda e: e.dma_start(out=it.kh[:, 0:nk], in_=Kn[h][:, 0:nk]), writes=[it.kh_b], dma=True,
             chan=kh_ch)
        P.op("sp", lambda e: e.dma_start(out=it.vh[:, 0:nkb, :], in_=V[h][:, 0:nkb, :]), writes=[it.vh_b], dma=True,
             chan=vh_ch)
        it.par = it.idx % 2
        it.pm = it.idx % NPM

    def qk(it, t, kc, bk):
        tc_ = slice(t * 128, (t + 1) * 128)
        kcs = slice(kc * 512, (kc + 1) * 512)
        self.mm_group(self.ps[bk][:], [(it.qn[:, it.h, tc_], it.kh[:, kcs]), (it.qr[:, it.h, tc_], kr[:, kcs])],
                      [it.qn_b, it.qr_b, it.kh_b, kr_b], self.psb[bk])

    def s1_chunk(it, t, kc):
        mx, mx_b = mxs[it.par], mxs_b[it.par]
        bk = nb()
        qk(it, t, kc, bk)
        P.op("dve", lambda e: e.tensor_reduce(out=mx[:, t, kc:kc + 1], in_=self.ps[bk][:], axis=AX.X, op=ALU.max),
             reads=[self.psb[bk]], writes=[mx_b[t][kc]])

    def s1_end(it):
        mx, mx_b, negm, negm_b = mxs[it.par], mxs_b[it.par], negms[it.par], negms_b[it.par]
        rs, rs_b = rss[it.par], rss_b[it.par]
        nch = it.nch
        P.op("dve", lambda e: e.tensor_reduce(out=negm[:], in_=mx[:, :, 0:nch], axis=AX.X, op=ALU.max),
             reads=[mx_b[t][c] for t in range(4) for c in range(nch)], writes=[negm_b])
        P.op("dve", lambda e: e.tensor_scalar(negm[:], negm[:], -1.0, None, ALU.mult), reads=[negm_b], writes=[negm_b])
        P.op("dve", lambda e: e.memset(rs[:], 0.0), writes=[rs_b[t][c] for t in range(4) for c in range(8)])

    def s2_chunk(it, t, kc):
        negm, negm_b = negms[it.par], negms_b[it.par]
        rs, rs_b = rss[it.par], rss_b[it.par]
        Pm, Pm_b = Pms[it.pm], Pms_b[it.pm]
        bk = nb()
        qk(it, t, kc, bk)
        kcs = slice(kc * 512, (kc + 1) * 512)
        if kc == it.nch - 1:
            dt_, dt_b, _ = rdt.next()
            P.op("dve", lambda e: e.tensor_tensor(out=dt_[:], in0=self.ps[bk][:], in1=mask[:, t, :], op=ALU.add),
                 reads=[self.psb[bk], mask_b], writes=[dt_b])
            P.op("act", lambda e: e.activation(out=Pm[:, t, kcs], in_=dt_[:], func=AF.Exp, bias=negm[:, t:t + 1],
                                               scale=1.0, accum_out=rs[:, t, kc:kc + 1]),
                 reads=[dt_b, negm_b], writes=[Pm_b[t], rs_b[t][kc]])
        else:
            P.op("act", lambda e: e.activation(out=Pm[:, t, kcs], in_=self.ps[bk][:], func=AF.Exp,
                                               bias=negm[:, t:t + 1], scale=1.0, accum_out=rs[:, t, kc:kc + 1]),
                 reads=[self.psb[bk], negm_b], writes=[Pm_b[t], rs_b[t][kc]])

    def s2_end(it):
        rs, rs_b, rsum, rsum_b = rss[it.par], rss_b[it.par], rsums[it.par], rsums_b[it.par]
        nch = it.nch
        P.op("dve", lambda e: e.tensor_reduce(out=rsum[:], in_=rs[:, :, 0:nch], axis=AX.X, op=ALU.add),
             reads=[rs_b[t][c] for t in range(4) for c in range(nch)], writes=[rsum_b])
        P.op("dve", lambda e: e.reciprocal(out=rsum[:], in_=rsum[:]), reads=[rsum_b], writes=[rsum_b])

    def s3(it):
        Pm, Pm_b = Pms[it.pm], Pms_b[it.pm]
        rsum, rsum_b = rsums[it.par], rsums_b[it.par]
        vh, vh_b, h = it.vh, it.vh_b, it.h
        npair = it.nkb // 2
        pend = None

        def emit_tr(jp):
            pst, pst_b = self.pst[jp % 2], self.pstb[jp % 2]

            def tr(e):
                ins = None
                for jl in range(2):
                    for t in range(4):
                        j = jp * 2 + jl
                        ins = e.transpose(pst[:, (jl * 4 + t) * 128:(jl * 4 + t + 1) * 128],
                                          Pm[:, t, j * 128:(j + 1) * 128], self.c_identb[:])
                return ins
            P.op("pe", tr, reads=Pm_b + [self.cb_const], writes=[pst_b])
            pt, pt_b, _ = rPT.next()
            cp_i[0] += 1
            if cp_i[0] % 2 == 0:
                P.op("act", lambda e: e.activation(out=pt[:], in_=pst[:], func=AF.Copy), reads=[pst_b], writes=[pt_b])
            else:
                P.op("dve", lambda e: e.tensor_copy(out=pt[:], in_=pst[:]), reads=[pst_b], writes=[pt_b])
            return pt, pt_b

        def emit_pv(jp, pt, pt_b):
            def pv(e):
                ins = None
                for jl in range(2):
                    j = jp * 2 + jl
                    ins = e.matmul(self.ps[4][:], vh[:, j, :], pt[:, jl * 512:(jl + 1) * 512],
                                   start=(jp == 0 and jl == 0), stop=(jp == npair - 1 and jl == 1))
                return ins
            P.op("pe", pv, reads=[pt_b, vh_b], writes=[self.psb[4]])

        for jp in range(npair):
            cur = emit_tr(jp)
            if pend is not None:
                emit_pv(jp - 1, *pend)
            pend = cur
        emit_pv(npair - 1, *pend)
        for t in range(4):
            P.op("dve", lambda e, t=t: e.tensor_scalar(diag[:, t, :], self.c_identf[:], rsum[:, t:t + 1], None, ALU.mult),
                 reads=[rsum_b, self.cb_const], writes=[diag_b])
        self.mm_group(self.ps[5][:], [(onesf[:], diag[:].rearrange("p t n -> p (t n)"))], [onesf_b, diag_b],
                      self.psb[5])
        P.op("act", lambda e: e.activation(out=rbs[:], in_=self.ps[5][:], func=AF.Copy), reads=[self.psb[5]],
             writes=[rbs_b])
        P.op("dve", lambda e: e.tensor_tensor(out=oT[:, h, :], in0=self.ps[4][:], in1=rbs[:], op=ALU.mult),
             reads=[self.psb[4], rbs_b], writes=[oT_b[h]])
        if h == 7:
            qb = it.qb
            cols = slice(qb * 512, (qb + 1) * 512)
            for o in range(8):
                bk = nb()
                self.mm_group(self.ps[bk][:], [(wo[:, hh, o * 128:(o + 1) * 128], oT[:, hh, :]) for hh in range(8)],
                              [wo_b] + oT_b, self.psb[bk])
                xr, xr_b, xr_ch = rxr.next()
                P.op("sp", lambda e, xr=xr, o=o: e.dma_start(out=xr[:], in_=self.xT[o * 128:(o + 1) * 128, cols]),
                     reads=[self.xTb[o][qb]], writes=[xr_b], dma=True, chan=xr_ch)
                xo, xo_b, xo_ch = rxo.next()
                P.op("dve", lambda e, xo=xo, xr=xr, bk=bk: e.tensor_tensor(out=xo[:], in0=self.ps[bk][:], in1=xr[:],
                                                                         op=ALU.add),
                     reads=[self.psb[bk], xr_b], writes=[xo_b])
                P.op("sp", lambda e, xo=xo, o=o: e.dma_start(out=self.xT[o * 128:(o + 1) * 128, cols], in_=xo[:]),
                     reads=[xo_b], writes=[self.xTb[o][qb]], dma=True, chan=xo_ch)

    n = len(items)
    for i in range(n + 2):
        a = items[i] if i < n else None
        b = items[i - 1] if 0 <= i - 1 < n else None
        c = items[i - 2] if 0 <= i - 2 < n else None
        if a is not None:
            start_item(a)
        if c is not None:
            s3(c)
        la = [(t, kc) for t in range(4) for kc in range(a.nch)] if a is not None else []
        lb = [(t, kc) for t in range(4) for kc in range(b.nch)] if b is not None else []
        for j in range(max(len(la), len(lb))):
            if j < len(la):
                s1_chunk(a, *la[j])
            if j < len(lb):
                s2_chunk(b, *lb[j])
        if a is not None:
            s1_end(a)
        if b is not None:
            s2_end(b)
    P.barrier()
    A.reset(m0)


K.mla = _mla


def lay_cols(w, p=128):
    w = np.asarray(w, np.float32)
    kc = w.shape[0] // p
    return np.ascontiguousarray(w.reshape(kc, p, w.shape[1]).transpose(1, 0, 2)).reshape(p, kc * w.shape[1])


def lay_vecp(v, p=128):
    v = np.asarray(v, np.float32)
    return np.ascontiguousarray(v.reshape(-1, p).T)


def mla_host(inputs, pre, name, S):
    if name == pre + "_mla_w_in":
        w = np.asarray(inputs[name], np.float32)
        kr = w[:, 640:704]
        w2 = np.concatenate([w, kr[:, 32:64], kr[:, 0:32]], axis=1)
        return lay_cols(w2)
    if name == pre + "_mla_w_uq":
        w = np.asarray(inputs[name], np.float32).reshape(384, 8, 192)
        w2 = np.concatenate([w, w[:, :, 160:192], w[:, :, 128:160]], axis=2)
        return lay_cols(w2.reshape(384, 8 * 256))
    if name == pre + "_mla_w_uk":
        w = np.asarray(inputs[pre + "_mla_w_ukv"], np.float32).reshape(256, 8, 256)
        return lay_cols(np.ascontiguousarray(w[:, :, 0:128]).reshape(256, 1024))
    if name == pre + "_mla_w_uv":
        w = np.asarray(inputs[pre + "_mla_w_ukv"], np.float32).reshape(256, 8, 256)
        return lay_cols(np.ascontiguousarray(w[:, :, 128:256]).reshape(256, 1024))
    if name == pre + "_mla_w_out":
        return lay_cols(inputs[name])
    if name in (pre + "_mla_q_norm", pre + "_mla_kv_norm", pre + "_mix_norm"):
        return lay_vecp(inputs[name])
    return None


def const_tables(name):
    if name == "c_rope":
        inv = (1.0 / (10000.0 ** (np.arange(0, 64, 2, dtype=np.float32) / 64))).astype(np.float32)
        inv2 = np.concatenate([inv, inv])
        sgn = np.concatenate([-np.ones(32), np.ones(32)])
        return np.stack([inv2, sgn, -np.pi * sgn, -np.pi * np.ones(64)], 1).astype(np.float32)
    if name == "c_mask":
        q = np.arange(128)[:, None, None]
        t = np.arange(4)[None, :, None]
        c = np.arange(512)[None, None, :]
        m = np.where(c <= t * 128 + q, 0.0, NEG).astype(np.float32)
        return np.ascontiguousarray(m).reshape(128, 4 * 512)
    return None


def _gla(self, pre):
    P, A, S, NCB = self.P, self.A, self.S, self.NCB
    nw_d = self.dram_in(pre + "_mix_norm", [128, 8])
    win_d = self.dram_in(pre + "_gla_w_in", [128, 8 * 3088])
    wgb_d = self.dram_in(pre + "_gla_w_gate_b", [16, 512])
    bg_d = self.dram_in(pre + "_gla_b_gate", [1, 512])
    gnw_d = self.dram_in(pre + "_gla_norm", [128, 1024])
    wo_d = self.dram_in(pre + "_gla_w_out", [128, 8 * 1024])
    cum_d = self.dram_in("c_gla_cum", [128, 3 * 128])
    tri4_d = self.dram_in("c_tri4", [128, 512])
    m0 = A.mark()

    def ld(dst, src, eng="sp", name="w"):
        b = P.buf(name)
        P.op(eng, lambda e: e.dma_start(out=dst, in_=src), writes=[b], dma=True, chan=P.chan(name, eng))
        return b

    nw = A.alloc([128, 8], F32, "nw")
    nw_b = ld(nw[:], nw_d, name="nw")
    win = A.alloc([128, 8, 3088], BF16, "gwin")
    win_b = ld(win[:].rearrange("p k n -> p (k n)"), win_d, "pool", "gwin")
    wo = A.alloc([128, 8, 1024], BF16, "gwo")
    wo_b = ld(wo[:].rearrange("p k n -> p (k n)"), wo_d, "pool", "gwo")
    wgb = A.alloc([16, 512], BF16, "wgb")
    wgb_b = ld(wgb[:], wgb_d, "pool", "wgb")
    bg = A.alloc([1, 512], BF16, "bg")
    bg_b = ld(bg[:], bg_d, "pool", "bg")
    gnw = A.alloc([128, 1024], F32, "gnw")
    gnw_b = ld(gnw[:], gnw_d, "sp", "gnw")
    cum = A.alloc([128, 3, 128], F32, "cum")
    cum_b = ld(cum[:].rearrange("p a n -> p (a n)"), cum_d, "sp", "cum")
    tri4 = A.alloc([128, 4, 128], F32, "tri4")
    tri4_b = ld(tri4[:].rearrange("p a n -> p (a n)"), tri4_d, "sp", "tri4")
    ones1 = A.alloc([1, 128], BF16, "ones1")
    ones1_b = P.buf("ones1")
    P.op("dve", lambda e: e.memset(ones1[:], 1.0), writes=[ones1_b])
    c1 = A.alloc([128, 1], F32, "c1")
    c1_b = P.buf("c1")
    P.op("dve", lambda e: e.memset(c1[:], 1.0), writes=[c1_b])

    rin = Ring(self, 2, [128, 8, 512], F32, "xin")
    sq = A.alloc([128, 8, 512], BF16, "sq")
    sq_b = P.buf("sq")
    rstd = A.alloc([128, 512], F32, "rstd")
    rstd_b = P.buf("rstd")
    xn = A.alloc([128, 8, 512], BF16, "xn")
    xn_b = P.buf("xn")
    qTf = A.alloc([128, 4, 512], F32, "qTf")
    qTf_b = P.buf("qTf")
    kTf = A.alloc([128, 4, 512], F32, "kTf")
    kTf_b = P.buf("kTf")
    alr = A.alloc([16, 512], BF16, "alr")
    alr_b = P.buf("alr")
    r_ktm = Ring(self, 2, [128, 512], F32, "ktm", chan=False)
    r_vsb = Ring(self, 2, [128, 1024], BF16, "vsb", chan=False)
    r_sgw = Ring(self, 2, [128, 1024], F32, "sgw", chan=False)
    r_sp = Ring(self, 2, [128, 512], F32, "sp", chan=False)
    dend = A.alloc([128, 512], F32, "dend")
    dend_b = P.buf("dend")
    kend = A.alloc([128, 512], BF16, "kend")
    kend_b = P.buf("kend")
    eb = A.alloc([128, 4, 128], F32, "eb")
    eb_b = P.buf("eb")
    enb = A.alloc([128, 4, 128], F32, "enb")
    enb_b = P.buf("enb")
    qdec = A.alloc([128, 4, 128], BF16, "qdec")
    qdec_b = P.buf("qdec")
    kinv = A.alloc([128, 4, 128], BF16, "kinv")
    kinv_b = P.buf("kinv")
    attnT = A.alloc([128, 4, 128], BF16, "attnT")
    attnT_b = P.buf("attnT")
    stf = A.alloc([128, 4, 256], F32, "stf")
    stf_b = [P.buf(f"stf{h}") for h in range(4)]
    stb = A.alloc([128, 4, 256], BF16, "stb")
    stb_b = [P.buf(f"stb{h}") for h in range(4)]
    junk = A.alloc([128, 256], F32, "junk")
    junk_b = P.buf("junk")
    ssq = A.alloc([128, 4], F32, "ssq")
    ssq_b = [P.buf(f"ssq{h}") for h in range(4)]
    orstd = A.alloc([128, 4], F32, "orstd")
    orstd_b = P.buf("orstd")
    og = A.alloc([128, 1024], BF16, "og")
    og_b = P.buf("og")
    ogT = A.alloc([128, 8, 512], BF16, "ogT")
    ogT_b = [P.buf(f"ogT{t}") for t in range(4)]
    rxr = Ring(self, 2, [128, 512], F32, "xres")
    rxo = Ring(self, 2, [128, 512], F32, "xo")
    for h in range(4):
        P.op("dve", lambda e, h=h: e.memset(stf[:, h, :], 0.0), writes=[stf_b[h]])
        P.op("pool", lambda e, h=h: e.memset(stb[:, h, :], 0.0), writes=[stb_b[h]])
    bank = [0]

    def nb():
        bank[0] = (bank[0] + 1) % 5
        return bank[0]
    SCQ = float(128 ** -0.5)

    for cb in range(NCB):
        cols = slice(cb * 512, (cb + 1) * 512)
        xin, xin_b, xin_ch = rin.next()
        self.load_xcols(cb, xin, xin_b, xin_ch)
        self.norm_block(cb, xin, xin_b, sq, sq_b, rstd, rstd_b, nw, nw_b, 5, lambda c: (xn[:, c, :], xn_b))
        for h in range(4):
            bk = nb()
            self.mm_group(self.ps[bk][:], [(win[:, kc, h * 128:(h + 1) * 128], xn[:, kc, :]) for kc in range(8)],
                          [win_b, xn_b], self.psb[bk])
            P.op("act", lambda e, h=h, bk=bk: e.activation(out=qTf[:, h, :], in_=self.ps[bk][:], func=AF.Copy,
                                                           scale=SCQ), reads=[self.psb[bk]], writes=[qTf_b])
            bk = nb()
            self.mm_group(self.ps[bk][:], [(win[:, kc, 512 + h * 128:512 + (h + 1) * 128], xn[:, kc, :])
                                           for kc in range(8)], [win_b, xn_b], self.psb[bk])
            P.op("dve", lambda e, h=h, bk=bk: e.tensor_copy(out=kTf[:, h, :], in_=self.ps[bk][:]),
                 reads=[self.psb[bk]], writes=[kTf_b])
        bk = nb()
        self.mm_group(self.ps[bk][0:16, :], [(win[:, kc, 3072:3088], xn[:, kc, :]) for kc in range(8)],
                      [win_b, xn_b], self.psb[bk])
        P.op("act", lambda e, bk=bk: e.activation(out=alr[:], in_=self.ps[bk][0:16, :], func=AF.Copy),
             reads=[self.psb[bk]], writes=[alr_b])
        def partA(tb):
            tcs = slice(tb * 128, (tb + 1) * 128)
            ktm, ktm_b, _ = r_ktm.next()
            vsb, vsb_b, _ = r_vsb.next()
            sgw, sgw_b, _ = r_sgw.next()
            sp_, sp_b, _ = r_sp.next()
            bk = nb()
            self.mm_group(self.ps[bk][:], [(xn[:, kc, tcs], win[:, kc, 512:1024]) for kc in range(8)],
                          [win_b, xn_b], self.psb[bk])
            P.op("act", lambda e, bk=bk: e.activation(out=ktm[:], in_=self.ps[bk][:], func=AF.Copy),
                 reads=[self.psb[bk]], writes=[ktm_b])
            for half in range(2):
                bk = nb()
                self.mm_group(self.ps[bk][:], [(xn[:, kc, tcs], win[:, kc, 1024 + half * 512:1024 + (half + 1) * 512])
                                               for kc in range(8)], [win_b, xn_b], self.psb[bk])
                P.op("dve", lambda e, bk=bk, half=half: e.tensor_copy(out=vsb[:, half * 512:(half + 1) * 512],
                                                                      in_=self.ps[bk][:]),
                     reads=[self.psb[bk]], writes=[vsb_b])
            for half in range(2):
                bk = nb()
                self.mm_group(self.ps[bk][:], [(xn[:, kc, tcs], win[:, kc, 2048 + half * 512:2048 + (half + 1) * 512])
                                               for kc in range(8)], [win_b, xn_b], self.psb[bk])
                hs = slice(half * 512, (half + 1) * 512)
                P.op("act", lambda e, bk=bk, hs=hs: e.activation(out=sgw[:, hs], in_=self.ps[bk][:], func=AF.Silu),
                     reads=[self.psb[bk]], writes=[sgw_b])
                P.op("dve", lambda e, hs=hs: e.tensor_tensor(out=sgw[:, hs], in0=sgw[:, hs], in1=gnw[:, hs],
                                                             op=ALU.mult), reads=[sgw_b, gnw_b], writes=[sgw_b])
            bk = nb()
            self.mm_group(self.ps[bk][:], [(alr[:, tcs], wgb[:]), (ones1[:], bg[:])],
                          [alr_b, wgb_b, ones1_b, bg_b], self.psb[bk])
            P.op("act", lambda e, bk=bk: e.activation(out=sp_[:], in_=self.ps[bk][:], func=AF.Exp, scale=-1.0),
                 reads=[self.psb[bk]], writes=[sp_b])
            P.op("act", lambda e: e.activation(out=sp_[:], in_=sp_[:], func=AF.Ln, bias=c1[:], scale=1.0),
                 reads=[sp_b, c1_b], writes=[sp_b])
            return (ktm, ktm_b, vsb, vsb_b, sgw, sgw_b, sp_, sp_b)

        def partB(tb, bufs):
            tcs = slice(tb * 128, (tb + 1) * 128)
            ktm, ktm_b, vsb, vsb_b, sgw, sgw_b, sp_, sp_b = bufs
            bk = nb()
            self.mm_group(self.ps[bk][:], [(cum[:, 0, :], sp_[:])], [cum_b, sp_b], self.psb[bk])
            P.op("act", lambda e, bk=bk: e.activation(out=dend[:], in_=self.ps[bk][:], func=AF.Exp),
                 reads=[self.psb[bk]], writes=[dend_b])
            P.op("dve", lambda e: e.tensor_tensor(out=kend[:], in0=ktm[:], in1=dend[:], op=ALU.mult),
                 reads=[ktm_b, dend_b], writes=[kend_b])
            bk = nb()

            def cumT(e, bk=bk):
                ins = None
                for h in range(4):
                    ins = e.matmul(self.ps[bk][:, h * 128:(h + 1) * 128], sp_[:, h * 128:(h + 1) * 128],
                                   cum[:, 1, :], start=True, stop=True)
                return ins
            P.op("pe", cumT, reads=[sp_b, cum_b], writes=[self.psb[bk]])
            P.op("act", lambda e, bk=bk: e.activation(out=eb[:].rearrange("p h n -> p (h n)"), in_=self.ps[bk][:],
                                                      func=AF.Exp), reads=[self.psb[bk]], writes=[eb_b])
            P.op("act", lambda e, bk=bk: e.activation(out=enb[:].rearrange("p h n -> p (h n)"), in_=self.ps[bk][:],
                                                      func=AF.Exp, scale=-1.0), reads=[self.psb[bk]], writes=[enb_b])
            P.op("dve", lambda e, tcs=tcs: e.tensor_tensor(out=qdec[:], in0=qTf[:, :, tcs], in1=eb[:], op=ALU.mult),
                 reads=[qTf_b, eb_b], writes=[qdec_b])
            P.op("dve", lambda e, tcs=tcs: e.tensor_tensor(out=kinv[:], in0=kTf[:, :, tcs], in1=enb[:], op=ALU.mult),
                 reads=[kTf_b, enb_b], writes=[kinv_b])
            bk = nb()

            def att(e, bk=bk):
                ins = None
                for h in range(4):
                    ins = e.matmul(self.ps[bk][:, h * 128:(h + 1) * 128], kinv[:, h, :], qdec[:, h, :],
                                   start=True, stop=True)
                return ins
            P.op("pe", att, reads=[kinv_b, qdec_b], writes=[self.psb[bk]])
            P.op("dve", lambda e, bk=bk: e.tensor_tensor(out=attnT[:].rearrange("p h n -> p (h n)"), in0=self.ps[bk][:],
                                                         in1=tri4[:].rearrange("p h n -> p (h n)"), op=ALU.mult),
                 reads=[self.psb[bk], tri4_b], writes=[attnT_b])
            obanks = []
            for hp in range(2):
                bk = nb()
                obanks.append(bk)

                def omm(e, bk=bk, hp=hp):
                    ins = None
                    for hl in range(2):
                        h = hp * 2 + hl
                        e.matmul(self.ps[bk][:, hl * 256:(hl + 1) * 256], attnT[:, h, :], vsb[:, h * 256:(h + 1) * 256],
                                 start=True, stop=False)
                        ins = e.matmul(self.ps[bk][:, hl * 256:(hl + 1) * 256], qdec[:, h, :], stb[:, h, :],
                                       start=False, stop=True)
                    return ins
                P.op("pe", omm, reads=[attnT_b, vsb_b, qdec_b, stb_b[hp * 2], stb_b[hp * 2 + 1]],
                     writes=[self.psb[bk]])
            for hp in range(2):
                bk = nb()

                def smm(e, bk=bk, hp=hp):
                    ins = None
                    for hl in range(2):
                        h = hp * 2 + hl
                        ins = e.matmul(self.ps[bk][:, hl * 256:(hl + 1) * 256], kend[:, h * 128:(h + 1) * 128],
                                       vsb[:, h * 256:(h + 1) * 256], start=True, stop=True)
                    return ins
                P.op("pe", smm, reads=[kend_b, vsb_b], writes=[self.psb[bk]])
                for hl in range(2):
                    h = hp * 2 + hl
                    P.op("dve", lambda e, bk=bk, hl=hl, h=h: e.scalar_tensor_tensor(
                        out=stf[:, h, :], in0=stf[:, h, :], scalar=eb[:, h, 127:128],
                        in1=self.ps[bk][:, hl * 256:(hl + 1) * 256], op0=ALU.mult, op1=ALU.add),
                        reads=[stf_b[h], eb_b, self.psb[bk]], writes=[stf_b[h]])
                    P.op("pool", lambda e, h=h: e.tensor_copy(out=stb[:, h, :], in_=stf[:, h, :]),
                         reads=[stf_b[h]], writes=[stb_b[h]])
            for hp in range(2):
                bk = obanks[hp]
                for hl in range(2):
                    h = hp * 2 + hl
                    P.op("act", lambda e, bk=bk, hl=hl, h=h: e.activation(
                        out=junk[:], in_=self.ps[bk][:, hl * 256:(hl + 1) * 256], func=AF.Square,
                        accum_out=ssq[:, h:h + 1]), reads=[self.psb[bk]], writes=[junk_b, ssq_b[h]])
            P.op("act", lambda e: e.activation(out=orstd[:], in_=ssq[:], func=AF.Sqrt, bias=self.c_eps[:],
                                               scale=1.0 / 256), reads=ssq_b + [self.cb_const], writes=[orstd_b])
            P.op("dve", lambda e: e.reciprocal(out=orstd[:], in_=orstd[:]), reads=[orstd_b], writes=[orstd_b])
            for hp in range(2):
                bk = obanks[hp]
                for hl in range(2):
                    h = hp * 2 + hl
                    P.op("dve", lambda e, bk=bk, hl=hl, h=h: e.scalar_tensor_tensor(
                        out=og[:, h * 256:(h + 1) * 256], in0=self.ps[bk][:, hl * 256:(hl + 1) * 256],
                        scalar=orstd[:, h:h + 1], in1=sgw[:, h * 256:(h + 1) * 256], op0=ALU.mult, op1=ALU.mult),
                        reads=[self.psb[bk], orstd_b, sgw_b], writes=[og_b])
            pst, pst_b = self.pst[tb % 2], self.pstb[tb % 2]

            def tr(e, pst=pst):
                ins = None
                for c in range(8):
                    ins = e.transpose(pst[:, c * 128:(c + 1) * 128], og[:, c * 128:(c + 1) * 128], self.c_identb[:])
                return ins
            P.op("pe", tr, reads=[og_b, self.cb_const], writes=[pst_b])
            P.op("act", lambda e, pst=pst, tcs=tcs: e.activation(out=ogT[:, :, tcs],
                                                                in_=pst[:].rearrange("p (c n) -> p c n", c=8),
                                                                func=AF.Copy), reads=[pst_b], writes=[ogT_b[tb]])
        pa = {0: partA(0)}
        for tb in range(4):
            if tb + 1 < 4:
                pa[tb + 1] = partA(tb + 1)
            partB(tb, pa[tb])
        for o in range(8):
            bk = nb()
            self.mm_group(self.ps[bk][:], [(wo[:, kc, o * 128:(o + 1) * 128], ogT[:, kc, :]) for kc in range(8)],
                          [wo_b] + ogT_b, self.psb[bk])
            xr, xr_b, xr_ch = rxr.next()
            P.op("sp", lambda e, xr=xr, o=o, cols=cols: e.dma_start(out=xr[:], in_=self.xT[o * 128:(o + 1) * 128, cols]),
                 reads=[self.xTb[o][cb]], writes=[xr_b], dma=True, chan=xr_ch)
            xo, xo_b, xo_ch = rxo.next()
            P.op("dve", lambda e, xo=xo, xr=xr, bk=bk: e.tensor_tensor(out=xo[:], in0=self.ps[bk][:], in1=xr[:],
                                                                     op=ALU.add),
                 reads=[self.psb[bk], xr_b], writes=[xo_b])
            P.op("sp", lambda e, xo=xo, o=o, cols=cols: e.dma_start(out=self.xT[o * 128:(o + 1) * 128, cols], in_=xo[:]),
                 reads=[xo_b], writes=[self.xTb[o][cb]], dma=True, chan=xo_ch)
    P.barrier()
    A.reset(m0)


K.gla = _gla


def gla_host(inputs, pre, name):
    if name == pre + "_gla_w_in" or name == pre + "_gla_w_out":
        return lay_cols(inputs[name])
    if name == pre + "_gla_w_gate_b":
        return np.ascontiguousarray(np.asarray(inputs[name], np.float32))
    if name == pre + "_gla_b_gate":
        return np.ascontiguousarray(np.asarray(inputs[name], np.float32).reshape(1, 512))
    if name == pre + "_gla_norm":
        return np.ascontiguousarray(np.broadcast_to(np.asarray(inputs[name], np.float32)[None, :], (128, 1024)))
    if name == pre + "_mix_norm":
        return lay_vecp(inputs[name])
    return None


def gla_consts(name):
    j = np.arange(128)[:, None]
    i = np.arange(128)[None, :]
    if name == "c_gla_cum":
        ms = np.where(j > i, -1.0 / 16.0, 0.0)
        mi = np.where(j <= i, -1.0 / 16.0, 0.0)
        tri = np.where(j <= i, 1.0, 0.0)
        return np.ascontiguousarray(np.concatenate([ms, mi, tri], axis=1).astype(np.float32))
    if name == "c_tri4":
        tri = np.where(j <= i, 1.0, 0.0)
        return np.ascontiguousarray(np.concatenate([tri] * 4, axis=1).astype(np.float32))
    return None


def _ssd(self, pre):
    P, A, S, NCB = self.P, self.A, self.S, self.NCB
    nw_d = self.dram_in(pre + "_mix_norm", [128, 8])
    wz_d = self.dram_in(pre + "_ssd_wz", [16, 128, 1024])
    wx_d = self.dram_in(pre + "_ssd_wxbc", [24, 128, 1024])
    wdt_d = self.dram_in(pre + "_ssd_wdt", [128, 8 * 32])
    cw_d = self.dram_in(pre + "_ssd_conv_w", [128, 24 * 4])
    cbias_d = self.dram_in(pre + "_ssd_conv_b", [128, 24])
    dtb_d = self.dram_in(pre + "_ssd_dt_bias", [128, 32])
    alog_d = self.dram_in(pre + "_ssd_a_log", [128, 32])
    dsk_d = self.dram_in(pre + "_ssd_d_skip", [128, 32])
    snw_d = self.dram_in(pre + "_ssd_norm", [128, 16])
    wo_d = self.dram_in(pre + "_ssd_w_out", [8, 128, 2048])
    cs_d = self.dram_in("c_ssd", [128, 3 * 512])
    m0 = A.mark()

    def ld(shape, dtype, src, eng="sp", name="w", view=None):
        t = A.alloc(shape, dtype, name)
        b = P.buf(name)
        dst = t[:] if view is None else view(t)
        P.op(eng, lambda e: e.dma_start(out=dst, in_=src), writes=[b], dma=True, chan=P.chan(name, eng))
        return t, b

    nw, nw_b = ld([128, 8], F32, nw_d, name="nw")
    wdt, wdt_b = ld([128, 8, 32], BF16, wdt_d, "pool", "wdt", lambda t: t[:].rearrange("p k n -> p (k n)"))
    cw, cw_b = ld([128, 24, 4], F32, cw_d, name="cw", view=lambda t: t[:].rearrange("p c k -> p (c k)"))
    cbias, cbias_b = ld([128, 24], F32, cbias_d, name="cbias")
    dtb, dtb_b = ld([128, 32], F32, dtb_d, name="dtb")
    ealog, ealog_b = ld([128, 32], F32, alog_d, name="alog")
    dsk, dsk_b = ld([128, 32], F32, dsk_d, name="dsk")
    snw, snw_b = ld([128, 16], F32, snw_d, name="snw")
    cs, cs_b = ld([128, 3, 512], F32, cs_d, name="cssd", view=lambda t: t[:].rearrange("p a n -> p (a n)"))
    ident4 = cs[:, 0, :]
    maskT4 = cs[:, 1, :]
    tri = cs[:, 2, 0:128]
    sel = cs[:, 2, 128:256]
    onesf = cs[:, 2, 256:384]
    P.op("act", lambda e: e.activation(out=ealog[:], in_=ealog[:], func=AF.Exp), reads=[ealog_b], writes=[ealog_b])
    c1 = A.alloc([128, 1], F32, "c1")
    c1_b = P.buf("c1")
    P.op("dve", lambda e: e.memset(c1[:], 1.0), writes=[c1_b])

    xin = A.alloc([128, 8, 512], F32, "xin")
    xin_b = P.buf("xin")
    xin_ch = P.chan("xin")
    sq = A.alloc([128, 8, 512], BF16, "sq")
    sq_b = P.buf("sq")
    rstd = A.alloc([128, 512], F32, "rstd")
    rstd_b = P.buf("rstd")
    xn = A.alloc([128, 8, 512], BF16, "xn")
    xn_b = P.buf("xn")
    rw = Ring(self, 3, [128, 8, 128], BF16, "wst", eng="pool")
    ru = Ring(self, 2, [128, 515], F32, "u", chan=False)
    racc = Ring(self, 2, [128, 512], F32, "acc", chan=False)
    rxc = Ring(self, 2, [128, 512], BF16, "xc", chan=False)
    halo = A.alloc([128, 24, 3], F32, "halo")
    halo_b = [P.buf(f"halo{c}") for c in range(24)]
    P.op("pool", lambda e: e.memset(halo[:], 0.0), writes=halo_b)
    xtm = A.alloc([128, 4, 2048], BF16, "xtm")
    xtm_b = [P.buf(f"xtm{c}") for c in range(16)]
    btm = A.alloc([128, 4, 512], BF16, "btm")
    btm_b = [P.buf(f"btm{g}") for g in range(4)]
    BT = A.alloc([128, 4, 512], BF16, "BT")
    BT_b = [P.buf(f"BT{g}") for g in range(4)]
    CT = A.alloc([128, 4, 512], BF16, "CT")
    CT_b = [P.buf(f"CT{g}") for g in range(4)]
    szT = A.alloc([128, 16, 512], BF16, "szT")
    szT_b = [P.buf(f"szT{c}") for c in range(16)]
    dtp = A.alloc([128, 4, 32], F32, "dtp")
    dtp_b = P.buf("dtp")
    dt = A.alloc([128, 4, 32], F32, "dt")
    dt_b = P.buf("dt")
    dta = A.alloc([128, 4, 32], F32, "dta")
    dta_b = P.buf("dta")
    acs = A.alloc([128, 4, 32], F32, "acs")
    acs_b = P.buf("acs")
    ea = A.alloc([128, 4, 32], F32, "ea")
    ea_b = P.buf("ea")
    dec = A.alloc([128, 4, 32], F32, "dec")
    dec_b = P.buf("dec")
    r2 = A.alloc([128, 8, 64], F32, "r2")
    r2_b = P.buf("r2")
    rtot = Ring(self, 2, [128, 512], F32, "totbc", chan=False)
    xdt = A.alloc([128, 4, 512], BF16, "xdt")
    xdt_b = [P.buf(f"xdt{g}") for g in range(4)]
    xdd = A.alloc([128, 4, 512], BF16, "xdd")
    xdd_b = [P.buf(f"xdd{g}") for g in range(4)]
    cbT = A.alloc([128, 4, 128], F32, "cbT")
    cbT_b = P.buf("cbT")
    rD = Ring(self, 2, [128, 4, 128], F32, "Dm", chan=False)
    rR = Ring(self, 2, [128, 4, 128], F32, "r23", chan=False)
    rLT = Ring(self, 2, [128, 4, 128], F32, "LT", chan=False)
    rWT = Ring(self, 2, [128, 4, 128], BF16, "WT", chan=False)
    stf = A.alloc([128, 4, 512], F32, "sstf")
    stf_b = [P.buf(f"sstf{g}") for g in range(4)]
    stb = A.alloc([128, 4, 512], BF16, "sstb")
    stb_b = [P.buf(f"sstb{g}") for g in range(4)]
    for g in range(4):
        P.op("dve", lambda e, g=g: e.memset(stf[:, g, :], 0.0), writes=[stf_b[g]])
        P.op("pool", lambda e, g=g: e.memset(stb[:, g, :], 0.0), writes=[stb_b[g]])
    rtmp = Ring(self, 2, [128, 8, 64], F32, "ytmp", chan=False)
    rtmp2 = Ring(self, 2, [128, 8, 64], F32, "ytmp2", chan=False)
    ry = Ring(self, 2, [128, 512], F32, "yg", chan=False)
    ygT = A.alloc([128, 16, 128], F32, "ygT")
    ygT_b = [P.buf(f"ygT{g}") for g in range(4)]
    ysq = A.alloc([128, 16, 128], BF16, "ysq")
    ysq_b = [P.buf(f"ysq{g}") for g in range(4)]
    yrs = A.alloc([128, 4, 128], F32, "yrs")
    yrs_b = P.buf("yrs")
    ynT = A.alloc([128, 16, 512], BF16, "ynT")
    ynT_b = [[P.buf(f"ynT{t}_{kc}") for kc in range(16)] for t in range(4)]
    rwo = Ring(self, 2, [128, 16, 128], BF16, "swo", eng="pool")
    rxr = Ring(self, 2, [128, 512], F32, "xres")
    rxo = Ring(self, 2, [128, 512], F32, "xo")
    bank = [0]
    cpi = [0]

    def nb():
        bank[0] = (bank[0] + 1) % 5
        return bank[0]

    def bc(ap2d, n):
        k = ap2d.shape[1]
        return ap2d.unsqueeze(2).to_broadcast([128, k, n])

    for cb in range(NCB):
        cols = slice(cb * 512, (cb + 1) * 512)
        self.load_xcols(cb, xin, xin_b, xin_ch)
        self.norm_block(cb, xin, xin_b, sq, sq_b, rstd, rstd_b, nw, nw_b, 5, lambda c: (xn[:, c, :], xn_b))
        bk = nb()

        def dtmm(e, bk=bk):
            ins = None
            for tb in range(4):
                for kc in range(8):
                    ins = e.matmul(self.ps[bk][:, tb * 32:(tb + 1) * 32], xn[:, kc, tb * 128:(tb + 1) * 128],
                                   wdt[:, kc, :], start=(kc == 0), stop=(kc == 7))
            return ins
        P.op("pe", dtmm, reads=[xn_b, wdt_b], writes=[self.psb[bk]])
        P.op("dve", lambda e, bk=bk: e.tensor_tensor(
            out=dtp[:], in0=self.ps[bk][:, 0:128].rearrange("p (t h) -> p t h", t=4),
            in1=dtb[:].unsqueeze(1).to_broadcast([128, 4, 32]), op=ALU.add),
            reads=[self.psb[bk], dtb_b], writes=[dtp_b])
        P.op("act", lambda e: e.activation(out=dtp[:], in_=dtp[:], func=AF.Exp), reads=[dtp_b], writes=[dtp_b])
        P.op("act", lambda e: e.activation(out=dt[:], in_=dtp[:], func=AF.Ln, bias=c1[:], scale=1.0),
             reads=[dtp_b, c1_b], writes=[dt_b])
        P.op("dve", lambda e: e.scalar_tensor_tensor(out=dta[:], in0=dt[:], scalar=-1.0,
                                                     in1=ealog[:].unsqueeze(1).to_broadcast([128, 4, 32]),
                                                     op0=ALU.mult, op1=ALU.mult),
             reads=[dt_b, ealog_b], writes=[dta_b])
        bk = nb()
        self.mm_group(self.ps[bk][:, 0:128], [(tri, dta[:].rearrange("p t h -> p (t h)"))], [cs_b, dta_b], self.psb[bk])
        P.op("dve", lambda e, bk=bk: e.tensor_copy(out=acs[:].rearrange("p t h -> p (t h)"), in_=self.ps[bk][:, 0:128]),
             reads=[self.psb[bk]], writes=[acs_b])
        P.op("act", lambda e: e.activation(out=ea[:], in_=acs[:], func=AF.Exp), reads=[acs_b], writes=[ea_b])
        bk = nb()
        self.mm_group(self.ps[bk][:, 0:128], [(sel, acs[:].rearrange("p t h -> p (t h)"))], [cs_b, acs_b], self.psb[bk])
        P.op("dve", lambda e, bk=bk: e.tensor_tensor(out=dec[:].rearrange("p t h -> p (t h)"), in0=self.ps[bk][:, 0:128],
                                                     in1=acs[:].rearrange("p t h -> p (t h)"), op=ALU.subtract),
             reads=[self.psb[bk], acs_b], writes=[dec_b])
        P.op("act", lambda e: e.activation(out=dec[:], in_=dec[:], func=AF.Exp), reads=[dec_b], writes=[dec_b])
        for c in range(24):
            w, w_b, w_ch = rw.next()
            P.op("pool", lambda e, w=w, c=c: e.dma_start(out=w[:].rearrange("p k j -> p (k j)"), in_=wx_d[c]),
                 writes=[w_b], dma=True, chan=w_ch)
            bk = nb()
            self.mm_group(self.ps[bk][:], [(w[:, kc, :], xn[:, kc, :]) for kc in range(8)], [w_b, xn_b], self.psb[bk])
            u, u_b, _ = ru.next()
            P.op("act", lambda e, u=u, bk=bk: e.activation(out=u[:, 3:515], in_=self.ps[bk][:], func=AF.Copy),
                 reads=[self.psb[bk]], writes=[u_b])
            P.op("pool", lambda e, u=u, c=c: e.tensor_copy(out=u[:, 0:3], in_=halo[:, c, :]),
                 reads=[halo_b[c]], writes=[u_b])
            P.op("pool", lambda e, u=u, c=c: e.tensor_copy(out=halo[:, c, :], in_=u[:, 512:515]),
                 reads=[u_b], writes=[halo_b[c]])
            acc, acc_b, _ = racc.next()
            P.op("dve", lambda e, u=u, acc=acc, c=c: e.tensor_scalar(acc[:], u[:, 0:512], cw[:, c, 0:1], None, ALU.mult),
                 reads=[u_b, cw_b], writes=[acc_b])
            for k in range(1, 4):
                P.op("dve", lambda e, u=u, acc=acc, c=c, k=k: e.scalar_tensor_tensor(
                    out=acc[:], in0=u[:, k:k + 512], scalar=cw[:, c, k:k + 1], in1=acc[:], op0=ALU.mult, op1=ALU.add),
                    reads=[u_b, cw_b, acc_b], writes=[acc_b])
            if c < 20:
                xc, xc_b, _ = rxc.next()
                if c >= 16:
                    dst, dst_b = BT[:, c - 16, :], BT_b[c - 16]
                    P.op("act", lambda e, acc=acc, dst=dst, c=c: e.activation(out=dst, in_=acc[:], func=AF.Silu,
                                                                            bias=cbias[:, c:c + 1], scale=1.0),
                         reads=[acc_b, cbias_b], writes=[dst_b])
                    src, src_b = dst, dst_b
                else:
                    P.op("act", lambda e, acc=acc, xc=xc, c=c: e.activation(out=xc[:], in_=acc[:], func=AF.Silu,
                                                                          bias=cbias[:, c:c + 1], scale=1.0),
                         reads=[acc_b, cbias_b], writes=[xc_b])
                    src, src_b = xc[:], xc_b
                pst, pst_b = self.pst[c % 2], self.pstb[c % 2]

                def tr(e, pst=pst, src=src):
                    ins = None
                    for tb in range(4):
                        ins = e.transpose(pst[:, tb * 128:(tb + 1) * 128], src[:, tb * 128:(tb + 1) * 128],
                                          self.c_identb[:])
                    return ins
                P.op("pe", tr, reads=[src_b, self.cb_const], writes=[pst_b])
                if c < 16:
                    o_ap, o_b = xtm[:, :, c * 128:(c + 1) * 128], xtm_b[c]
                else:
                    o_ap, o_b = btm[:, :, (c - 16) * 128:(c - 15) * 128], btm_b[c - 16]
                cpi[0] += 1
                i_ap = pst[:, 0:512].rearrange("p (t n) -> p t n", t=4)
                if cpi[0] % 2 == 0:
                    P.op("act", lambda e, o_ap=o_ap, i_ap=i_ap: e.activation(out=o_ap, in_=i_ap, func=AF.Copy),
                         reads=[pst_b], writes=[o_b])
                else:
                    P.op("dve", lambda e, o_ap=o_ap, i_ap=i_ap: e.tensor_copy(out=o_ap, in_=i_ap),
                         reads=[pst_b], writes=[o_b])
            else:
                g = c - 20
                P.op("act", lambda e, acc=acc, g=g, c=c: e.activation(out=CT[:, g, :], in_=acc[:], func=AF.Silu,
                                                                    bias=cbias[:, c:c + 1], scale=1.0),
                     reads=[acc_b, cbias_b], writes=[CT_b[g]])
        for c in range(16):
            w, w_b, w_ch = rw.next()
            P.op("pool", lambda e, w=w, c=c: e.dma_start(out=w[:].rearrange("p k j -> p (k j)"), in_=wz_d[c]),
                 writes=[w_b], dma=True, chan=w_ch)
            bk = nb()
            self.mm_group(self.ps[bk][:], [(w[:, kc, :], xn[:, kc, :]) for kc in range(8)], [w_b, xn_b], self.psb[bk])
            P.op("act", lambda e, c=c, bk=bk: e.activation(out=szT[:, c, :], in_=self.ps[bk][:], func=AF.Silu),
                 reads=[self.psb[bk]], writes=[szT_b[c]])
        for tb in range(4):
            tcs = slice(tb * 128, (tb + 1) * 128)
            for g in range(4):
                gs = slice(g * 8, (g + 1) * 8)
                gc = slice(g * 512, (g + 1) * 512)
                P.op("pool", lambda e, g=g, gs=gs, gc=gc, tb=tb: e.tensor_tensor(
                    out=xdt[:, g, :].rearrange("p (h n) -> p h n", h=8),
                    in0=xtm[:, tb, gc].rearrange("p (h n) -> p h n", h=8), in1=bc(dt[:, tb, gs], 64), op=ALU.mult),
                    reads=xtm_b[g * 4:(g + 1) * 4] + [dt_b], writes=[xdt_b[g]])
                P.op("pool", lambda e, g=g, gs=gs, tb=tb: e.tensor_tensor(
                    out=xdd[:, g, :].rearrange("p (h n) -> p h n", h=8),
                    in0=xdt[:, g, :].rearrange("p (h n) -> p h n", h=8), in1=bc(dec[:, tb, gs], 64), op=ALU.mult),
                    reads=[xdt_b[g], dec_b], writes=[xdd_b[g]])
            bk = nb()

            def cbmm(e, bk=bk, tcs=tcs):
                ins = None
                for g in range(4):
                    ins = e.matmul(self.ps[bk][:, g * 128:(g + 1) * 128], BT[:, g, tcs], CT[:, g, tcs],
                                   start=True, stop=True)
                return ins
            P.op("pe", cbmm, reads=BT_b + CT_b, writes=[self.psb[bk]])
            P.op("act", lambda e, bk=bk: e.activation(out=cbT[:].rearrange("p g n -> p (g n)"), in_=self.ps[bk][:],
                                                      func=AF.Copy), reads=[self.psb[bk]], writes=[cbT_b])
            for g in range(4):
                gs = slice(g * 8, (g + 1) * 8)
                gc = slice(g * 512, (g + 1) * 512)
                ybk = nb()
                ydms = []
                for sl in range(2):
                    h0 = g * 8 + sl * 4
                    hs = slice(h0, h0 + 4)
                    Dm, Dm_b, _ = rD.next()
                    r23, r23_b, _ = rR.next()
                    P.op("dve", lambda e, Dm=Dm, hs=hs, tb=tb: e.tensor_tensor(
                        out=Dm[:], in0=ident4.rearrange("p (h n) -> p h n", h=4), in1=bc(acs[:, tb, hs], 128), op=ALU.mult),
                        reads=[cs_b, acs_b], writes=[Dm_b])
                    P.op("pool", lambda e, r23=r23, hs=hs, tb=tb: e.tensor_tensor(
                        out=r23[:], in0=maskT4.rearrange("p (h n) -> p h n", h=4), in1=bc(acs[:, tb, hs], 128),
                        op=ALU.subtract), reads=[cs_b, acs_b], writes=[r23_b])
                    bk = nb()
                    if bk == ybk:
                        bk = nb()
                    self.mm_group(self.ps[bk][:], [(onesf, Dm[:].rearrange("p h n -> p (h n)")),
                                                   (self.c_identf[:], r23[:].rearrange("p h n -> p (h n)"))],
                                  [cs_b, Dm_b, r23_b, self.cb_const], self.psb[bk])
                    LT, LT_b, _ = rLT.next()
                    P.op("act", lambda e, LT=LT, bk=bk: e.activation(out=LT[:].rearrange("p h n -> p (h n)"),
                                                                   in_=self.ps[bk][:], func=AF.Exp),
                         reads=[self.psb[bk]], writes=[LT_b])
                    WT, WT_b, _ = rWT.next()
                    P.op("dve", lambda e, WT=WT, LT=LT, g=g: e.tensor_tensor(
                        out=WT[:], in0=LT[:], in1=cbT[:, g:g + 1, :].to_broadcast([128, 4, 128]), op=ALU.mult),
                        reads=[LT_b, cbT_b], writes=[WT_b])

                    def ydm(e, WT=WT, ybk=ybk, g=g, sl=sl):
                        ins = None
                        for hl in range(4):
                            hh = sl * 4 + hl
                            ins = e.matmul(self.ps[ybk][:, hh * 64:(hh + 1) * 64], WT[:, hl, :],
                                           xdt[:, g, hh * 64:(hh + 1) * 64], start=True, stop=True)
                        return ins
                    ydms.append((ydm, WT_b))
                for ydm, WT_b in ydms:
                    P.op("pe", ydm, reads=[WT_b, xdt_b[g]], writes=[self.psb[ybk]])
                obk = nb()
                if obk == ybk:
                    obk = nb()
                self.mm_group(self.ps[obk][:], [(CT[:, g, tcs], stb[:, g, :])], [CT_b[g], stb_b[g]], self.psb[obk])
                tmp, tmp_b, _ = rtmp.next()
                tmp2, tmp2_b, _ = rtmp2.next()
                P.op("dve", lambda e, tmp=tmp, obk=obk, gs=gs, tb=tb: e.tensor_tensor(
                    out=tmp[:], in0=self.ps[obk][:].rearrange("p (h n) -> p h n", h=8), in1=bc(ea[:, tb, gs], 64),
                    op=ALU.mult), reads=[self.psb[obk], ea_b], writes=[tmp_b])
                P.op("pool", lambda e, tmp2=tmp2, gs=gs, gc=gc, tb=tb: e.tensor_tensor(
                    out=tmp2[:], in0=xtm[:, tb, gc].rearrange("p (h n) -> p h n", h=8), in1=bc(dsk[:, gs], 64),
                    op=ALU.mult), reads=xtm_b[g * 4:(g + 1) * 4] + [dsk_b], writes=[tmp2_b])
                P.op("pool", lambda e, tmp=tmp, tmp2=tmp2: e.tensor_tensor(out=tmp[:], in0=tmp[:], in1=tmp2[:],
                                                                          op=ALU.add),
                     reads=[tmp_b, tmp2_b], writes=[tmp_b])
                y, y_b, _ = ry.next()
                P.op("dve", lambda e, y=y, tmp=tmp, ybk=ybk: e.tensor_tensor(
                    out=y[:], in0=self.ps[ybk][:], in1=tmp[:].rearrange("p h n -> p (h n)"), op=ALU.add),
                    reads=[self.psb[ybk], tmp_b], writes=[y_b])
                P.op("pool", lambda e, gs=gs, tb=tb: e.tensor_copy(out=r2[:], in_=bc(acs[:, tb, gs], 64)),
                     reads=[acs_b], writes=[r2_b])
                bk = nb()
                self.mm_group(self.ps[bk][:], [(sel, r2[:].rearrange("p h n -> p (h n)"))], [cs_b, r2_b], self.psb[bk])
                tot, tot_b, _ = rtot.next()
                P.op("act", lambda e, bk=bk, tot=tot: e.activation(out=tot[:], in_=self.ps[bk][:], func=AF.Exp),
                     reads=[self.psb[bk]], writes=[tot_b])
                sbk = nb()
                self.mm_group(self.ps[sbk][:], [(btm[:, tb, g * 128:(g + 1) * 128], xdd[:, g, :])],
                              [btm_b[g], xdd_b[g]], self.psb[sbk])
                P.op("dve", lambda e, g=g, tot=tot: e.tensor_tensor(out=stf[:, g, :], in0=stf[:, g, :], in1=tot[:],
                                                                    op=ALU.mult),
                     reads=[stf_b[g], tot_b], writes=[stf_b[g]])
                P.op("dve", lambda e, g=g, sbk=sbk: e.tensor_tensor(out=stf[:, g, :], in0=stf[:, g, :],
                                                                    in1=self.ps[sbk][:], op=ALU.add),
                     reads=[stf_b[g], self.psb[sbk]], writes=[stf_b[g]])
                P.op("act", lambda e, g=g: e.activation(out=stb[:, g, :], in_=stf[:, g, :], func=AF.Copy),
                     reads=[stf_b[g]], writes=[stb_b[g]])
                tbk = nb()

                def ytr(e, tbk=tbk, y=y):
                    ins = None
                    for q in range(4):
                        ins = e.transpose(self.ps[tbk][:, q * 128:(q + 1) * 128], y[:, q * 128:(q + 1) * 128],
                                          self.c_identf[:])
                    return ins
                P.op("pe", ytr, reads=[y_b, self.cb_const], writes=[self.psb[tbk]])
                P.op("dve", lambda e, tbk=tbk, g=g, tcs=tcs: e.tensor_tensor(
                    out=ygT[:, g * 4:(g + 1) * 4, :], in0=self.ps[tbk][:].rearrange("p (q n) -> p q n", q=4),
                    in1=szT[:, g * 4:(g + 1) * 4, tcs], op=ALU.mult),
                    reads=[self.psb[tbk]] + szT_b[g * 4:(g + 1) * 4], writes=[ygT_b[g]])
                P.op("act", lambda e, g=g: e.activation(out=ysq[:, g * 4:(g + 1) * 4, :], in_=ygT[:, g * 4:(g + 1) * 4, :],
                                                        func=AF.Square), reads=[ygT_b[g]], writes=[ysq_b[g]])
            nbk = nb()

            def nrm(e, nbk=nbk):
                ins = None
                for g in range(4):
                    for q in range(4):
                        ins = e.matmul(self.ps[nbk][:, g * 128:(g + 1) * 128], self.c_onesb[:], ysq[:, g * 4 + q, :],
                                       start=(q == 0), stop=(q == 3))
                return ins
            P.op("pe", nrm, reads=ysq_b + [self.cb_const], writes=[self.psb[nbk]])
            P.op("act", lambda e, nbk=nbk: e.activation(out=yrs[:].rearrange("p g n -> p (g n)"), in_=self.ps[nbk][:],
                                                        func=AF.Sqrt, bias=self.c_eps[:], scale=1.0 / 512),
                 reads=[self.psb[nbk], self.cb_const], writes=[yrs_b])
            P.op("dve", lambda e: e.reciprocal(out=yrs[:], in_=yrs[:]), reads=[yrs_b], writes=[yrs_b])
            for kc in range(16):
                g = kc // 4
                P.op("dve", lambda e, kc=kc, g=g, tcs=tcs: e.scalar_tensor_tensor(
                    out=ynT[:, kc, tcs], in0=ygT[:, kc, :], scalar=snw[:, kc:kc + 1], in1=yrs[:, g, :],
                    op0=ALU.mult, op1=ALU.mult), reads=[ygT_b[g], snw_b, yrs_b], writes=[ynT_b[tb][kc]])
        for o in range(8):
            wo, wo_b, wo_ch = rwo.next()
            P.op("pool", lambda e, wo=wo, o=o: e.dma_start(out=wo[:].rearrange("p k j -> p (k j)"), in_=wo_d[o]),
                 writes=[wo_b], dma=True, chan=wo_ch)
            bk = nb()
            self.mm_group(self.ps[bk][:], [(wo[:, kc, :], ynT[:, kc, :]) for kc in range(16)],
                          [wo_b] + [ynT_b[t][kc] for t in range(4) for kc in range(16)],
                          self.psb[bk])
            xr, xr_b, xr_ch = rxr.next()
            P.op("sp", lambda e, xr=xr, o=o, cols=cols: e.dma_start(out=xr[:], in_=self.xT[o * 128:(o + 1) * 128, cols]),
                 reads=[self.xTb[o][cb]], writes=[xr_b], dma=True, chan=xr_ch)
            xo, xo_b, xo_ch = rxo.next()
            P.op("dve", lambda e, xo=xo, xr=xr, bk=bk: e.tensor_tensor(out=xo[:], in0=self.ps[bk][:], in1=xr[:],
                                                                     op=ALU.add),
                 reads=[self.psb[bk], xr_b], writes=[xo_b])
            P.op("sp", lambda e, xo=xo, o=o, cols=cols: e.dma_start(out=self.xT[o * 128:(o + 1) * 128, cols], in_=xo[:]),
                 reads=[xo_b], writes=[self.xTb[o][cb]], dma=True, chan=xo_ch)
    P.barrier()
    A.reset(m0)


K.ssd = _ssd


def lay_chunks(w):
    w = np.asarray(w, np.float32)
    kc, n = w.shape[0] // 128, w.shape[1] // 128
    a = w.reshape(kc, 128, n, 128).transpose(2, 1, 0, 3)
    return np.ascontiguousarray(a).reshape(n, 128, kc * 128)


def rep128(v):
    v = np.asarray(v, np.float32).reshape(1, -1)
    return np.ascontiguousarray(np.broadcast_to(v, (128, v.shape[1])))


def ssd_host(inputs, pre, name):
    if name == pre + "_ssd_wz":
        return lay_chunks(np.asarray(inputs[pre + "_ssd_w_in"])[:, 0:2048])
    if name == pre + "_ssd_wxbc":
        return lay_chunks(np.asarray(inputs[pre + "_ssd_w_in"])[:, 2048:5120])
    if name == pre + "_ssd_wdt":
        return lay_cols(np.asarray(inputs[pre + "_ssd_w_in"])[:, 5120:5152])
    if name == pre + "_ssd_conv_w":
        w = np.asarray(inputs[name], np.float32)
        return np.ascontiguousarray(w.reshape(4, 24, 128).transpose(2, 1, 0)).reshape(128, 96)
    if name == pre + "_ssd_conv_b":
        return lay_vecp(inputs[name])
    if name in (pre + "_ssd_dt_bias", pre + "_ssd_a_log", pre + "_ssd_d_skip"):
        return rep128(inputs[name])
    if name == pre + "_ssd_norm" or name == pre + "_mix_norm":
        return lay_vecp(inputs[name])
    if name == pre + "_ssd_w_out":
        return lay_chunks(inputs[name])
    return None


def ssd_consts(name):
    if name != "c_ssd":
        return None
    j = np.arange(128)[:, None]
    i = np.arange(128)[None, :]
    ident = (j == i).astype(np.float32)
    maskT = np.where(i < j, -30000.0, 0.0).astype(np.float32)
    tri = (j <= i).astype(np.float32)
    sel = np.zeros((128, 128), np.float32)
    sel[127, :] = 1.0
    ones = np.ones((128, 128), np.float32)
    z = np.zeros((128, 128), np.float32)
    return np.ascontiguousarray(np.concatenate([ident] * 4 + [maskT] * 4 + [tri, sel, ones, z], axis=1))
```
